# Optimizing a Trainium2 kernel written in Bass

```python
import math
import functools
import jax
import jax.numpy as jnp
from jax import lax
import numpy as np

D_MODEL = 1024
BATCH = 16
SEQ = 2048
DEPTH = 2

GRID_W = 64
CTX_LEN = 256
F32 = jnp.float32

HY_W = 384
HY_ORDER = 2
HY_SHORT = 3
HY_EMB = 33
HY_HID = 64
RET_HEADS = 6
RET_DH = 64
RET_W = RET_HEADS * RET_DH
RET_CHUNK = 64
ROPE_BASE = 10000.0
ML_HEADS = 4
ML_DH = 64
ML_W = ML_HEADS * ML_DH
ML_SHORT = 3
ML_CHUNK = 64

MIX_W = HY_W + RET_W + ML_W
N_BRANCH = 3
HY_OFF = 0
RET_OFF = HY_OFF + 3 * HY_W
ML_OFF = RET_OFF + 4 * RET_W
MLG_OFF = ML_OFF + 4 * ML_W
GATE_OFF = MLG_OFF + 4 * ML_HEADS
N_IN = GATE_OFF + N_BRANCH * D_MODEL

D_FF = ((8 * D_MODEL // 3 + 127) // 128) * 128
N_EXPERTS = 8
TOP_K = 2
N_DENSE = (DEPTH + 1) // 2
N_MOE = DEPTH // 2
EPS = 1e-6

kernel_name = 'hybrid_hyena_retnet_mlstm_moe_dit'


def rmsnorm(x, g):
    xf = x.astype(F32)
    y = xf * lax.rsqrt(jnp.mean(xf * xf, axis=-1, keepdims=True) + EPS)
    return (y * g.astype(F32)).astype(x.dtype)


def modulate(h, shift, scale):
    return h * (1 + scale) + shift


def head_norm(x):
    mu = jnp.mean(x, axis=-1, keepdims=True)
    xc = x - mu
    return xc * lax.rsqrt(jnp.mean(xc * xc, axis=-1, keepdims=True) + EPS)


def split_heads(x, n_heads):
    b, l, w = x.shape
    return x.reshape(b, l, n_heads, w // n_heads).transpose(0, 2, 1, 3)


def merge_heads(x):
    b, h, l, d = x.shape
    return x.transpose(0, 2, 1, 3).reshape(b, l, h * d)


def short_conv(u, w):
    k_w = w.shape[0]
    l = u.shape[1]
    pad = k_w // 2
    up = jnp.pad(u, ((0, 0), (pad, k_w - 1 - pad), (0, 0)))
    return sum(up[:, j:j + l] * w[j] for j in range(k_w))


def to_chunks(x, ch):
    b, h, l = x.shape[:3]
    x = x.reshape(b, h, l // ch, ch, *x.shape[3:])
    return jnp.moveaxis(x, 2, 0)


def from_chunks(y):
    n, b, h, ch = y.shape[:4]
    return jnp.moveaxis(y, 0, 2).reshape(b, h, n * ch, *y.shape[4:])


def flip_seq(a):
    return jnp.flip(a, axis=2)


def hyena_filters(length, p):
    pos = jnp.arange(length, dtype=F32)
    t = pos / max(length - 1, 1)
    n_bands = (HY_EMB - 1) // 2
    bands = jnp.linspace(1e-4, n_bands - 1, n_bands, dtype=F32)
    z = (2.0 * math.pi * pos / length)[:, None] * bands[None, :]
    feats = jnp.concatenate([t[:, None], jnp.cos(z), -jnp.sin(z)], axis=-1)
    hid = jnp.sin(feats @ p['hy_f1'].astype(F32) + p['hy_fb1'].astype(F32))
    hid = jnp.sin(hid @ p['hy_f2'].astype(F32) + p['hy_fb2'].astype(F32))
    h = hid @ p['hy_f3'].astype(F32)
    window = jnp.exp(-t[:, None] * jnp.abs(p['hy_decay'].astype(F32))[None, :])
    return (h * window).reshape(length, HY_ORDER, 2, HY_W)


def long_conv(u, h_fwd, h_bwd, skip):
    b, l, ch = u.shape
    filt = jnp.concatenate([h_fwd, jnp.zeros((1, ch), F32), h_bwd[1:][::-1]], axis=0)
    u_f = jnp.fft.rfft(u.astype(F32), n=2 * l, axis=1)
    k_f = jnp.fft.rfft(filt, axis=0)
    y = jnp.fft.irfft(u_f * k_f[None], n=2 * l, axis=1)[:, :l]
    return y + u.astype(F32) * skip.astype(F32)


def hyena_branch(u, p):
    length = u.shape[1]
    u = short_conv(u, p['hy_conv'])
    v, x1, x2 = jnp.split(u, 3, axis=-1)
    filt = hyena_filters(length, p)
    z = v.astype(F32)
    for o, gate in enumerate((x1, x2)):
        z = gate.astype(F32) * long_conv(z, filt[:, o, 0], filt[:, o, 1], p['hy_skip'][o])
    return z


def rotary_tables(row_id, col_id):
    q4 = RET_DH // 4
    inv = 1.0 / (ROPE_BASE ** (jnp.arange(q4, dtype=F32) / q4))
    ang_r = row_id.astype(F32)[:, None] * inv[None, :]
    ang_c = col_id.astype(F32)[:, None] * inv[None, :]
    return (jnp.cos(ang_r), jnp.sin(ang_r), jnp.cos(ang_c), jnp.sin(ang_c))


def apply_axial_rotary(x, rope):
    cos_r, sin_r, cos_c, sin_c = rope
    half = RET_DH // 2
    q4 = half // 2

    def rot(y, cs, sn):
        y1, y2 = y[..., :q4], y[..., q4:]
        return jnp.concatenate([y1 * cs - y2 * sn, y1 * sn + y2 * cs], axis=-1)

    return jnp.concatenate([rot(x[..., :half], cos_r, sin_r), rot(x[..., half:], cos_c, sin_c)], axis=-1)


def retention_inputs(u, rope):
    seg = u[..., RET_OFF:ML_OFF].astype(F32)
    q, k, v, g = jnp.split(seg, 4, axis=-1)
    q = split_heads(q, RET_HEADS) * RET_DH ** -0.5
    k = split_heads(k, RET_HEADS)
    v = split_heads(v, RET_HEADS)
    if rope is not None:
        q = apply_axial_rotary(q, rope)
        k = apply_axial_rotary(k, rope)
    return q, k, v, g


def retention_scan(q, k, v, log_g, s0):
    idx = jnp.arange(RET_CHUNK, dtype=F32)
    rel = idx[:, None] - idx[None, :]
    causal = rel >= 0
    intra_decay = jnp.where(causal, jnp.exp(jnp.where(causal, rel, 0.0) * log_g[:, None, None]), 0.0)
    q_decay = jnp.exp((idx + 1.0) * log_g[:, None])
    k_decay = jnp.exp((RET_CHUNK - 1.0 - idx) * log_g[:, None])
    chunk_decay = jnp.exp(RET_CHUNK * log_g)[:, None, None]

    def step(s, qkv):
        qc, kc, vc = qkv
        scores = jnp.einsum('bhtd,bhsd->bhts', qc, kc) * intra_decay
        o = jnp.einsum('bhts,bhse->bhte', scores, vc) + jnp.einsum('bhtd,bhde->bhte', qc, s) * q_decay[..., None]
        s = s * chunk_decay + jnp.einsum('bhsd,bhse->bhde', kc * k_decay[..., None], vc)
        return s, o

    s_fin, o = lax.scan(step, s0, (to_chunks(q, RET_CHUNK), to_chunks(k, RET_CHUNK), to_chunks(v, RET_CHUNK)))
    return from_chunks(o), s_fin


def retention_bidir(q, k, v, log_gamma, s_fwd0, s_bwd0):
    o_f, s_f = retention_scan(q, k, v, log_gamma[0], s_fwd0)
    o_b, s_b = retention_scan(flip_seq(q), flip_seq(k), flip_seq(v), log_gamma[1], s_bwd0)
    return o_f + flip_seq(o_b), s_f, s_b


def retention_out(o, g):
    return merge_heads(head_norm(o)) * jax.nn.silu(g)


def mlstm_inputs(u, p):
    b, l = u.shape[:2]
    qk = jax.nn.silu(short_conv(u[..., ML_OFF:ML_OFF + 2 * ML_W], p['ml_conv'])).astype(F32)
    q = split_heads(qk[..., :ML_W], ML_HEADS)
    k = split_heads(qk[..., ML_W:], ML_HEADS) * ML_DH ** -0.5
    v = split_heads(u[..., ML_OFF + 2 * ML_W:ML_OFF + 3 * ML_W].astype(F32), ML_HEADS)
    o = u[..., ML_OFF + 3 * ML_W:MLG_OFF].astype(F32)
    gates = u[..., MLG_OFF:GATE_OFF].astype(F32).reshape(b, l, 2, 2, ML_HEADS) + p['ml_gate_bias'].astype(F32)
    gates = jnp.moveaxis(gates, 1, -1)
    return q, k, v, o, gates[:, :, 0], jax.nn.log_sigmoid(gates[:, :, 1])


def mlstm_scan(q, k, v, ig, lf, state):
    idx = jnp.arange(ML_CHUNK)
    causal = idx[:, None] >= idx[None, :]

    def step(carry, inp):
        c_mat, n_vec, m = carry
        qc, kc, vc, ic, fc = inp
        b = jnp.cumsum(fc, axis=-1)
        dlog = jnp.where(causal, b[..., :, None] - b[..., None, :] + ic[..., None, :], -jnp.inf)
        inter = b + m[..., None]
        m_t = jnp.maximum(inter, jnp.max(dlog, axis=-1))
        w = jnp.exp(dlog - m_t[..., None])
        a = jnp.exp(inter - m_t)
        s = jnp.einsum('bhtd,bhsd->bhts', qc, kc) * w
        num = jnp.einsum('bhts,bhse->bhte', s, vc) + a[..., None] * jnp.einsum('bhtd,bhde->bhte', qc, c_mat)
        den = jnp.sum(s, axis=-1) + a * jnp.einsum('bhtd,bhd->bht', qc, n_vec)
        h = num / jnp.maximum(jnp.abs(den), jnp.exp(-m_t))[..., None]
        b_last = b[..., -1]
        wlog = b_last[..., None] - b + ic
        m_new = jnp.maximum(b_last + m, jnp.max(wlog, axis=-1))
        wk = jnp.exp(wlog - m_new[..., None])
        a_s = jnp.exp(b_last + m - m_new)
        c_mat = a_s[..., None, None] * c_mat + jnp.einsum('bhs,bhsd,bhse->bhde', wk, kc, vc)
        n_vec = a_s[..., None] * n_vec + jnp.einsum('bhs,bhsd->bhd', wk, kc)
        return (c_mat, n_vec, m_new), h

    xs = (to_chunks(q, ML_CHUNK), to_chunks(k, ML_CHUNK), to_chunks(v, ML_CHUNK),
          to_chunks(ig, ML_CHUNK), to_chunks(lf, ML_CHUNK))
    state, h = lax.scan(step, state, xs)
    return from_chunks(h), state


def mlstm_bidir(q, k, v, ig, lf, st_fwd0, st_bwd0):
    h_f, st_f = mlstm_scan(q, k, v, ig[:, 0], lf[:, 0], st_fwd0)
    h_b, st_b = mlstm_scan(flip_seq(q), flip_seq(k), flip_seq(v), flip_seq(ig[:, 1]), flip_seq(lf[:, 1]), st_bwd0)
    return h_f + flip_seq(h_b), st_f, st_b


def mlstm_out(h, o):
    return jax.nn.sigmoid(o) * merge_heads(head_norm(h))


def merge_branches(u, ys, p):
    b, l = u.shape[:2]
    gates = jax.nn.sigmoid(u[..., GATE_OFF:].reshape(b, l, N_BRANCH, D_MODEL))
    bounds = (0, HY_W, HY_W + RET_W, MIX_W)
    acc = 0
    for i, y in enumerate(ys):
        acc = acc + gates[:, :, i] * (y.astype(u.dtype) @ p['w_up'][bounds[i]:bounds[i + 1]])
    return acc @ p['w_out']


def mixer(h_lat, h_ctx, rope, p, need_ctx):
    batch = h_lat.shape[0]
    u_lat = h_lat @ p['w_in'] + p['b_in']
    u_ctx = h_ctx @ p['w_in'] + p['b_in']
    log_gamma = jax.nn.log_sigmoid(p['ret_decay'].astype(F32))
    s0 = jnp.zeros((batch, RET_HEADS, RET_DH, RET_DH), F32)
    q_c, k_c, v_c, g_c = retention_inputs(u_ctx, None)
    r_ctx, s_fwd, s_bwd = retention_bidir(q_c, k_c, v_c, log_gamma, s0, s0)
    q_l, k_l, v_l, g_l = retention_inputs(u_lat, rope)
    r_lat, _, _ = retention_bidir(q_l, k_l, v_l, log_gamma, s_fwd, s_bwd)
    st0 = (jnp.zeros((batch, ML_HEADS, ML_DH, ML_DH), F32),
           jnp.zeros((batch, ML_HEADS, ML_DH), F32),
           jnp.zeros((batch, ML_HEADS), F32))
    mq_c, mk_c, mv_c, mo_c, ig_c, lf_c = mlstm_inputs(u_ctx, p)
    m_ctx, st_fwd, st_bwd = mlstm_bidir(mq_c, mk_c, mv_c, ig_c, lf_c, st0, st0)
    mq_l, mk_l, mv_l, mo_l, ig_l, lf_l = mlstm_inputs(u_lat, p)
    m_lat, _, _ = mlstm_bidir(mq_l, mk_l, mv_l, ig_l, lf_l, st_fwd, st_bwd)
    y_lat = merge_branches(u_lat, (hyena_branch(u_lat[..., HY_OFF:RET_OFF], p),
                                   retention_out(r_lat, g_l),
                                   mlstm_out(m_lat, mo_l)), p)
    if not need_ctx:
        return y_lat, None
    y_ctx = merge_branches(u_ctx, (hyena_branch(u_ctx[..., HY_OFF:RET_OFF], p),
                                   retention_out(r_ctx, g_c),
                                   mlstm_out(m_ctx, mo_c)), p)
    return y_lat, y_ctx


def swiglu(h, w1, w3, w2):
    return (jax.nn.silu(h @ w1) * (h @ w3)) @ w2


def moe_ffn(h, w_router, b_router, w1, w3, w2):
    logits = (h @ w_router + b_router).astype(F32)
    top_val, top_idx = lax.top_k(logits, TOP_K)
    top_w = jax.nn.softmax(top_val, axis=-1)
    combine = jnp.sum(jax.nn.one_hot(top_idx, N_EXPERTS, dtype=F32) * top_w[..., None], axis=-2).astype(h.dtype)
    out = 0
    for e in range(N_EXPERTS):
        out = out + combine[..., e:e + 1] * swiglu(h, w1[e], w3[e], w2[e])
    return out


def setup_inputs(seed: int = 0) -> dict:
    key = jax.random.key(seed)
    ks = jax.random.split(key, 40)

    def nrm(k, shape, s):
        return jax.random.normal(k, shape, F32) * s

    x = nrm(ks[0], (BATCH, SEQ, D_MODEL), 1.0)
    c = nrm(ks[1], (BATCH, D_MODEL), 1.0)
    ctx = nrm(ks[2], (BATCH, CTX_LEN, D_MODEL), 1.0)
    c_ctx = nrm(ks[3], (D_MODEL,), 1.0)
    w_mod = nrm(ks[4], (DEPTH, D_MODEL, 6 * D_MODEL), 0.5 * D_MODEL ** -0.5)
    b_mod = nrm(ks[5], (DEPTH, 6 * D_MODEL), 0.02)
    g_mix = 1.0 + nrm(ks[6], (DEPTH, D_MODEL), 0.02)
    g_ffn = 1.0 + nrm(ks[7], (DEPTH, D_MODEL), 0.02)
    w_in = nrm(ks[8], (DEPTH, D_MODEL, N_IN), D_MODEL ** -0.5)
    b_in = nrm(ks[9], (DEPTH, N_IN), 0.02)
    hy_conv = nrm(ks[10], (DEPTH, HY_SHORT, 3 * HY_W), HY_SHORT ** -0.5)
    hy_f1 = nrm(ks[11], (DEPTH, HY_EMB, HY_HID), HY_EMB ** -0.5)
    hy_fb1 = nrm(ks[12], (DEPTH, HY_HID), 0.1)
    hy_f2 = nrm(ks[13], (DEPTH, HY_HID, HY_HID), HY_HID ** -0.5)
    hy_fb2 = nrm(ks[14], (DEPTH, HY_HID), 0.1)
    hy_f3 = nrm(ks[15], (DEPTH, HY_HID, HY_ORDER * 2 * HY_W), 0.01)
    base_decay = jnp.linspace(abs(math.log(1e-2)) / 1.5, abs(math.log(1e-2)) / 0.3, HY_W, dtype=F32)
    base_decay = jnp.broadcast_to(base_decay[None, :], (HY_ORDER * 2, HY_W)).reshape(-1)
    hy_decay = base_decay * (1.0 + nrm(ks[16], (DEPTH, HY_ORDER * 2 * HY_W), 0.05))
    hy_skip = nrm(ks[17], (DEPTH, HY_ORDER, HY_W), 0.5)
    gamma = 1.0 - 2.0 ** (-5.0 - jnp.arange(RET_HEADS, dtype=F32))
    ret_decay = jnp.log(gamma / (1.0 - gamma)) + nrm(ks[18], (DEPTH, 2, RET_HEADS), 0.1)
    ml_conv = nrm(ks[19], (DEPTH, ML_SHORT, 2 * ML_W), ML_SHORT ** -0.5)
    ig_bias = nrm(ks[20], (DEPTH, 2, ML_HEADS), 0.1)
    fg_bias = jnp.linspace(3.0, 6.0, ML_HEADS, dtype=F32) + nrm(ks[21], (DEPTH, 2, ML_HEADS), 0.1)
    ml_gate_bias = jnp.stack([ig_bias, fg_bias], axis=2)
    row_scale = jnp.concatenate([jnp.full((HY_W,), HY_W ** -0.5, F32),
                                 jnp.full((RET_W,), RET_W ** -0.5, F32),
                                 jnp.full((ML_W,), ML_W ** -0.5, F32)])
    w_up = nrm(ks[22], (DEPTH, MIX_W, D_MODEL), 1.0) * row_scale[:, None]
    w_out = nrm(ks[23], (DEPTH, D_MODEL, D_MODEL), D_MODEL ** -0.5)
    ffn_w1 = nrm(ks[24], (N_DENSE, D_MODEL, D_FF), D_MODEL ** -0.5)
    ffn_w3 = nrm(ks[25], (N_DENSE, D_MODEL, D_FF), D_MODEL ** -0.5)
    ffn_w2 = nrm(ks[26], (N_DENSE, D_FF, D_MODEL), D_FF ** -0.5)
    moe_router = nrm(ks[27], (N_MOE, D_MODEL, N_EXPERTS), D_MODEL ** -0.5)
    moe_router_b = nrm(ks[28], (N_MOE, N_EXPERTS), 0.01)
    moe_w1 = nrm(ks[29], (N_MOE, N_EXPERTS, D_MODEL, D_FF), D_MODEL ** -0.5)
    moe_w3 = nrm(ks[30], (N_MOE, N_EXPERTS, D_MODEL, D_FF), D_MODEL ** -0.5)
    moe_w2 = nrm(ks[31], (N_MOE, N_EXPERTS, D_FF, D_MODEL), D_FF ** -0.5)
    g_final = 1.0 + nrm(ks[32], (D_MODEL,), 0.02)
    return {'x': x, 'c': c, 'ctx': ctx, 'c_ctx': c_ctx, 'w_mod': w_mod, 'b_mod': b_mod,
            'g_mix': g_mix, 'g_ffn': g_ffn, 'w_in': w_in, 'b_in': b_in, 'hy_conv': hy_conv,
            'hy_f1': hy_f1, 'hy_fb1': hy_fb1, 'hy_f2': hy_f2, 'hy_fb2': hy_fb2, 'hy_f3': hy_f3,
            'hy_decay': hy_decay, 'hy_skip': hy_skip, 'ret_decay': ret_decay, 'ml_conv': ml_conv,
            'ml_gate_bias': ml_gate_bias, 'w_up': w_up, 'w_out': w_out, 'ffn_w1': ffn_w1,
            'ffn_w3': ffn_w3, 'ffn_w2': ffn_w2, 'moe_router': moe_router, 'moe_router_b': moe_router_b,
            'moe_w1': moe_w1, 'moe_w3': moe_w3, 'moe_w2': moe_w2, 'g_final': g_final}


def reference(x, c, ctx, c_ctx, w_mod, b_mod, g_mix, g_ffn, w_in, b_in, hy_conv, hy_f1, hy_fb1,
              hy_f2, hy_fb2, hy_f3, hy_decay, hy_skip, ret_decay, ml_conv, ml_gate_bias, w_up, w_out,
              ffn_w1, ffn_w3, ffn_w2, moe_router, moe_router_b, moe_w1, moe_w3, moe_w2, g_final):
    batch, n_lat, _ = x.shape
    ROWS = n_lat // GRID_W
    row_id = jnp.broadcast_to(jnp.arange(ROWS)[:, None], (ROWS, GRID_W)).reshape(-1)
    col_id = jnp.broadcast_to(jnp.arange(GRID_W)[None, :], (ROWS, GRID_W)).reshape(-1)
    rope = rotary_tables(row_id, col_id)
    s_lat = jax.nn.silu(c)
    s_ctx = jax.nn.silu(c_ctx)
    xl, xc = x, ctx
    for layer in range(DEPTH):
        last = layer == DEPTH - 1
        mod_l = (s_lat @ w_mod[layer] + b_mod[layer]).reshape(batch, 6, 1, D_MODEL)
        mod_c = (s_ctx @ w_mod[layer] + b_mod[layer]).reshape(6, D_MODEL)
        p = {'w_in': w_in[layer], 'b_in': b_in[layer], 'hy_conv': hy_conv[layer],
             'hy_f1': hy_f1[layer], 'hy_fb1': hy_fb1[layer], 'hy_f2': hy_f2[layer],
             'hy_fb2': hy_fb2[layer], 'hy_f3': hy_f3[layer], 'hy_decay': hy_decay[layer],
             'hy_skip': hy_skip[layer], 'ret_decay': ret_decay[layer], 'ml_conv': ml_conv[layer],
             'ml_gate_bias': ml_gate_bias[layer], 'w_up': w_up[layer], 'w_out': w_out[layer]}
        hl = modulate(rmsnorm(xl, g_mix[layer]), mod_l[:, 0], mod_l[:, 1])
        hc = modulate(rmsnorm(xc, g_mix[layer]), mod_c[0], mod_c[1])
        yl, yc = mixer(hl, hc, rope, p, not last)
        xl = xl + mod_l[:, 2] * yl
        if not last:
            xc = xc + mod_c[2] * yc
        j = layer // 2
        if layer % 2 == 0:
            ffn = functools.partial(swiglu, w1=ffn_w1[j], w3=ffn_w3[j], w2=ffn_w2[j])
        else:
            ffn = functools.partial(moe_ffn, w_router=moe_router[j], b_router=moe_router_b[j],
                                    w1=moe_w1[j], w3=moe_w3[j], w2=moe_w2[j])
        fl = modulate(rmsnorm(xl, g_ffn[layer]), mod_l[:, 3], mod_l[:, 4])
        xl = xl + mod_l[:, 5] * ffn(fl)
        if not last:
            fc = modulate(rmsnorm(xc, g_ffn[layer]), mod_c[3], mod_c[4])
            xc = xc + mod_c[5] * ffn(fc)
    return rmsnorm(xl, g_final)
```

```python
import contextlib
import math
import types
import numpy as np
import ml_dtypes
import concourse.bass as bass
import concourse.mybir as mybir
from concourse.bass_utils import run_bass_kernel_spmd

F32 = mybir.dt.float32
BF16 = mybir.dt.bfloat16
AF = mybir.ActivationFunctionType
ALU = mybir.AluOpType
AX = mybir.AxisListType

NCORES = 8
D = 1024
KC = 8
LAT = 2048
CTX = 256
TAU = CTX + LAT
NB = 2
DEPTH = 2
N_IN = 6800
D_FF = 2816
FC = 22
NEXP = 8
EPS = 1e-6
HY_W = 384
OFF_HY = 0
OFF_RQ, OFF_RK, OFF_RV, OFF_RG = 1152, 1536, 1920, 2304
OFF_MQ, OFF_MK, OFF_MV, OFF_MO = 2688, 2944, 3200, 3456
OFF_MG = 3712
OFF_GATE = 3728

NDMASEM = 8


def _freeze(fn, depth=0):
    if not isinstance(fn, types.FunctionType) or fn.__closure__ is None or depth > 3:
        return fn
    cells = []
    for c in fn.__closure__:
        try:
            v = c.cell_contents
        except ValueError:
            cells.append(c)
            continue
        if isinstance(v, types.FunctionType):
            v = _freeze(v, depth + 1)
        cells.append(types.CellType(v))
    g = types.FunctionType(fn.__code__, fn.__globals__, fn.__name__, fn.__defaults__, tuple(cells))
    g.__kwdefaults__ = fn.__kwdefaults__
    return g


class Op:
    __slots__ = ("eng", "fn", "deps", "dma", "needs_inc", "semval", "slot", "prev_slot_op")

    def __init__(self, eng, fn, dma):
        self.eng = eng
        self.fn = fn
        self.deps = []
        self.dma = dma
        self.needs_inc = False
        self.semval = None
        self.slot = None
        self.prev_slot_op = None


class Prog:
    ENGS = ("pe", "act", "dve", "pool", "sp")

    def __init__(self, nc):
        self.nc = nc
        self.ops = {e: [] for e in self.ENGS}
        self.last_w = {}
        self.readers = {}
        self.dma_count = {e: 0 for e in self.ENGS}
        self.dma_slot_last = {}
        self.last_op = {}
        self.nops = 0

    def begin_capture(self):
        self._cap = []

    def end_capture(self):
        c, self._cap = self._cap, None
        return c

    def replay(self, *lists):
        total = sum(len(l) for l in lists)
        pos = [0] * len(lists)
        for _ in range(total):
            best, bf_ = None, None
            for i, l in enumerate(lists):
                if pos[i] < len(l):
                    f = pos[i] / len(l)
                    if bf_ is None or f < bf_:
                        best, bf_ = i, f
            eng, fn, reads, writes, dma = lists[best][pos[best]]
            pos[best] += 1
            self.op(eng, fn, reads, writes, dma)

    def op(self, eng, fn, reads=(), writes=(), dma=False):
        if getattr(self, "_cap", None) is not None:
            self._cap.append((eng, _freeze(fn), list(reads), list(writes), dma))
            return None
        o = Op(eng, _freeze(fn), dma)
        self.nops += 1
        deps = set()
        for k in reads:
            w = self.last_w.get(k)
            if w is not None:
                deps.add(w)
        for k in writes:
            w = self.last_w.get(k)
            if w is not None:
                deps.add(w)
            for r in self.readers.get(k, ()):
                deps.add(r)
        o.deps = list(deps)
        for d in o.deps:
            d.needs_inc = True
        for k in reads:
            self.readers.setdefault(k, []).append(o)
        for k in writes:
            self.last_w[k] = o
            self.readers[k] = []
        if dma:
            j = self.dma_count[eng]
            self.dma_count[eng] = j + 1
            o.slot = (eng, j % NDMASEM)
            o.prev_slot_op = self.dma_slot_last.get(o.slot)
            self.dma_slot_last[o.slot] = o
            o.needs_inc = True
        else:
            self.last_op[eng] = o
        self.ops[eng].append(o)
        return o

    def dma(self, out, in_, reads, writes, eng="sp", **kw):
        return self.op(eng, lambda e: e.dma_start(out=out, in_=in_, **kw), reads, writes, dma=True)

    def barrier(self):
        deps = list(self.last_op.values()) + list(self.dma_slot_last.values())
        for d in deps:
            d.needs_inc = True
        for e in self.ENGS:
            o = Op(e, None, False)
            o.deps = list(deps)
            self.ops[e].append(o)
        self.last_w = {}
        self.readers = {}

    def emit(self, final_ops):
        nc = self.nc
        with contextlib.ExitStack() as st:
            csem = {e: st.enter_context(nc.semaphore("cs_" + e)) for e in ("pe", "act", "dve", "pool")}
            dsem = {}
            for e in self.ENGS:
                if self.dma_count[e] > 0:
                    for s in range(NDMASEM):
                        dsem[(e, s)] = st.enter_context(nc.semaphore("ds_%s_%d" % (e, s)))
            for e in self.ENGS:
                cnt = 0
                dcnt = {}
                for o in self.ops[e]:
                    if o.dma:
                        dcnt[o.slot] = dcnt.get(o.slot, 0) + 16
                        o.semval = dcnt[o.slot]
                    elif o.needs_inc and o.fn is not None:
                        cnt += 1
                        o.semval = cnt
            block = st.enter_context(nc.Block())
            engobj = {"pe": block.tensor, "act": block.scalar, "dve": block.vector,
                      "pool": block.gpsimd, "sp": block.sync}

            def make(e):
                ops = self.ops[e]

                def body(eng):
                    waited = {}

                    def need(semkey, sem, val):
                        if val is None or waited.get(semkey, 0) >= val:
                            return
                        waited[semkey] = val
                        eng.wait_ge(sem, val)

                    for o in ops:
                        for d in o.deps:
                            if d.dma:
                                need(d.slot, dsem[d.slot], d.semval)
                            else:
                                need(d.eng, csem[d.eng], d.semval)
                        if o.dma and o.prev_slot_op is not None:
                            p = o.prev_slot_op
                            need(p.slot, dsem[p.slot], p.semval)
                        if o.fn is None:
                            continue
                        ins = o.fn(eng)
                        if isinstance(ins, (list, tuple)):
                            ins = ins[-1]
                        if o.dma:
                            ins.then_inc(dsem[o.slot], 16)
                        elif o.needs_inc:
                            ins.then_inc(csem[e], 1)
                    if e == "sp":
                        for o in final_ops:
                            need(o.slot, dsem[o.slot], o.semval)
                return body

            for e in self.ENGS:
                engobj[e](make(e))


F_CHUNKS = []
for j in range(9):
    F_CHUNKS.append(("uhy", j, OFF_HY + 128 * j, "id"))
for j in range(3):
    F_CHUNKS.append(("rq", j, OFF_RQ + 128 * j, "id"))
for j in range(3):
    F_CHUNKS.append(("rk", j, OFF_RK + 128 * j, "id"))
for j in range(2):
    F_CHUNKS.append(("mq", j, OFF_MQ + 128 * j, "id"))
for j in range(2):
    F_CHUNKS.append(("mk", j, OFF_MK + 128 * j, "id"))
for j in range(3):
    F_CHUNKS.append(("rg", j, OFF_RG + 128 * j, "silu"))
for j in range(2):
    F_CHUNKS.append(("mo", j, OFF_MO + 128 * j, "sig"))
for j in range(24):
    F_CHUNKS.append(("gate", j, OFF_GATE + 128 * j, "sig"))
SEG_NCH = {"uhy": 9, "rq": 3, "rk": 3, "mq": 2, "mk": 2, "rg": 3, "mo": 2, "gate": 24}


class Builder:
    def __init__(self, dbg=(), stop_after=None):
        self.nc = bass.Bass("TRN2", target_bir_lowering=False)
        self.P = Prog(self.nc)
        self.dbg = set(dbg)
        self.stop_after = stop_after
        self.dram = {}
        self.final_ops = []

    def din(self, name, shape, dt=F32):
        t = self.nc.dram_tensor(name, list(shape), dt, kind="ExternalInput").ap()
        self.dram[name] = t
        return t

    def dscr(self, name, shape, dt):
        kind = "ExternalOutput" if name in self.dbg else "Internal"
        t = self.nc.dram_tensor(name, list(shape), dt, kind=kind).ap()
        self.dram[name] = t
        return t

    def dout(self, name, shape, dt=F32):
        t = self.nc.dram_tensor(name, list(shape), dt, kind="ExternalOutput").ap()
        self.dram[name] = t
        return t

    def build(self):
        nc, P = self.nc, self.P
        din, dscr = self.din, self.dscr
        self.xin = din("xin", [NB, KC, 128, TAU])
        self.sT = din("sT", [128, KC, 3])
        self.w_mod = din("w_mod", [DEPTH, 128, KC, 6 * D])
        self.b_modT = din("b_modT", [DEPTH, 128, 48])
        self.g_mixT = din("g_mixT", [DEPTH, 128, KC])
        self.g_ffnT = din("g_ffnT", [DEPTH, 128, KC])
        self.g_finT = din("g_finT", [128, KC])
        self.w_in = din("w_in", [DEPTH, 128, KC, N_IN])
        self.b_inF = din("b_inF", [DEPTH, 128, len(F_CHUNKS)])
        self.b_in = din("b_in", [DEPTH, N_IN])
        self.ident_d = din("ident", [128, 128])
        self.hy_f1 = din("hy_f1", [DEPTH, 33, 64])
        self.hy_f2 = din("hy_f2", [DEPTH, 64, 64])
        self.hy_f3 = din("hy_f3", [DEPTH, 64, 1536])
        self.hy_fb1 = din("hy_fb1", [DEPTH, 64, 1])
        self.hy_fb2 = din("hy_fb2", [DEPTH, 64, 1])
        self.hy_decay = din("hy_decay", [DEPTH, 1536])
        self.hy_convT = din("hy_convT", [DEPTH, 128, 9, 3])
        self.hy_skipT = din("hy_skipT", [DEPTH, 128, 2, 3])
        self.rope_cos = din("rope_cos", [128, LAT])
        self.rope_sin = din("rope_sin", [128, LAT])
        self.rope_RT = din("rope_RT", [128, 128], BF16)
        self.ml_convT = din("ml_convT", [DEPTH, 128, 4, 3])
        self.w_up = din("w_up", [DEPTH, D, D])
        self.w_out = din("w_out", [DEPTH, D, D])
        self.ffn_w1 = din("ffn_w1", [1, D, D_FF])
        self.ffn_w3 = din("ffn_w3", [1, D, D_FF])
        self.ffn_w2 = din("ffn_w2", [1, D_FF, D])
        self.outT = self.dout("outT", [NB, KC, 128, LAT])
        self.moe_router = din("moe_router", [1, D, NEXP])
        self.moe_router_b = din("moe_router_b", [1, NEXP])
        self.moe_w1 = din("moe_w1", [1, NEXP, D, D_FF])
        self.moe_w3 = din("moe_w3", [1, NEXP, D, D_FF])
        self.moe_w2 = din("moe_w2", [1, NEXP, D_FF, D])
        self.moe_sel = din("moe_sel", [NEXP, NEXP, 128])
        self.ret_decay = din("ret_decay", [DEPTH, 2, 6])
        self.ret_decay_h = din("ret_decay_h", [DEPTH, 2, 2, 3])
        self.ml_gate_bias = din("ml_gate_bias", [DEPTH, 16])
        self.attc = {}
        for nm, shp, dt in (("U", [128, 128], F32), ("L", [128, 128], F32), ("Df", [128, 128], F32), ("Db", [128, 128], F32),
                            ("NEGf", [128, 128], F32), ("NEGb", [128, 128], F32), ("io", [128, 256], F32), ("ioc", [128, 2], F32),
                            ("J", [128, 128], BF16)):
            self.attc[nm] = din("ac_" + nm, shp, dt)
        self.hyc = {}
        for tag, L in (("l", LAT), ("c", CTX)):
            kch = L // 128
            self.hyc[tag] = {
                "featsT": din("hc_featsT_" + tag, [33, L]),
                "tcol": din("hc_tcol_" + tag, [128, kch]),
                "fC": din("hc_fC_" + tag, [kch, 128, kch, 128], BF16),
                "fS": din("hc_fS_" + tag, [kch, 128, kch, 128], BF16),
                "iC": din("hc_iC_" + tag, [L // min(512, L), 128, kch, min(512, L)], BF16),
                "iS": din("hc_iS_" + tag, [L // min(512, L), 128, kch, min(512, L)], BF16),
            }
        self.seg = {}
        for s, n in SEG_NCH.items():
            self.seg[s] = dscr("s_" + s, [NB, n, 128, TAU], BF16)
        self.s_rv = dscr("s_rv", [NB, TAU // 128, 128, 384], BF16)
        self.s_mv = dscr("s_mv", [NB, TAU // 128, 128, 256], BF16)
        self.s_mg = dscr("s_mg", [NB, TAU // 128, 128, 16], F32)
        self.s_mod = dscr("s_mod", [DEPTH, 128, 48, 3], F32)
        self.s_hyc = dscr("s_hyc", [NB, 9, 128, TAU], BF16)
        self.s_ktok = dscr("s_ktok", [NB, TAU // 128, 128, 640], BF16)
        self.s_hffn = dscr("s_hffn", [NB, KC, 128, LAT], BF16)
        self.s_comb = dscr("s_comb", [NEXP, NB * LAT], F32)
        self.s_yatt = dscr("s_yatt", [NB, 5, 128, TAU], BF16)
        self.s_z1 = dscr("s_z1", [NB, 3, 128, TAU], BF16)
        self.seg_yhy = dscr("s_yhy", [NB, 3, 128, TAU], BF16)
        self.s_ztok = {"l": dscr("s_ztok_l", [2, LAT // 128, 128, 768], BF16),
                       "c": dscr("s_ztok_c", [2, CTX // 128, 128, 768], BF16)}
        self.s_fs = {"l": dscr("s_fs_l", [2, 2, LAT // 128, 128, 384], BF16),
                     "c": dscr("s_fs_c", [2, 2, CTX // 128, 128, 384], BF16)}
        self.xa = dscr("xa", [NB, KC, 128, TAU], F32)
        self.xb = dscr("xb", [NB, KC, 128, TAU], F32)

        with contextlib.ExitStack() as st:
            self.gst = st
            self.bank = [st.enter_context(nc.psum_tensor("bank%d" % i, [128, 512], F32)) for i in range(8)]
            if self.stop_after == "moe_only":
                xa_in = self.din("xa_in", [NB, KC, 128, TAU])
                self.s_mod = self.din("smod_in", [DEPTH, 128, 48, 3])
                self.phase_E_moe(1, xa_in, self.xb)
            else:
                self.phase_mod()
            if self.stop_after not in ("mod", "moe_only"):
                for layer in range(DEPTH):
                    xsrc = self.xin if layer == 0 else self.xb
                    self.phase_A(layer, xsrc)
                    if self.stop_after == "A%d" % layer:
                        break
                    if "skipB" not in self.dbg:
                        self.phase_B(layer)
                    if self.stop_after == "B%d" % layer:
                        break
                    self.phase_C1(layer)
                    if self.stop_after == "C1%d" % layer:
                        break
                    self.phase_C2(layer)
                    if self.stop_after == "C2%d" % layer:
                        break
                    self.phase_D(layer, xsrc, self.xa)
                    if self.stop_after == "D%d" % layer:
                        break
                    if layer % 2 == 0:
                        self.phase_E_dense(layer, self.xa, self.xb)
                    else:
                        self.phase_E_moe(layer, self.xa, self.xb)
                    if self.stop_after == "E%d" % layer:
                        break
                else:
                    self.phase_final(self.xb)
            P.barrier()
            P.emit(self.final_ops)
        return nc

    def pk(self, i):
        return "bank%d" % i

    def mk_sb(self, st):
        self.uid = getattr(self, "uid", 0) + 1
        uid = self.uid
        return lambda name, shape, dt: st.enter_context(self.nc.sbuf_tensor("%s_u%d" % (name, uid), shape, dt))

    def phase_mod(self):
        nc, P = self.nc, self.P
        with contextlib.ExitStack() as st:
            sb = self.mk_sb(st)
            sT = sb("m_sT", [128, KC, 3], F32)
            sS = sb("m_sS", [128, KC, 3], F32)
            wblk = [sb("m_w%d" % i, [128, KC, 512], F32) for i in range(2)]
            bm = sb("m_b", [128, 48], F32)
            res = sb("m_res", [128, 48, 3], F32)
            P.dma(sT[:], self.sT[:, :, :], [], ["m_sT"])
            P.op("act", lambda e: e.activation(out=sS[:], in_=sT[:], func=AF.Silu), ["m_sT"], ["m_sS"])
            for layer in range(DEPTH):
                P.dma(bm[:], self.b_modT[layer], [], ["m_b"])
                for blk in range(12):
                    wb = wblk[blk % 2]
                    wk = "m_w%d" % (blk % 2)
                    P.dma(wb[:], self.w_mod[layer, :, :, blk * 512:(blk + 1) * 512], [], [wk],
                          eng=("sp" if blk % 2 == 0 else "pool"))
                    for m in range(4):
                        col = blk * 4 + m
                        bk = self.pk(col % 2)
                        ps = self.bank[col % 2]

                        def fn(e, wb=wb, m=m, ps=ps):
                            r = []
                            for k in range(KC):
                                r.append(e.matmul(ps[:, 0:3], wb[:, k, m * 128:(m + 1) * 128], sS[:, k, :],
                                                  start=(k == 0), stop=(k == KC - 1)))
                            return r
                        P.op("pe", fn, [wk, "m_sS"], [bk])
                        P.op("dve", lambda e, col=col, ps=ps: e.tensor_scalar(
                            out=res[:, col, :], in0=ps[:, 0:3], scalar1=bm[:, col:col + 1], scalar2=None,
                            op0=ALU.add), ["m_b"], [bk, ("m_res", col)])
                P.dma(self.s_mod[layer], res[:], [("m_res", c) for c in range(48)], [("s_mod", layer)])
            P.barrier()

    def phase_A(self, layer, xsrc):
        nc, P = self.nc, self.P
        last = layer == DEPTH - 1
        with contextlib.ExitStack() as st:
            sb = self.mk_sb(st)
            W = sb("a_w", [128, KC, N_IN], BF16)
            modt = sb("a_mod", [128, 48, 3], F32)
            gm = sb("a_g", [128, KC], F32)
            Am = sb("a_A", [128, KC, 3], F32)
            binF = sb("a_binF", [128, len(F_CHUNKS)], F32)
            bT = sb("a_bT", [128, 656], F32)
            ones = sb("a_ones", [128, 128], BF16)
            xg = [sb("a_xg%d" % i, [128, KC, 512], F32) for i in range(2)]
            sq = sb("a_sq", [128, KC, 512], BF16)
            rstd = sb("a_rstd", [128, 512], F32)
            t1 = sb("a_t1", [128, KC, 512], F32)
            hT = [sb("a_hT%d" % i, [128, KC, 512], BF16) for i in range(2)]
            stg = [sb("a_stg%d" % i, [128, 6, 512], BF16) for i in range(3)]
            stT = [sb("a_stT%d" % i, [128, 640], BF16) for i in range(2)]
            stG = [sb("a_stG%d" % i, [128, 16], F32) for i in range(2)]

            WB = 1024
            nwb = (N_IN + WB - 1) // WB
            for q in range(nwb):
                c0, c1 = q * WB, min(N_IN, (q + 1) * WB)
                P.dma(W[:, :, c0:c1], self.w_in[layer, :, :, c0:c1], [], [("a_w", q)], eng="pool")

            def wk_(c0, c1):
                return [("a_w", q) for q in range(c0 // WB, (c1 - 1) // WB + 1)]
            P.dma(modt[:], self.s_mod[layer], [("s_mod", layer)], ["a_mod"])
            P.dma(gm[:], self.g_mixT[layer], [], ["a_g"])
            P.dma(binF[:], self.b_inF[layer], [], ["a_binF"])
            P.dma(bT[:, 0:384], self.b_in[layer:layer + 1, OFF_RV:OFF_RV + 384].to_broadcast([128, 384]), [], ["a_bT"])
            P.dma(bT[:, 384:640], self.b_in[layer:layer + 1, OFF_MV:OFF_MV + 256].to_broadcast([128, 256]), [], ["a_bT"])
            P.dma(bT[:, 640:656], self.b_in[layer:layer + 1, OFF_MG:OFF_MG + 16].to_broadcast([128, 16]), [], ["a_bT"])
            P.op("pool", lambda e: e.memset(ones[:], 1.0 / D), [], ["a_ones"])
            P.op("dve", lambda e: e.tensor_scalar(out=Am[:], in0=modt[:, 8:16, :], scalar1=1.0, scalar2=None,
                                                   op0=ALU.add), ["a_mod"], ["a_A"])
            for s in range(3):
                P.op("dve", lambda e, s=s: e.tensor_tensor(out=Am[:, :, s], in0=Am[:, :, s], in1=gm[:], op=ALU.mult),
                     ["a_A", "a_g"], ["a_A"])

            groups = []
            for b in range(NB):
                groups.append((b, 2, 0, CTX))
                for g in range(4):
                    groups.append((b, b, CTX + 512 * g, 512))
            nstg = 0

            def load_xA(gi):
                b, s, t0, n = groups[gi]
                P.dma(xg[gi % 2][:, :, 0:n], xsrc[b, :, :, t0:t0 + n].rearrange("c p t -> p c t"), [("x", layer)], ["a_xg%d" % (gi % 2)])

            for gi, (b, s, t0, n) in enumerate(groups):
                is_ctx = t0 < CTX
                xt = xg[gi % 2]
                xk = "a_xg%d" % (gi % 2)
                h = hT[gi % 2]
                hk = "a_hT%d" % (gi % 2)
                hks = [(hk, k) for k in range(KC)]
                if gi == 0:
                    load_xA(0)
                if gi + 1 < len(groups):
                    load_xA(gi + 1)
                P.op("act", lambda e, xt=xt, n=n: e.activation(out=sq[:, :, 0:n], in_=xt[:, :, 0:n], func=AF.Square),
                     [xk], ["a_sq"])
                ps = self.bank[0]

                def fss(e, ps=ps, n=n):
                    return [e.matmul(ps[:, 0:n], ones[:], sq[:, k, 0:n], start=(k == 0), stop=(k == KC - 1))
                            for k in range(KC)]
                P.op("pe", fss, ["a_sq", "a_ones"], [self.pk(0)])
                P.op("dve", lambda e, ps=ps, n=n: e.tensor_scalar(out=rstd[:, 0:n], in0=ps[:, 0:n], scalar1=EPS,
                                                                    scalar2=None, op0=ALU.add),
                     [], [self.pk(0), "a_rstd"])
                P.op("act", lambda e, n=n: e.activation(out=rstd[:, 0:n], in_=rstd[:, 0:n], func=AF.Sqrt),
                     [], ["a_rstd"])
                P.op("dve", lambda e, n=n: e.reciprocal(out=rstd[:, 0:n], in_=rstd[:, 0:n]), [], ["a_rstd"])
                for k in range(KC):
                    P.op("dve", lambda e, k=k, xt=xt, n=n: e.tensor_tensor(out=t1[:, k, 0:n], in0=xt[:, k, 0:n],
                                                                            in1=rstd[:, 0:n], op=ALU.mult),
                         [xk, "a_rstd"], [("a_t1", k)])
                    P.op("act", lambda e, k=k, h=h, n=n, s=s: e.activation(
                        out=h[:, k, 0:n], in_=t1[:, k, 0:n], func=AF.Identity,
                        scale=Am[:, k, s:s + 1], bias=modt[:, k, s:s + 1]),
                         [("a_t1", k), "a_A", "a_mod"], [(hk, k)])
                cur_seg = None
                for ci, (sname, j, col0, actf) in enumerate(F_CHUNKS):
                    if last and is_ctx and sname in ("uhy", "gate", "rg", "mo"):
                        continue
                    bi = 1 + (ci % 4)
                    ps = self.bank[bi]

                    def fmm(e, ps=ps, col0=col0, h=h, n=n):
                        return [e.matmul(ps[:, 0:n], W[:, k, col0:col0 + 128], h[:, k, 0:n],
                                         start=(k == 0), stop=(k == KC - 1)) for k in range(KC)]
                    P.op("pe", fmm, hks + wk_(col0, col0 + 128), [self.pk(bi)])
                    slot = j % 6
                    if slot == 0:
                        nstg += 1
                    sg = stg[nstg % 3]
                    sgk = "a_stg%d" % (nstg % 3)
                    if actf == "id":
                        P.op("dve", lambda e, sg=sg, slot=slot, ps=ps, ci=ci, n=n: e.tensor_scalar(
                            out=sg[:, slot, 0:n], in0=ps[:, 0:n], scalar1=binF[:, ci:ci + 1], scalar2=None,
                            op0=ALU.add), ["a_binF"], [self.pk(bi), (sgk, slot)])
                    else:
                        fn_ = AF.Silu if actf == "silu" else AF.Sigmoid
                        P.op("act", lambda e, sg=sg, slot=slot, ps=ps, ci=ci, n=n, fn_=fn_: e.activation(
                            out=sg[:, slot, 0:n], in_=ps[:, 0:n], func=fn_, bias=binF[:, ci:ci + 1], scale=1.0),
                             ["a_binF"], [self.pk(bi), (sgk, slot)])
                    nseg = SEG_NCH[sname]
                    if slot == 5 or j == nseg - 1:
                        j0 = j - slot
                        P.dma(self.seg[sname][b, j0:j + 1, :, t0:t0 + n].rearrange("c p t -> p c t"),
                              sg[:, 0:slot + 1, 0:n], [(sgk, q) for q in range(slot + 1)], [("seg", sname)])
                for tt in range(n // 128):
                    ti = (t0 + tt * 128) // 128
                    pv, pg = self.bank[5 + (tt % 2)], self.bank[7]
                    pvk, pgk = self.pk(5 + (tt % 2)), self.pk(7)
                    sT_ = stT[tt % 2]
                    sTk = "a_stT%d" % (tt % 2)
                    sG_ = stG[tt % 2]
                    sGk = "a_stG%d" % (tt % 2)

                    def fv(e, pv=pv, h=h, tt=tt):
                        return [e.matmul(pv[:, 0:384], h[:, k, tt * 128:(tt + 1) * 128], W[:, k, OFF_RV:OFF_RV + 384],
                                         start=(k == 0), stop=(k == KC - 1)) for k in range(KC)]
                    P.op("pe", fv, hks + wk_(OFF_RV, OFF_RV + 384), [pvk])
                    P.op("dve", lambda e, pv=pv, sT_=sT_: e.tensor_tensor(out=sT_[:, 0:384], in0=pv[:, 0:384],
                                                                          in1=bT[:, 0:384], op=ALU.add),
                         ["a_bT"], [pvk, (sTk, 0)])

                    def fm(e, pg=pg, h=h, tt=tt):
                        r = [e.matmul(pg[:, 0:256], h[:, k, tt * 128:(tt + 1) * 128], W[:, k, OFF_MV:OFF_MV + 256],
                                      start=(k == 0), stop=(k == KC - 1)) for k in range(KC)]
                        r += [e.matmul(pg[:, 256:272], h[:, k, tt * 128:(tt + 1) * 128], W[:, k, OFF_MG:OFF_MG + 16],
                                       start=(k == 0), stop=(k == KC - 1)) for k in range(KC)]
                        return r
                    P.op("pe", fm, hks + wk_(OFF_MV, OFF_MG + 16), [pgk])
                    P.op("dve", lambda e, pg=pg, sT_=sT_: e.tensor_tensor(out=sT_[:, 384:640], in0=pg[:, 0:256],
                                                                          in1=bT[:, 384:640], op=ALU.add),
                         ["a_bT"], [pgk, (sTk, 1)])
                    P.op("dve", lambda e, pg=pg, sG_=sG_: e.tensor_tensor(out=sG_[:, :], in0=pg[:, 256:272],
                                                                          in1=bT[:, 640:656], op=ALU.add),
                         ["a_bT"], [pgk, sGk])
                    P.dma(self.s_rv[b, ti], sT_[:, 0:384], [(sTk, 0)], [("seg", "rv")])
                    P.dma(self.s_mv[b, ti], sT_[:, 384:640], [(sTk, 1)], [("seg", "mv")])
                    P.dma(self.s_mg[b, ti], sG_[:, :], [sGk], [("seg", "mg")])
            P.barrier()


    def bank_bf(self, i):
        return self.bank[i][:].bitcast(BF16)

    def phase_B(self, layer):
        last = layer == DEPTH - 1
        for (L, t0, tag) in ((LAT, CTX, "l"), (CTX, 0, "c")):
            if tag == "c" and last:
                continue
            self.hy_filters(layer, L, tag)
            self.hy_shortconv(layer, L, t0, tag)
            self.hy_pass(layer, L, t0, tag, 0)
            self.hy_pass(layer, L, t0, tag, 1)

    def hy_filters(self, layer, L, tag):
        nc, P = self.nc, self.P
        kch = L // 128
        cst = self.hyc[tag]
        with contextlib.ExitStack() as st:
            sb = self.mk_sb(st)
            featsT = sb("f_feats", [33, L], F32)
            f1 = sb("f_f1", [33, 64], F32)
            f2 = sb("f_f2", [64, 64], F32)
            f3 = sb("f_f3", [64, 1536], F32)
            b1 = sb("f_b1", [64, 3], F32)
            b2 = sb("f_b2", [64, 3], F32)
            absd = sb("f_absd", [128, 1536], F32)
            negd = sb("f_negd", [128, 1536], F32)
            tcol = sb("f_tcol", [128, kch], F32)
            hid1 = sb("f_hid1", [64, 512], F32)
            hid2 = sb("f_hid2", [64, 512], F32)
            sa = sb("f_sa", [64, 512], F32)
            sq_ = sb("f_sq", [64, 512], F32)
            win = sb("f_win", [128, 1536], F32)
            hf = sb("f_hf", [128, kch, 1536], BF16)
            fcm = [sb("f_fc%d" % i, [128, kch, 128], BF16) for i in range(2)]
            fsm = [sb("f_fs%d" % i, [128, kch, 128], BF16) for i in range(2)]
            hsd = sb("f_hsd", [128, kch, 2, 2, 384], BF16)
            outt = [sb("f_out%d" % i, [128, 2, 2, 384], BF16) for i in range(2)]
            P.dma(featsT[:], cst["featsT"][:, :], [], ["f_feats"])
            P.dma(f1[:], self.hy_f1[layer], [], ["f_f1"])
            P.dma(f2[:], self.hy_f2[layer], [], ["f_f2"])
            P.dma(f3[:], self.hy_f3[layer], [], ["f_f3"])
            P.dma(b1[:, 0:1], self.hy_fb1[layer], [], ["f_b1"])
            P.dma(b2[:, 0:1], self.hy_fb2[layer], [], ["f_b2"])
            P.dma(absd[:], self.hy_decay[layer:layer + 1, :].to_broadcast([128, 1536]), [], ["f_absd"])
            P.dma(tcol[:], cst["tcol"][:, :], [], ["f_tcol"])
            for bb, bk in ((b1, "f_b1"), (b2, "f_b2")):
                P.op("dve", lambda e, bb=bb: e.tensor_scalar(out=bb[:, 1:2], in0=bb[:, 0:1], scalar1=0.5, scalar2=None,
                                                            op0=ALU.mult), [bk], [bk])
                P.op("dve", lambda e, bb=bb: e.tensor_scalar(out=bb[:, 2:3], in0=bb[:, 0:1], scalar1=0.25, scalar2=None,
                                                            op0=ALU.mult), [bk], [bk])
            P.op("dve", lambda e: e.tensor_scalar(out=negd[:], in0=absd[:], scalar1=-1.0, scalar2=None, op0=ALU.mult),
                 ["f_absd"], ["f_negd"])
            P.op("dve", lambda e: e.tensor_tensor(out=absd[:], in0=absd[:], in1=negd[:], op=ALU.max),
                 ["f_negd"], ["f_absd"])

            def sin_layer(ps, bb, bk, out, outk, n):
                P.op("act", lambda e: e.activation(out=sa[:, 0:n], in_=ps[0:64, 0:n], func=AF.Sin, bias=bb[:, 1:2], scale=0.5),
                     [bk], [self.pk(0), "f_sa"])
                P.op("act", lambda e: e.activation(out=sq_[:, 0:n], in_=ps[0:64, 0:n], func=AF.Sin, bias=bb[:, 2:3], scale=0.25),
                     [bk], [self.pk(0), "f_sq"])
                P.op("dve", lambda e: e.tensor_tensor(out=sq_[:, 0:n], in0=sq_[:, 0:n], in1=sq_[:, 0:n], op=ALU.mult),
                     [], ["f_sq"])
                P.op("dve", lambda e: e.tensor_scalar(out=sq_[:, 0:n], in0=sq_[:, 0:n], scalar1=-4.0, scalar2=2.0,
                                                       op0=ALU.mult, op1=ALU.add), [], ["f_sq"])
                P.op("dve", lambda e: e.tensor_tensor(out=out[:, 0:n], in0=sa[:, 0:n], in1=sq_[:, 0:n], op=ALU.mult),
                     ["f_sa", "f_sq"], [outk])

            nblk = max(L // 512, 1)
            bn = min(L, 512)
            for blk in range(nblk):
                ps = self.bank[0]
                P.op("pe", lambda e, blk=blk, ps=ps: e.matmul(ps[0:64, 0:bn], f1[:, :], featsT[:, blk * bn:(blk + 1) * bn],
                                                              start=True, stop=True), ["f_f1", "f_feats"], [self.pk(0)])
                sin_layer(ps, b1, "f_b1", hid1, "f_hid1", bn)
                P.op("pe", lambda e, ps=ps: e.matmul(ps[0:64, 0:bn], f2[:, :], hid1[:, 0:bn], start=True, stop=True),
                     ["f_f2", "f_hid1"], [self.pk(0)])
                sin_layer(ps, b2, "f_b2", hid2, "f_hid2", bn)
                for tt in range(bn // 128):
                    ch = blk * (bn // 128) + tt
                    P.op("act", lambda e, ch=ch: e.activation(out=win[:], in_=absd[:], func=AF.Exp, scale=tcol[:, ch:ch + 1]),
                         ["f_absd", "f_tcol"], ["f_win"])
                    for q in range(3):
                        pq = self.bank[1 + q]
                        P.op("pe", lambda e, pq=pq, tt=tt, q=q: e.matmul(pq[:, :], hid2[:, tt * 128:(tt + 1) * 128],
                                                                          f3[:, q * 512:(q + 1) * 512], start=True, stop=True),
                             ["f_hid2", "f_f3"], [self.pk(1 + q)])
                        P.op("dve", lambda e, pq=pq, ch=ch, q=q: e.tensor_tensor(
                            out=hf[:, ch, q * 512:(q + 1) * 512], in0=pq[:, :], in1=win[:, q * 512:(q + 1) * 512], op=ALU.mult),
                             ["f_win"], [self.pk(1 + q), ("f_hf", ch)])
                    if ch == 0:
                        for o in range(2):
                            P.op("pool", lambda e, o=o: e.memset(hf[0:1, 0, o * 768 + 384:o * 768 + 768], 0.0),
                                 [], [("f_hf", 0)])
            hfk = [("f_hf", c) for c in range(kch)]
            for ch in range(kch):
                hv = hf[:, ch, :].rearrange("p (o d c) -> p o d c", o=2, d=2)
                P.op("dve", lambda e, ch=ch, hv=hv: e.tensor_tensor(out=hsd[:, ch, 0, :, :], in0=hv[:, :, 0, :], in1=hv[:, :, 1, :], op=ALU.add),
                     [("f_hf", ch)], [("f_hsd", ch, 0)])
                P.op("pool", lambda e, ch=ch, hv=hv: e.tensor_tensor(out=hsd[:, ch, 1, :, :], in0=hv[:, :, 0, :], in1=hv[:, :, 1, :], op=ALU.subtract),
                     [("f_hf", ch)], [("f_hsd", ch, 1)])
            hsk = [[("f_hsd", c, pi) for c in range(kch)] for pi in range(2)]
            for kc in range(kch):
                fc_, fs_ = fcm[kc % 2], fsm[kc % 2]
                fck, fsk = "f_fc%d" % (kc % 2), "f_fs%d" % (kc % 2)
                P.dma(fc_[:], cst["fC"][kc], [], [fck])
                P.dma(fs_[:], cst["fS"][kc], [], [fsk])
                ot = outt[kc % 2]
                otk = "f_out%d" % (kc % 2)
                for pi, (mt, mk) in enumerate(((fc_, fck), (fs_, fsk))):
                    for o in range(2):
                        bi = (kc % 2) * 4 + pi * 2 + o
                        pb = self.bank[bi]

                        def fsp(e, pb=pb, mt=mt, o=o, pi=pi):
                            return [e.matmul(pb[:, 0:384], mt[:, nch, :], hsd[:, nch, pi, o, :],
                                             start=(nch == 0), stop=(nch == kch - 1)) for nch in range(kch)]
                        P.op("pe", fsp, hsk[pi] + [mk], [self.pk(bi)])
                        if (pi + o) % 2 == 0:
                            P.op("act", lambda e, pb=pb, ot=ot, o=o, pi=pi: e.activation(out=ot[:, o, pi, :], in_=pb[:, 0:384], func=AF.Copy),
                                 [], [self.pk(bi), (otk, o, pi)])
                        else:
                            P.op("dve", lambda e, pb=pb, ot=ot, o=o, pi=pi: e.tensor_copy(out=ot[:, o, pi, :], in_=pb[:, 0:384]),
                                 [], [self.pk(bi), (otk, o, pi)])
                for o in range(2):
                    P.dma(self.s_fs[tag][o, :, kc].rearrange("r p c -> p r c"), ot[:, o, :, :],
                          [(otk, o, 0), (otk, o, 1)], [("s_fs", tag)])
            P.barrier()

    def hy_shortconv(self, layer, L, t0, tag):
        nc, P = self.nc, self.P
        kch = L // 128
        ztok = self.s_ztok[tag]
        with contextlib.ExitStack() as st:
            sb = self.mk_sb(st)
            cw = sb("c_w", [128, 9, 3], F32)
            ident = sb("c_id", [128, 128], BF16)
            idf = sb("c_idf", [128, 128], F32)
            u = [sb("c_u%d" % i, [128, L], BF16) for i in range(2)]
            acc = [sb("c_acc%d" % i, [128, L], F32) for i in range(2)]
            ob = [sb("c_ob%d" % i, [128, L], BF16) for i in range(2)]
            tk = [sb("c_tk%d" % i, [128, 4, 128], BF16) for i in range(2)]
            P.dma(cw[:], self.hy_convT[layer], [], ["c_w"])
            P.dma(idf[:], self.ident_d[:, :], [], ["c_idf"])
            P.op("dve", lambda e: e.tensor_copy(out=ident[:], in_=idf[:]), ["c_idf"], ["c_id"])
            it = 0

            def load_u(it_):
                b_, j_ = it_ // 9, it_ % 9
                P.dma(u[it_ % 2][:], self.seg["uhy"][b_, j_, :, t0:t0 + L], [], ["c_u%d" % (it_ % 2)])

            load_u(0)
            for b in range(NB):
                for j in range(9):
                    ut, uk = u[it % 2], "c_u%d" % (it % 2)
                    at, ak = acc[it % 2], "c_acc%d" % (it % 2)
                    ot, ok_ = ob[it % 2], "c_ob%d" % (it % 2)
                    it += 1
                    if it < NB * 9:
                        load_u(it)
                    eng = "dve"
                    P.op(eng, lambda e, ut=ut, at=at, j=j: e.tensor_scalar(out=at[:], in0=ut[:], scalar1=cw[:, j, 1:2],
                                                                          scalar2=None, op0=ALU.mult), [uk, "c_w"], [ak])
                    P.op(eng, lambda e, ut=ut, at=at, j=j: e.scalar_tensor_tensor(
                        out=at[:, 1:L], in0=ut[:, 0:L - 1], scalar=cw[:, j, 0:1], in1=at[:, 1:L], op0=ALU.mult, op1=ALU.add),
                         [uk, "c_w"], [ak])
                    P.op(eng, lambda e, ut=ut, at=at, j=j: e.scalar_tensor_tensor(
                        out=at[:, 0:L - 1], in0=ut[:, 1:L], scalar=cw[:, j, 2:3], in1=at[:, 0:L - 1], op0=ALU.mult, op1=ALU.add),
                         [uk, "c_w"], [ak])
                    P.op("act", lambda e, at=at, ot=ot: e.activation(out=ot[:], in_=at[:], func=AF.Copy), [ak], [ok_])
                    P.dma(self.s_hyc[b, j, :, t0:t0 + L], ot[:], [ok_], [("s_hyc", b, j)])
                    if j < 3:
                        for g4 in range(kch // 4 if kch >= 4 else 1):
                            nt = min(4, kch)
                            bi = g4 % 2
                            pbf = self.bank_bf(bi)
                            tkt, tkk = tk[g4 % 2], "c_tk%d" % (g4 % 2)

                            def ftr(e, pbf=pbf, ot=ot, g4=g4, nt=nt):
                                return [e.transpose(pbf[:, q * 128:(q + 1) * 128], ot[:, (g4 * 4 + q) * 128:(g4 * 4 + q + 1) * 128],
                                                    ident[:]) for q in range(nt)]
                            P.op("pe", ftr, [ok_, "c_id"], [self.pk(bi)])
                            P.op("dve", lambda e, pbf=pbf, tkt=tkt, nt=nt: e.tensor_copy(
                                out=tkt[:, 0:nt, :], in_=pbf[:, 0:nt * 128].rearrange("p (q c) -> p q c", c=128)),
                                 [], [self.pk(bi), tkk])
                            P.dma(ztok[0, g4 * 4:g4 * 4 + nt, :, b * 384 + j * 128:b * 384 + (j + 1) * 128].rearrange("q p c -> p q c"),
                                  tkt[:, 0:nt, :], [tkk], [("s_ztok", tag, 0)])
            P.barrier()

    def hy_pass(self, layer, L, t0, tag, o):
        nc, P = self.nc, self.P
        kch = L // 128
        cst = self.hyc[tag]
        ztok = self.s_ztok[tag]
        zin_d = self.s_hyc if o == 0 else self.s_z1
        zout_d = self.s_z1 if o == 0 else self.seg_yhy
        GS = min(512, L)
        ngn = L // GS
        with contextlib.ExitStack() as st:
            sb = self.mk_sb(st)
            zt = sb("p_zt", [128, kch, 768], BF16)
            Fs2 = [sb("p_F%d" % i, [128, 2, 384], BF16) for i in range(2)]
            Y = sb("p_Y", [128, kch, 2, 768], BF16)
            fcm = [sb("p_fc%d" % i, [128, kch, 128], BF16) for i in range(2)]
            fsm = [sb("p_fs%d" % i, [128, kch, 128], BF16) for i in range(2)]
            icm = [sb("p_ic%d" % i, [128, kch, GS], BF16) for i in range(2)]
            ism = [sb("p_is%d" % i, [128, kch, GS], BF16) for i in range(2)]
            tm = [sb("p_tm%d" % i, [128, 384], F32) for i in range(4)]
            skp = sb("p_skip", [128, 2, 3], F32)
            ident = sb("p_id", [128, 128], BF16)
            idf = sb("p_idf", [128, 128], F32)
            zi = [sb("p_zi%d" % i, [128, 6, GS], BF16) for i in range(2)]
            gt = [sb("p_gt%d" % i, [128, 6, GS], BF16) for i in range(2)]
            tf = [sb("p_tf%d" % i, [128, GS], F32) for i in range(2)]
            zo = [sb("p_zo%d" % i, [128, 6, GS], BF16) for i in range(2)]
            tk = [sb("p_tk%d" % i, [128, GS // 128, 128], BF16) for i in range(2)]
            P.dma(zt[:], ztok[o].rearrange("q p c -> p q c"), [], ["p_zt"])
            P.dma(skp[:], self.hy_skipT[layer], [], ["p_skip"])
            P.dma(idf[:], self.ident_d[:, :], [], ["p_idf"])
            P.op("dve", lambda e: e.tensor_copy(out=ident[:], in_=idf[:]), ["p_idf"], ["p_id"])
            for kc in range(kch):
                fc_, fs_ = fcm[kc % 2], fsm[kc % 2]
                fck, fsk = "p_fc%d" % (kc % 2), "p_fs%d" % (kc % 2)
                P.dma(fc_[:], cst["fC"][kc], [], [fck])
                P.dma(fs_[:], cst["fS"][kc], [], [fsk])
                Fst, Fsk = Fs2[kc % 2], "p_F%d" % (kc % 2)
                P.dma(Fst[:], self.s_fs[tag][o, :, kc].rearrange("r p c -> p r c"), [], [Fsk])
                for b in range(NB):
                    br, bi_ = (kc * NB + b) % 4 * 2, (kc * NB + b) % 4 * 2 + 1
                    pr, pi_ = self.bank[br], self.bank[bi_]

                    def ffw(e, pp, mt, b=b):
                        return [e.matmul(pp[:, 0:384], mt[:, nch, :], zt[:, nch, b * 384:(b + 1) * 384],
                                         start=(nch == 0), stop=(nch == kch - 1)) for nch in range(kch)]
                    P.op("pe", lambda e, pr=pr, fc_=fc_, b=b: ffw(e, pr, fc_, b), ["p_zt", fck], [self.pk(br)])
                    P.op("pe", lambda e, pi_=pi_, fs_=fs_, b=b: ffw(e, pi_, fs_, b), ["p_zt", fsk], [self.pk(bi_)])
                    t1, t2, t3, t4 = tm
                    Fr, Fi = Fst[:, 0, :], Fst[:, 1, :]
                    P.op("dve", lambda e, pr=pr, Fr=Fr: e.tensor_tensor(out=t1[:], in0=pr[:, 0:384], in1=Fr, op=ALU.mult),
                         [Fsk], [self.pk(br), "p_tm0"])
                    P.op("dve", lambda e, pi_=pi_, Fi=Fi: e.tensor_tensor(out=t2[:], in0=pi_[:, 0:384], in1=Fi, op=ALU.mult),
                         [Fsk], [self.pk(bi_), "p_tm1"])
                    P.op("dve", lambda e, pr=pr, Fi=Fi: e.tensor_tensor(out=t3[:], in0=pr[:, 0:384], in1=Fi, op=ALU.mult),
                         [Fsk], [self.pk(br), "p_tm2"])
                    P.op("dve", lambda e, pi_=pi_, Fr=Fr: e.tensor_tensor(out=t4[:], in0=pi_[:, 0:384], in1=Fr, op=ALU.mult),
                         [Fsk], [self.pk(bi_), "p_tm3"])
                    P.op("pool", lambda e, kc=kc, b=b: e.tensor_tensor(out=Y[:, kc, 0, b * 384:(b + 1) * 384], in0=t1[:], in1=t2[:],
                                                                      op=ALU.subtract), ["p_tm0", "p_tm1"], [("p_Y", kc)])
                    P.op("pool", lambda e, kc=kc, b=b: e.tensor_tensor(out=Y[:, kc, 1, b * 384:(b + 1) * 384], in0=t3[:], in1=t4[:],
                                                                      op=ALU.add), ["p_tm2", "p_tm3"], [("p_Y", kc)])
            yk = [("p_Y", kc) for kc in range(kch)]
            def load_inv(ng_):
                P.dma(icm[ng_ % 2][:], cst["iC"][ng_], [], ["p_ic%d" % (ng_ % 2)])
                P.dma(ism[ng_ % 2][:], cst["iS"][ng_], [], ["p_is%d" % (ng_ % 2)])
                n0_ = t0 + ng_ * GS
                for b_ in range(NB):
                    P.dma(zi[ng_ % 2][:, b_ * 3:(b_ + 1) * 3, :], zin_d[b_, 0:3, :, n0_:n0_ + GS].rearrange("c p t -> p c t"),
                          [], [("p_zi%d" % (ng_ % 2), b_)], eng="pool")
                    P.dma(gt[ng_ % 2][:, b_ * 3:(b_ + 1) * 3, :],
                          self.s_hyc[b_, 3 + 3 * o:6 + 3 * o, :, n0_:n0_ + GS].rearrange("c p t -> p c t"),
                          [], [("p_gt%d" % (ng_ % 2), b_)], eng="pool")

            load_inv(0)
            for ng in range(ngn):
                ic_, is_ = icm[ng % 2], ism[ng % 2]
                ick, isk = "p_ic%d" % (ng % 2), "p_is%d" % (ng % 2)
                zit, zik = zi[ng % 2], "p_zi%d" % (ng % 2)
                gtt, gtk = gt[ng % 2], "p_gt%d" % (ng % 2)
                zot, zok = zo[ng % 2], "p_zo%d" % (ng % 2)
                n0 = t0 + ng * GS
                if ng + 1 < ngn:
                    load_inv(ng + 1)
                for b in range(NB):
                    for cj in range(3):
                        q = b * 3 + cj
                        bi = q % 4
                        pb = self.bank[bi]

                        def finv(e, pb=pb, q=q, ic_=ic_, is_=is_):
                            r = []
                            for kc in range(kch):
                                r.append(e.matmul(pb[:, 0:GS], Y[:, kc, 0, q * 128:(q + 1) * 128], ic_[:, kc, :],
                                                  start=(kc == 0), stop=False))
                                r.append(e.matmul(pb[:, 0:GS], Y[:, kc, 1, q * 128:(q + 1) * 128], is_[:, kc, :],
                                                  start=False, stop=(kc == kch - 1)))
                            return r
                        P.op("pe", finv, yk + [ick, isk], [self.pk(bi)])
                        tft, tfk = tf[q % 2], "p_tf%d" % (q % 2)
                        P.op("dve", lambda e, pb=pb, tft=tft, zit=zit, q=q, cj=cj: e.scalar_tensor_tensor(
                            out=tft[:], in0=zit[:, q, :], scalar=skp[:, o, cj:cj + 1], in1=pb[:, 0:GS],
                            op0=ALU.mult, op1=ALU.add), [(zik, b), "p_skip"], [self.pk(bi), tfk])
                        P.op("pool", lambda e, tft=tft, zot=zot, gtt=gtt, q=q: e.tensor_tensor(
                            out=zot[:, q, :], in0=tft[:], in1=gtt[:, q, :], op=ALU.mult), [tfk, (gtk, b)], [(zok, q)])
                        if o == 0:
                            bt = 4 + (q % 2)
                            pbf = self.bank_bf(bt)
                            tkt, tkk = tk[q % 2], "p_tk%d" % (q % 2)

                            def ftr(e, pbf=pbf, zot=zot, q=q):
                                return [e.transpose(pbf[:, h * 128:(h + 1) * 128], zot[:, q, h * 128:(h + 1) * 128], ident[:])
                                        for h in range(GS // 128)]
                            P.op("pe", ftr, [(zok, q), "p_id"], [self.pk(bt)])
                            P.op("act", lambda e, pbf=pbf, tkt=tkt: e.activation(
                                out=tkt[:], in_=pbf[:, 0:GS].rearrange("p (h c) -> p h c", c=128), func=AF.Copy),
                                 [], [self.pk(bt), tkk])
                            P.dma(ztok[1, ng * (GS // 128):(ng + 1) * (GS // 128), :, q * 128:(q + 1) * 128].rearrange("h p c -> p h c"),
                                  tkt[:], [tkk], [("s_ztok", tag, 1)])
                    P.dma(zout_d[b, 0:3, :, n0:n0 + GS].rearrange("c p t -> p c t"), zot[:, b * 3:(b + 1) * 3, :],
                          [(zok, b * 3 + cj) for cj in range(3)], [("zout", b)])
            P.barrier()


    def phase_C1(self, layer):
        nc, P = self.nc, self.P
        with contextlib.ExitStack() as st:
            sb = self.mk_sb(st)
            cosT = sb("r_cos", [128, LAT], F32)
            sinT = sb("r_sin", [128, LAT], F32)
            RT = sb("r_RT", [128, 128], BF16)
            ident = sb("r_id", [128, 128], BF16)
            idf = sb("r_idf", [128, 128], F32)
            mcw = sb("r_mcw", [128, 4, 3], F32)
            raw = [sb("r_raw%d" % i, [128, TAU], BF16) for i in range(2)]
            q8 = [sb("r_q8%d" % i, [128, TAU], BF16) for i in range(2)]
            tmp = [sb("r_tmp%d" % i, [128, LAT], F32) for i in range(2)]
            acc = sb("r_acc", [128, TAU], F32)
            outb = [sb("r_out%d" % i, [128, TAU], BF16) for i in range(2)]
            tk = [sb("r_tk%d" % i, [128, 6, 128], BF16) for i in range(2)]
            P.dma(cosT[:], self.rope_cos[:, :], [], ["r_cos"])
            P.dma(sinT[:], self.rope_sin[:, :], [], ["r_sin"])
            P.dma(RT[:], self.rope_RT[:, :], [], ["r_RT"])
            P.dma(mcw[:], self.ml_convT[layer], [], ["r_mcw"])
            P.dma(idf[:], self.ident_d[:, :], [], ["r_idf"])
            P.op("dve", lambda e: e.tensor_copy(out=ident[:], in_=idf[:]), ["r_idf"], ["r_id"])
            it = 0
            ntk = 0
            for b in range(NB):
                items = [("rq", j, "ret", False, None) for j in range(3)] + [("rk", j, "ret", True, j * 128) for j in range(3)] + \
                        [("mq", j, "ml", False, None) for j in range(2)] + [("mk", j, "ml", True, 384 + j * 128) for j in range(2)]
                for ii, (sname, j, kind, is_k, kcol) in enumerate(items):
                    rw, rwk = raw[it % 2], "r_raw%d" % (it % 2)
                    qq, qqk = q8[it % 2], "r_q8%d" % (it % 2)
                    tp, tpk = tmp[it % 2], "r_tmp%d" % (it % 2)
                    ob, obk = outb[it % 2], "r_out%d" % (it % 2)
                    if it == 0:
                        P.dma(rw[:], self.seg[sname][b, j], [("seg", sname, b, j)], [rwk])
                    it += 1
                    nxt = (b, ii + 1) if ii + 1 < len(items) else ((b + 1, 0) if b + 1 < NB else None)
                    if nxt is not None:
                        sn2, j2 = items[nxt[1]][0], items[nxt[1]][1]
                        P.dma(raw[it % 2][:], self.seg[sn2][nxt[0], j2], [("seg", sn2, nxt[0], j2)], ["r_raw%d" % (it % 2)])
                    if kind == "ret":
                        sc = 1.0 if is_k else 0.125
                        P.op("act", lambda e, rw=rw, ob=ob, sc=sc: e.activation(out=ob[:, 0:CTX], in_=rw[:, 0:CTX], func=AF.Copy, scale=sc),
                             [rwk], [(obk, 0)])
                        P.op("act", lambda e, rw=rw, qq=qq, sc=sc: e.activation(out=qq[:, CTX:TAU], in_=rw[:, CTX:TAU], func=AF.Copy, scale=sc),
                             [rwk], [qqk])
                        for g in range(4):
                            pb = self.bank[g]
                            P.op("pe", lambda e, pb=pb, qq=qq, g=g: e.matmul(pb[:, :], RT[:, :], qq[:, CTX + g * 512:CTX + (g + 1) * 512],
                                                                            start=True, stop=True), [qqk, "r_RT"], [self.pk(g)])
                            P.op("dve", lambda e, pb=pb, tp=tp, g=g: e.tensor_tensor(out=tp[:, g * 512:(g + 1) * 512], in0=pb[:, :],
                                                                                    in1=sinT[:, g * 512:(g + 1) * 512], op=ALU.mult),
                                 ["r_sin"], [self.pk(g), (tpk, g)])
                        P.op("dve", lambda e, qq=qq: e.tensor_tensor(out=acc[:, CTX:TAU], in0=qq[:, CTX:TAU], in1=cosT[:, :], op=ALU.mult),
                             [qqk, "r_cos"], ["r_acc"])
                        P.op("pool", lambda e, tp=tp, ob=ob: e.tensor_tensor(out=ob[:, CTX:TAU], in0=acc[:, CTX:TAU], in1=tp[:, :], op=ALU.add),
                             ["r_acc"] + [(tpk, g) for g in range(4)], [(obk, 1)])
                    else:
                        wi = (2 if is_k else 0) + j
                        for (a0, a1) in ((0, CTX), (CTX, TAU)):
                            n = a1 - a0
                            P.op("dve", lambda e, rw=rw, a0=a0, a1=a1, wi=wi: e.tensor_scalar(
                                out=acc[:, a0:a1], in0=rw[:, a0:a1], scalar1=mcw[:, wi, 1:2], scalar2=None, op0=ALU.mult),
                                 [rwk, "r_mcw"], ["r_acc"])
                            P.op("dve", lambda e, rw=rw, a0=a0, a1=a1, wi=wi: e.scalar_tensor_tensor(
                                out=acc[:, a0 + 1:a1], in0=rw[:, a0:a1 - 1], scalar=mcw[:, wi, 0:1], in1=acc[:, a0 + 1:a1],
                                op0=ALU.mult, op1=ALU.add), [rwk, "r_mcw"], ["r_acc"])
                            P.op("dve", lambda e, rw=rw, a0=a0, a1=a1, wi=wi: e.scalar_tensor_tensor(
                                out=acc[:, a0:a1 - 1], in0=rw[:, a0 + 1:a1], scalar=mcw[:, wi, 2:3], in1=acc[:, a0:a1 - 1],
                                op0=ALU.mult, op1=ALU.add), [rwk, "r_mcw"], ["r_acc"])
                        if is_k:
                            P.op("act", lambda e: e.activation(out=acc[:, :], in_=acc[:, :], func=AF.Silu), [], ["r_acc"])
                            P.op("pool", lambda e, ob=ob: e.tensor_scalar(out=ob[:, :], in0=acc[:, :], scalar1=0.125, scalar2=None,
                                                                         op0=ALU.mult), ["r_acc"], [(obk, 0), (obk, 1)])
                        else:
                            P.op("act", lambda e, ob=ob: e.activation(out=ob[:, :], in_=acc[:, :], func=AF.Silu),
                                 ["r_acc"], [(obk, 0), (obk, 1)])
                    P.dma(self.seg[sname][b, j], ob[:], [(obk, 0), (obk, 1)], [("seg", sname, b, j)])
                    if is_k:
                        for g6 in range(3):
                            bi = 4 + (ntk % 2)
                            pbf = self.bank_bf(bi)
                            tkt, tkk = tk[ntk % 2], "r_tk%d" % (ntk % 2)
                            ntk += 1

                            def ftr(e, pbf=pbf, ob=ob, g6=g6):
                                return [e.transpose(pbf[:, q * 128:(q + 1) * 128], ob[:, (g6 * 6 + q) * 128:(g6 * 6 + q + 1) * 128],
                                                    ident[:]) for q in range(6)]
                            P.op("pe", ftr, [(obk, 0), (obk, 1), "r_id"], [self.pk(bi)])
                            P.op("dve", lambda e, pbf=pbf, tkt=tkt: e.tensor_copy(
                                out=tkt[:], in_=pbf[:, 0:768].rearrange("p (q c) -> p q c", c=128)), [], [self.pk(bi), tkk])
                            P.dma(self.s_ktok[b, g6 * 6:g6 * 6 + 6, :, kcol:kcol + 128].rearrange("q p c -> p q c"), tkt[:],
                                  [tkk], [("s_ktok", b)])
            P.barrier()


    def phase_C2(self, layer):
        nc, P = self.nc, self.P
        last = layer == DEPTH - 1
        NCH = TAU // 128
        with contextlib.ExitStack() as st:
            sb = self.mk_sb(st)
            U = sb("t_U", [128, 128], F32)
            Lw = sb("t_L", [128, 128], F32)
            Df = sb("t_Df", [128, 128], F32)
            Db = sb("t_Db", [128, 128], F32)
            NEGf = sb("t_NEGf", [128, 128], F32)
            NEGb = sb("t_NEGb", [128, 128], F32)
            io = sb("t_io", [128, 256], F32)
            ioc = sb("t_ioc", [128, 2], F32)
            Jm = sb("t_J", [128, 128], BF16)
            onesF = sb("t_onesF", [128, 128], F32)
            ones64 = sb("t_ones64", [128, 64], BF16)
            epsc = sb("t_epsc", [128, 1], F32)
            lgfull = sb("t_lgfull", [128, 2, 6], F32)
            lgcol = sb("t_lgcol", [128, 2, 3], F32)
            ETr = sb("t_ETr", [128, 6, 128], F32)
            et1 = sb("t_et1", [128, 128], F32)
            et2 = sb("t_et2", [128, 128], F32)
            dqr = sb("t_dqr", [128, 2, 3, 128], F32)
            wkr = sb("t_wkr", [128, 2, 6], F32)
            decr = sb("t_decr", [128, 2, 3], F32)
            gbias = sb("t_gbias", [128, 16], F32)
            qT = sb("t_qT", [128, 5, TAU], BF16)
            kT = sb("t_kT", [128, 5, TAU], BF16)
            gT = sb("t_gT", [128, 5, TAU], BF16)
            ktok = sb("t_ktok", [128, NCH, 640], BF16)
            vtok = sb("t_vtok", [128, NCH, 640], BF16)
            mg = sb("t_mg", [128, NCH, 16], F32)
            gi = sb("t_gi", [128, NCH, 8], F32)
            lf = sb("t_lf", [128, NCH, 8], F32)
            gtmp = sb("t_gtmp", [128, NCH, 8], F32)
            gtmp2 = sb("t_gtmp2", [128, NCH, 8], F32)
            bias1 = sb("t_bias1", [128, NCH, 8], F32)
            wkm = sb("t_wkm", [128, NCH, 8], F32)
            decm = sb("t_decm", [128, NCH, 8], F32)
            decp = sb("t_decp", [128, NCH, 2, 2], F32)
            Sf = sb("t_Sf", [128, 5, 64], F32)
            Sbk = sb("t_Sb", [128, 5, 64], F32)
            Nf = sb("t_Nf", [128, 2, 64], F32)
            Nbk = sb("t_Nb", [128, 2, 64], F32)
            Sf_bf = sb("t_Sfbf", [128, 5, 64], BF16)
            Nf_bf = sb("t_Nfbf", [128, 2, 64], BF16)
            Sb_st = sb("t_Sbst", [128, NCH, 5, 64], BF16)
            Nb_st = sb("t_Nbst", [128, NCH, 2, 64], BF16)
            kw = [sb("t_kw%d" % i, [128, 640], BF16) for i in range(2)]
            lfb = [sb("t_lfb%d" % i, [128, 128], F32) for i in range(4)]
            xe = [sb("t_xe%d" % i, [128, 4, 128], F32) for i in range(2)]
            Ef = sb("t_Ef", [128, 4, 128], F32)
            Eb = sb("t_Eb", [128, 4, 128], F32)
            hh_t = sb("t_hh", [128, 512], F32)
            dqm = sb("t_dqm", [128, 2, 2, 128], F32)
            PT = [sb("t_PT%d" % i, [128, 14, 128], BF16) for i in range(2)]
            qd = [sb("t_qd%d" % i, [128, 2, 5, 128], BF16) for i in range(2)]
            den = sb("t_den", [128, 512], F32)
            o_bf = sb("t_obf", [128, 640], BF16)
            xc = sb("t_xc", [128, 640], F32)
            sq = sb("t_sq", [128, 640], BF16)
            rs = sb("t_rs", [128, 640], F32)
            yo = [sb("t_yo%d" % i, [128, 5, 128], BF16) for i in range(2)]

            for (t_, d_, k_) in ((U, "U", "t_U"), (Lw, "L", "t_L"), (Df, "Df", "t_Df"), (Db, "Db", "t_Db"), (NEGf, "NEGf", "t_NEGf"),
                                 (NEGb, "NEGb", "t_NEGb"), (io, "io", "t_io"), (ioc, "ioc", "t_ioc"), (Jm, "J", "t_J")):
                P.dma(t_[:], self.attc[d_][:, :], [], [k_])
            P.op("pool", lambda e: e.memset(onesF[:], 1.0), [], ["t_onesF"])
            P.op("pool", lambda e: e.memset(ones64[:], 1.0), [], ["t_ones64"])
            P.op("pool", lambda e: e.memset(epsc[:], EPS), [], ["t_epsc"])
            rd = self.ret_decay[layer]
            P.dma(lgfull[:], rd.rearrange("d h -> (d h)").unsqueeze(0).to_broadcast([128, 12]).rearrange("p (d h) -> p d h", d=2),
                  [], ["t_lgfull"])
            for hh in range(2):
                for d in range(2):
                    P.dma(lgcol[hh * 64:(hh + 1) * 64, d, :],
                          self.ret_decay_h[layer, d, hh:hh + 1, :].to_broadcast([64, 3]), [], ["t_lgcol"])
            for (t_, k_, n_) in ((lgfull, "t_lgfull", 12), (lgcol, "t_lgcol", 6)):
                fl = t_[:].rearrange("p a b -> p (a b)")
                P.op("act", lambda e, fl=fl: e.activation(out=fl, in_=fl, func=AF.Exp, scale=-1.0), [], [k_])
                P.op("act", lambda e, fl=fl: e.activation(out=fl, in_=fl, func=AF.Ln, bias=1.0), [], [k_])
                P.op("dve", lambda e, fl=fl: e.tensor_scalar(out=fl, in0=fl, scalar1=-1.0, scalar2=None, op0=ALU.mult), [], [k_])
            for h in range(6):
                P.op("act", lambda e, h=h: e.activation(out=et1[:], in_=Df[:], func=AF.Exp, scale=lgfull[:, 0, h:h + 1]),
                     ["t_Df", "t_lgfull"], ["t_et1"])
                P.op("dve", lambda e: e.tensor_tensor(out=et1[:], in0=et1[:], in1=U[:], op=ALU.mult), ["t_U"], ["t_et1"])
                P.op("act", lambda e, h=h: e.activation(out=et2[:], in_=Db[:], func=AF.Exp, scale=lgfull[:, 1, h:h + 1]),
                     ["t_Db", "t_lgfull"], ["t_et2"])
                P.op("dve", lambda e: e.tensor_tensor(out=et2[:], in0=et2[:], in1=Lw[:], op=ALU.mult), ["t_L"], ["t_et2"])
                P.op("pool", lambda e, h=h: e.tensor_tensor(out=ETr[:, h, :], in0=et1[:], in1=et2[:], op=ALU.add),
                     ["t_et1", "t_et2"], ["t_ETr"])
            for d in range(2):
                for j in range(3):
                    P.op("act", lambda e, d=d, j=j: e.activation(out=dqr[:, d, j, :], in_=io[:, d * 128:(d + 1) * 128], func=AF.Exp,
                                                                  scale=lgcol[:, d, j:j + 1]), ["t_io", "t_lgcol"], ["t_dqr"])
                P.op("act", lambda e, d=d: e.activation(out=wkr[:, d, :], in_=lgfull[:, d, :], func=AF.Exp, scale=ioc[:, d:d + 1]),
                     ["t_ioc", "t_lgfull"], ["t_wkr"])
            P.op("act", lambda e: e.activation(out=decr[:].rearrange("p a b -> p (a b)"), in_=lgcol[:].rearrange("p a b -> p (a b)"),
                                               func=AF.Exp, scale=128.0), ["t_lgcol"], ["t_decr"])
            P.dma(gbias[:], self.ml_gate_bias[layer:layer + 1, :].to_broadcast([128, 16]), [], ["t_gbias"])

            import os as _os
            c2stop = _os.environ.get("C2STOP", "")
            if c2stop == "k":
                P.barrier()
                return
            fwd_order = list(range(NCH))
            bwd_order = [1, 0] + list(range(NCH - 1, 1, -1))
            BK = self.bank
            pk = self.pk
            for b in range(NB):
                for j in range(3):
                    P.dma(qT[:, j, :], self.seg["rq"][b, j], [], [("t_qT", j)])
                    P.dma(kT[:, j, :], self.seg["rk"][b, j], [], [("t_kT", j)])
                    P.dma(gT[:, j, :], self.seg["rg"][b, j], [], [("t_gT", j)], eng="pool")
                for j in range(2):
                    P.dma(qT[:, 3 + j, :], self.seg["mq"][b, j], [], [("t_qT", 3 + j)])
                    P.dma(kT[:, 3 + j, :], self.seg["mk"][b, j], [], [("t_kT", 3 + j)])
                    P.dma(gT[:, 3 + j, :], self.seg["mo"][b, j], [], [("t_gT", 3 + j)], eng="pool")
                qk_keys = [("t_qT", j) for j in range(5)] + [("t_kT", j) for j in range(5)]
                P.dma(ktok[:], self.s_ktok[b].rearrange("c p n -> p c n"), [], ["t_ktok"], eng="pool")
                P.dma(vtok[:, :, 0:384], self.s_rv[b].rearrange("c p n -> p c n"), [], [("t_vtok", 0)])
                P.dma(vtok[:, :, 384:640], self.s_mv[b].rearrange("c p n -> p c n"), [], [("t_vtok", 1)])
                vkeys = [("t_vtok", 0), ("t_vtok", 1)]
                P.dma(mg[:], self.s_mg[b].rearrange("c p n -> p c n"), [], ["t_mg"])
                P.op("dve", lambda e: e.tensor_tensor(out=mg[:], in0=mg[:], in1=gbias[:].unsqueeze(1).to_broadcast([128, NCH, 16]),
                                                      op=ALU.add), ["t_gbias"], ["t_mg"])
                mg4 = mg[:].rearrange("p c (d g h) -> p c d g h", d=2, g=2)
                gi4 = gi[:].rearrange("p c (d h) -> p c d h", d=2)
                lf4 = lf[:].rearrange("p c (d h) -> p c d h", d=2)
                for d in range(2):
                    P.op("dve", lambda e, d=d: e.tensor_copy(out=gi4[:, :, d, :], in_=mg4[:, :, d, 0, :]), ["t_mg"], [("t_gi", d)])
                    P.op("dve", lambda e, d=d: e.tensor_copy(out=lf4[:, :, d, :], in_=mg4[:, :, d, 1, :]), ["t_mg"], [("t_lf", d)])
                gik = [("t_gi", 0), ("t_gi", 1)]
                lfk = [("t_lf", 0), ("t_lf", 1)]
                P.op("dve", lambda e: e.tensor_scalar(out=gtmp[:], in0=lf[:], scalar1=-1.0, scalar2=None, op0=ALU.mult), lfk, ["t_gtmp"])
                P.op("dve", lambda e: e.tensor_tensor(out=gtmp[:], in0=gtmp[:], in1=lf[:], op=ALU.max), lfk, ["t_gtmp"])
                P.op("act", lambda e: e.activation(out=gtmp[:], in_=gtmp[:], func=AF.Exp, scale=-1.0), [], ["t_gtmp"])
                P.op("act", lambda e: e.activation(out=gtmp[:], in_=gtmp[:], func=AF.Ln, bias=1.0), [], ["t_gtmp"])
                P.op("dve", lambda e: e.tensor_scalar(out=gtmp2[:], in0=lf[:], scalar1=0.0, scalar2=None, op0=ALU.min), lfk, ["t_gtmp2"])
                P.op("dve", lambda e: e.tensor_tensor(out=lf[:], in0=gtmp2[:], in1=gtmp[:], op=ALU.subtract),
                     ["t_gtmp", "t_gtmp2"], lfk)
                for c in range(NCH):
                    ps = BK[0]
                    P.op("pe", lambda e, c=c, ps=ps: [
                        e.matmul(ps[:, 0:4], U[:, :], lf[:, c, 0:4], start=True, stop=True),
                        e.matmul(ps[:, 4:8], Lw[:, :], lf[:, c, 4:8], start=True, stop=True),
                        e.matmul(ps[:, 8:16], onesF[:, :], lf[:, c, 0:8], start=True, stop=True)],
                         lfk + ["t_U", "t_L", "t_onesF"], [pk(0)])
                    P.op("dve", lambda e, c=c, ps=ps: e.tensor_tensor(out=bias1[:, c, :], in0=gi[:, c, :], in1=ps[:, 0:8],
                                                                     op=ALU.subtract), gik, [pk(0), ("t_bias1", c)])
                    P.op("dve", lambda e, c=c, ps=ps: e.tensor_tensor(out=wkm[:, c, :], in0=bias1[:, c, :], in1=ps[:, 8:16],
                                                                     op=ALU.add), [("t_bias1", c)], [pk(0), ("t_wkm", c)])
                    P.op("act", lambda e, c=c, ps=ps: e.activation(out=decm[:, c, :], in_=ps[:, 8:16], func=AF.Exp),
                         [], [pk(0), ("t_decm", c)])
                wkmk = [("t_wkm", c) for c in range(NCH)]
                decmk = [("t_decm", c) for c in range(NCH)]
                P.op("act", lambda e: e.activation(out=wkm[:], in_=wkm[:], func=AF.Exp), [], wkmk)
                dm5 = decm[:].rearrange("p c (d j t) -> p c d j t", d=2, t=2)
                for hh in range(2):
                    P.op("dve", lambda e, hh=hh: e.tensor_copy(out=decp[hh * 64:(hh + 1) * 64], in_=dm5[hh * 64:(hh + 1) * 64, :, :, :, hh]),
                         decmk, [("t_decp", hh)])
                decpk = [("t_decp", 0), ("t_decp", 1)]
                if c2stop == "g":
                    P.barrier()
                    return

                def kw_ops(c, d, kwt, kwk):
                    P.op("pool", lambda e: e.tensor_tensor(
                        out=kwt[:, 0:384].rearrange("p (h c) -> p h c", c=64), in0=ktok[:, c, 0:384].rearrange("p (h c) -> p h c", c=64),
                        in1=wkr[:, d, :].unsqueeze(2).to_broadcast([128, 6, 64]), op=ALU.mult), ["t_ktok", "t_wkr"], [(kwk, 0)])
                    P.op("pool", lambda e: e.tensor_tensor(
                        out=kwt[:, 384:640].rearrange("p (h c) -> p h c", c=64), in0=ktok[:, c, 384:640].rearrange("p (h c) -> p h c", c=64),
                        in1=wkm[:, c, d * 4:(d + 1) * 4].unsqueeze(2).to_broadcast([128, 4, 64]), op=ALU.mult),
                         ["t_ktok"] + wkmk, [(kwk, 1)])

                def state_update(c, d, kwt, kwk, S_, Sk, N_, Nk, bank_i):
                    ps = BK[bank_i]

                    def fds(e):
                        r = []
                        for h in range(10):
                            j, hh = h // 2, h % 2
                            r.append(e.matmul(ps[hh * 64:(hh + 1) * 64, j * 64:(j + 1) * 64], kwt[:, h * 64:(h + 1) * 64],
                                              vtok[:, c, h * 64:(h + 1) * 64], start=True, stop=True))
                        for h in range(6, 10):
                            j, hh = h // 2, h % 2
                            r.append(e.matmul(ps[hh * 64:(hh + 1) * 64, 320 + (j - 3) * 64:320 + (j - 2) * 64], kwt[:, h * 64:(h + 1) * 64],
                                              ones64[:, :], start=True, stop=True))
                        return r
                    P.op("pe", fds, [(kwk, 0), (kwk, 1), "t_ones64"] + vkeys, [pk(bank_i)])
                    for j in range(5):
                        dec_ap = decr[:, d, j:j + 1] if j < 3 else decp[:, c, d, j - 3:j - 2]
                        P.op("dve", lambda e, j=j, dec_ap=dec_ap: e.scalar_tensor_tensor(
                            out=S_[:, j, :], in0=S_[:, j, :], scalar=dec_ap, in1=ps[:, j * 64:(j + 1) * 64], op0=ALU.mult, op1=ALU.add),
                             ["t_decr"] + decpk, [pk(bank_i), (Sk, j)])
                    for j in range(2):
                        dec_ap = decp[:, c, d, j:j + 1]
                        P.op("dve", lambda e, j=j, dec_ap=dec_ap: e.scalar_tensor_tensor(
                            out=N_[:, j, :], in0=N_[:, j, :], scalar=dec_ap, in1=ps[:, 320 + j * 64:320 + (j + 1) * 64],
                            op0=ALU.mult, op1=ALU.add), decpk, [pk(bank_i), (Nk, j)])

                Sfk = [("t_Sf", j) for j in range(5)]
                Sbk_ = [("t_Sb", j) for j in range(5)]
                Nfk = [("t_Nf", j) for j in range(2)]
                Nbk_ = [("t_Nb", j) for j in range(2)]
                P.op("pool", lambda e: e.memset(Sf[:], 0.0), [], Sfk)
                P.op("pool", lambda e: e.memset(Sbk[:], 0.0), [], Sbk_)
                P.op("pool", lambda e: e.memset(Nf[:], 0.0), [], Nfk)
                P.op("pool", lambda e: e.memset(Nbk[:], 0.0), [], Nbk_)
                P.op("pool", lambda e: e.memset(Sf_bf[:], 0.0), [], ["t_Sfbf"])
                P.op("pool", lambda e: e.memset(Nf_bf[:], 0.0), [], ["t_Nfbf"])
                for ic, c in enumerate(bwd_order):
                    P.op("act", lambda e, c=c: e.activation(out=Sb_st[:, c], in_=Sbk[:], func=AF.Copy), Sbk_, [("t_Sbst", c)])
                    P.op("act", lambda e, c=c: e.activation(out=Nb_st[:, c], in_=Nbk[:], func=AF.Copy), Nbk_, [("t_Nbst", c)])
                    kwt, kwk = kw[ic % 2], "t_kw%d" % (ic % 2)
                    kw_ops(c, 1, kwt, kwk)
                    state_update(c, 1, kwt, kwk, Sbk, "t_Sb", Nbk, "t_Nb", 1 + (ic % 2))
                if c2stop == "b":
                    P.barrier()
                    return
                def fwd_vars(ic, c):
                    return (slice(c * 128, (c + 1) * 128), not (last and c < 2), PT[ic % 2], "t_PT%d" % (ic % 2),
                            qd[ic % 2], "t_qd%d" % (ic % 2), yo[ic % 2], "t_yo%d" % (ic % 2))

                def stage1(ic, c):
                    ts, need_out, PTt, PTk, qdt, qdk, yot, yok = fwd_vars(ic, c)
                    if not need_out:
                        return
                    for d in range(2):
                        tri = U if d == 0 else Lw
                        trik = "t_U" if d == 0 else "t_L"
                        neg = NEGf if d == 0 else NEGb
                        negk = "t_NEGf" if d == 0 else "t_NEGb"
                        pa = BK[d]
                        for hm in range(4):
                            lb, lbk = lfb[hm], "t_lfb%d" % hm
                            P.op("dve", lambda e, lb=lb, c=c, d=d, hm=hm: e.tensor_copy(
                                out=lb[:], in_=lf[:, c, d * 4 + hm:d * 4 + hm + 1].to_broadcast([128, 128])), lfk, [lbk])
                            P.op("pe", lambda e, pa=pa, lb=lb, tri=tri, hm=hm: e.matmul(
                                pa[:, hm * 128:(hm + 1) * 128], lb[:, :], tri[:, :], start=True, stop=True), [lbk, trik], [pk(d)])
                        xet, xek = xe[d], "t_xe%d" % d
                        P.op("dve", lambda e, pa=pa, xet=xet, neg=neg: e.tensor_tensor(
                            out=xet[:], in0=pa[:, :].rearrange("p (h t) -> p h t", t=128),
                            in1=neg[:].unsqueeze(1).to_broadcast([128, 4, 128]), op=ALU.add), [negk], [pk(d), xek])
                        Et = Ef if d == 0 else Eb
                        Ek = "t_Ef" if d == 0 else "t_Eb"
                        for hm in range(4):
                            P.op("act", lambda e, Et=Et, xet=xet, hm=hm, c=c, d=d: e.activation(
                                out=Et[:, hm, :], in_=xet[:, hm, :], func=AF.Exp, bias=bias1[:, c, d * 4 + hm:d * 4 + hm + 1], scale=1.0),
                                 [xek, ("t_bias1", c)], [(Ek, hm)])
                            j, hh = hm // 2, hm % 2
                            P.op("act", lambda e, pa=pa, d=d, j=j, hh=hh, hm=hm: e.activation(
                                out=dqm[hh * 64:(hh + 1) * 64, d, j, :], in_=pa[hh * 64:(hh + 1) * 64, hm * 128:(hm + 1) * 128], func=AF.Exp),
                                 [], [pk(d), ("t_dqm", d, j, hh)])
                    dqmk = [("t_dqm", d, j, hh) for d in range(2) for j in range(2) for hh in range(2)]
                    sc_dst = {}
                    for h in range(10):
                        if h < 8:
                            sc_dst[h] = (2 + h % 2, (h // 2) * 128)
                        else:
                            sc_dst[h] = (0 if h == 8 else 1, 0)

                    def fsc(e, c=c):
                        r = []
                        for h in range(10):
                            j, hh = h // 2, h % 2
                            bnk, col = sc_dst[h]
                            r.append(e.matmul(BK[bnk][:, col:col + 128],
                                              kT[hh * 64:(hh + 1) * 64, j, c * 128:(c + 1) * 128],
                                              qT[hh * 64:(hh + 1) * 64, j, c * 128:(c + 1) * 128], start=True, stop=True))
                        return r
                    P.op("pe", fsc, qk_keys, [pk(0), pk(1), pk(2), pk(3)])
                    for par in range(2):
                        P.op("dve", lambda e, PTt=PTt, par=par: e.tensor_tensor(
                            out=PTt[:, par:6:2, :], in0=BK[2 + par][:, 0:384].rearrange("p (h t) -> p h t", t=128),
                            in1=ETr[:, par:6:2, :], op=ALU.mult), ["t_ETr"], [pk(2 + par), (PTk, par)])
                    for hm in range(4):
                        bnk, col = sc_dst[6 + hm]
                        P.op("dve", lambda e, PTt=PTt, hm=hm, bnk=bnk, col=col: e.tensor_tensor(
                            out=PTt[:, 6 + hm, :], in0=BK[bnk][:, col:col + 128], in1=Ef[:, hm, :], op=ALU.mult),
                             [("t_Ef", hm)], [pk(bnk), (PTk, 2 + hm)])
                        P.op("dve", lambda e, PTt=PTt, hm=hm, bnk=bnk, col=col: e.tensor_tensor(
                            out=PTt[:, 10 + hm, :], in0=BK[bnk][:, col:col + 128], in1=Eb[:, hm, :], op=ALU.mult),
                             [("t_Eb", hm)], [pk(bnk), (PTk, 6 + hm)])
                    PTks = [(PTk, i) for i in range(10)]
                    for d in range(2):
                        P.op("pool", lambda e, qdt=qdt, d=d, ts=ts: e.tensor_tensor(out=qdt[:, d, 0:3, :], in0=qT[:, 0:3, ts],
                                                                                   in1=dqr[:, d, :, :], op=ALU.mult),
                             qk_keys + ["t_dqr"], [(qdk, d, 0)])
                        P.op("pool", lambda e, qdt=qdt, d=d, ts=ts: e.tensor_tensor(out=qdt[:, d, 3:5, :], in0=qT[:, 3:5, ts],
                                                                                   in1=dqm[:, d, :, :], op=ALU.mult),
                             qk_keys + dqmk, [(qdk, d, 1)])
                    qdks = [(qdk, d, i) for d in range(2) for i in range(2)]

                def stage2(ic, c):
                    ts, need_out, PTt, PTk, qdt, qdk, yot, yok = fwd_vars(ic, c)
                    PTks = [(PTk, i) for i in range(10)]
                    qdks = [(qdk, d, i) for d in range(2) for i in range(2)]
                    dqmk = [("t_dqm", d, j, hh) for d in range(2) for j in range(2) for hh in range(2)]
                    if need_out:
                        def fo(e, c=c, PTt=PTt, qdt=qdt):
                            r = []
                            for h in range(6):
                                j, hh = h // 2, h % 2
                                rows = slice(hh * 64, (hh + 1) * 64)
                                dst = BK[4][rows, j * 128:(j + 1) * 128]
                                r.append(e.matmul(dst, vtok[:, c, h * 64:(h + 1) * 64], PTt[:, h, :], start=True, stop=False))
                                r.append(e.matmul(dst, Sf_bf[rows, j, :], qdt[rows, 0, j, :], start=False, stop=False))
                                r.append(e.matmul(dst, Sb_st[rows, c, j, :], qdt[rows, 1, j, :], start=False, stop=True))
                            for hm in range(4):
                                h = 6 + hm
                                j, hh, jj = h // 2, h % 2, hm // 2
                                rows = slice(hh * 64, (hh + 1) * 64)
                                for d in range(2):
                                    pt = PTt[:, 6 + 4 * d + hm, :]
                                    st_S = Sf_bf[rows, j, :] if d == 0 else Sb_st[rows, c, j, :]
                                    st_N = Nf_bf[rows, jj, :] if d == 0 else Nb_st[rows, c, jj, :]
                                    dn = BK[5][rows, d * 256 + jj * 128:d * 256 + (jj + 1) * 128]
                                    dd = BK[6][rows, d * 256 + jj * 128:d * 256 + (jj + 1) * 128]
                                    r.append(e.matmul(dn, vtok[:, c, h * 64:(h + 1) * 64], pt, start=True, stop=False))
                                    r.append(e.matmul(dn, st_S, qdt[rows, d, j, :], start=False, stop=True))
                                    r.append(e.matmul(dd, ones64[:, :], pt, start=True, stop=False))
                                    r.append(e.matmul(dd, st_N, qdt[rows, d, j, :], start=False, stop=True))
                            return r
                        P.op("pe", fo, PTks + qdks + vkeys + ["t_Sfbf", "t_Nfbf", ("t_Sbst", c), ("t_Nbst", c), "t_ones64"],
                             [pk(4), pk(5), pk(6)])
                    kwt, kwk = kw[ic % 2], "t_kw%d" % (ic % 2)
                    kw_ops(c, 0, kwt, kwk)
                    state_update(c, 0, kwt, kwk, Sf, "t_Sf", Nf, "t_Nf", 7)
                    P.op("act", lambda e: e.activation(out=Sf_bf[:], in_=Sf[:], func=AF.Copy), Sfk, ["t_Sfbf"])
                    P.op("act", lambda e: e.activation(out=Nf_bf[:], in_=Nf[:], func=AF.Copy), Nfk, ["t_Nfbf"])
                    if need_out:
                        P.op("act", lambda e: e.activation(out=den[:], in_=BK[6][:, :], func=AF.Abs), [], [pk(6), "t_den"])
                        P.op("dve", lambda e: e.tensor_scalar(out=den[:], in0=den[:], scalar1=1.0, scalar2=None, op0=ALU.max), [], ["t_den"])
                        P.op("act", lambda e: e.activation(out=den[:], in_=den[:], func=AF.Ln), [], ["t_den"])
                        P.op("act", lambda e: e.activation(out=den[:], in_=den[:], func=AF.Exp, scale=-1.0), [], ["t_den"])
                        P.op("act", lambda e: e.activation(out=o_bf[:, 0:384], in_=BK[4][:, 0:384], func=AF.Copy), [], [pk(4), ("t_obf", 0)])
                        P.op("dve", lambda e: e.tensor_tensor(out=hh_t[:], in0=BK[5][:, :], in1=den[:], op=ALU.mult),
                             ["t_den"], [pk(5), "t_hh"])
                        P.op("pool", lambda e: e.tensor_tensor(out=o_bf[:, 384:640], in0=hh_t[:, 0:256], in1=hh_t[:, 256:512], op=ALU.add),
                             ["t_hh"], [("t_obf", 1), ("t_obf", 2)])
                        obk = [("t_obf", i) for i in range(3)]
                        P.op("pe", lambda e: [e.matmul(BK[4][:, 0:512], Jm[:, :], o_bf[:, 0:512], start=True, stop=True),
                                              e.matmul(BK[5][:, 0:128], Jm[:, :], o_bf[:, 512:640], start=True, stop=True)],
                             obk + ["t_J"], [pk(4), pk(5)])
                        P.op("dve", lambda e: e.tensor_tensor(out=xc[:, 0:512], in0=o_bf[:, 0:512], in1=BK[4][:, 0:512], op=ALU.subtract),
                             obk, [pk(4), ("t_xc", 0)])
                        P.op("dve", lambda e: e.tensor_tensor(out=xc[:, 512:640], in0=o_bf[:, 512:640], in1=BK[5][:, 0:128], op=ALU.subtract),
                             obk, [pk(5), ("t_xc", 1)])
                        xck = [("t_xc", 0), ("t_xc", 1)]
                        P.op("act", lambda e: e.activation(out=sq[:], in_=xc[:], func=AF.Square), xck, ["t_sq"])
                        P.op("pe", lambda e: [e.matmul(BK[4][:, 0:512], Jm[:, :], sq[:, 0:512], start=True, stop=True),
                                              e.matmul(BK[5][:, 0:128], Jm[:, :], sq[:, 512:640], start=True, stop=True)],
                             ["t_sq", "t_J"], [pk(4), pk(5)])
                        P.op("act", lambda e: e.activation(out=rs[:, 0:512], in_=BK[4][:, 0:512], func=AF.Ln, bias=epsc[:, 0:1]),
                             ["t_epsc"], [pk(4), ("t_rs", 0)])
                        P.op("act", lambda e: e.activation(out=rs[:, 512:640], in_=BK[5][:, 0:128], func=AF.Ln, bias=epsc[:, 0:1]),
                             ["t_epsc"], [pk(5), ("t_rs", 1)])
                        rsk = [("t_rs", 0), ("t_rs", 1)]
                        P.op("act", lambda e: e.activation(out=rs[:], in_=rs[:], func=AF.Exp, scale=-0.5), [], rsk)
                        P.op("dve", lambda e: e.tensor_tensor(out=xc[:], in0=xc[:], in1=rs[:], op=ALU.mult), rsk, xck)
                        P.op("pool", lambda e, yot=yot, ts=ts: e.tensor_tensor(out=yot[:], in0=xc[:].rearrange("p (j t) -> p j t", t=128),
                                                                               in1=gT[:, :, ts], op=ALU.mult),
                             xck + [("t_gT", j) for j in range(5)], [yok])
                        P.dma(self.s_yatt[b, :, :, ts].rearrange("j p t -> p j t"), yot[:], [yok], [("s_yatt", b)])

                stage1(0, fwd_order[0])
                for ic, c in enumerate(fwd_order):
                    P.begin_capture()
                    stage2(ic, c)
                    capA = P.end_capture()
                    P.begin_capture()
                    if ic + 1 < len(fwd_order):
                        stage1(ic + 1, fwd_order[ic + 1])
                    capB = P.end_capture()
                    P.replay(capA, capB)
            P.barrier()


    def rms_rstd(self, xt, xk, n, sq2, rstd, ones, bank_i):
        P = self.P
        ps = self.bank[bank_i]
        for k in range(KC):
            sqt, sqk = sq2[k % 2], "sq2_%d" % (k % 2)
            P.op("act", lambda e, k=k, sqt=sqt: e.activation(out=sqt[:, 0:n], in_=xt[:, k, 0:n], func=AF.Square), [xk], [sqk])
            P.op("pe", lambda e, k=k, sqt=sqt: e.matmul(ps[:, 0:n], ones[:], sqt[:, 0:n], start=(k == 0), stop=(k == KC - 1)),
                 [sqk, "ones_d"], [self.pk(bank_i)])
        P.op("dve", lambda e: e.tensor_scalar(out=rstd[:, 0:n], in0=ps[:, 0:n], scalar1=EPS, scalar2=None, op0=ALU.add),
             [], [self.pk(bank_i), "rstd"])
        P.op("act", lambda e: e.activation(out=rstd[:, 0:n], in_=rstd[:, 0:n], func=AF.Sqrt), [], ["rstd"])
        P.op("dve", lambda e: e.reciprocal(out=rstd[:, 0:n], in_=rstd[:, 0:n]), [], ["rstd"])

    def token_groups(self, with_ctx):
        groups = []
        for b in range(NB):
            if with_ctx:
                groups.append((b, 2, 0, CTX))
            for g in range(4):
                groups.append((b, b, CTX + 512 * g, 512))
        return groups

    def phase_D(self, layer, xsrc, xdst):
        nc, P = self.nc, self.P
        last = layer == DEPTH - 1
        with contextlib.ExitStack() as st:
            sb = self.mk_sb(st)
            Wu = sb("d_wu", [128, KC, D], BF16)
            Wo = sb("d_wo", [128, KC, D], BF16)
            modt = sb("d_mod", [128, 48, 3], F32)
            xg = [sb("d_xg%d" % i, [128, KC, 512], F32) for i in range(2)]
            yb = [sb("d_yb%d" % i, [128, 8, 512], BF16) for i in range(2)]
            gtile = [sb("d_gt%d" % i, [128, 24, 512], BF16) for i in range(2)]
            mm_ = [sb("d_mm%d" % i, [128, 512], F32) for i in range(3)]
            accT = sb("d_acc", [128, KC, 512], BF16)
            P.dma(Wu[:], self.w_up[layer].rearrange("(c p) n -> p c n", p=128), [], ["d_wu"], eng="pool")
            P.dma(Wo[:], self.w_out[layer].rearrange("(c p) n -> p c n", p=128), [], ["d_wo"], eng="pool")
            P.dma(modt[:], self.s_mod[layer], [], ["d_mod"])
            branches = ((0, (0, 1, 2)), (1, (3, 4, 5)), (2, (6, 7)))
            dgroups = self.token_groups(not last)

            def load_D(gi2):
                b2, s2, t2, n2 = dgroups[gi2]
                P.dma(xg[gi2 % 2][:, :, 0:n2], xsrc[b2, :, :, t2:t2 + n2].rearrange("c p t -> p c t"), [], ["d_xg%d" % (gi2 % 2)])
                P.dma(yb[gi2 % 2][:, 0:3, 0:n2], self.seg_yhy[b2, :, :, t2:t2 + n2].rearrange("c p t -> p c t"), [],
                      [("d_yb%d" % (gi2 % 2), 0)])
                P.dma(yb[gi2 % 2][:, 3:8, 0:n2], self.s_yatt[b2, :, :, t2:t2 + n2].rearrange("c p t -> p c t"), [],
                      [("d_yb%d" % (gi2 % 2), 1)])
                P.dma(gtile[gi2 % 2][:, :, 0:n2], self.seg["gate"][b2, :, :, t2:t2 + n2].rearrange("c p t -> p c t"), [],
                      ["d_gt%d" % (gi2 % 2)], eng="pool")

            load_D(0)
            for gi_, (b, s_, t0, n) in enumerate(dgroups):
                xt, xk = xg[gi_ % 2], "d_xg%d" % (gi_ % 2)
                yt, yk = yb[gi_ % 2], "d_yb%d" % (gi_ % 2)
                gt_, gk = gtile[gi_ % 2], "d_gt%d" % (gi_ % 2)
                if gi_ + 1 < len(dgroups):
                    load_D(gi_ + 1)
                for m in range(KC):
                    for (bi_, ks) in branches:
                        ps = self.bank[bi_]

                        def fup(e, ps=ps, ks=ks, m=m, yt=yt, n=n):
                            return [e.matmul(ps[:, 0:n], Wu[:, k, m * 128:(m + 1) * 128], yt[:, k, 0:n],
                                             start=(k == ks[0]), stop=(k == ks[-1])) for k in ks]
                        P.op("pe", fup, ["d_wu", (yk, 0), (yk, 1)], [self.pk(bi_)])
                        P.op("dve", lambda e, ps=ps, bi_=bi_, m=m, gt_=gt_, n=n: e.tensor_tensor(
                            out=mm_[bi_][:, 0:n], in0=ps[:, 0:n], in1=gt_[:, bi_ * 8 + m, 0:n], op=ALU.mult),
                             [gk], [self.pk(bi_), "d_mm%d" % bi_])
                    P.op("pool", lambda e, n=n: e.tensor_tensor(out=mm_[0][:, 0:n], in0=mm_[0][:, 0:n], in1=mm_[1][:, 0:n], op=ALU.add),
                         ["d_mm1"], ["d_mm0"])
                    P.op("dve", lambda e, m=m, n=n: e.tensor_tensor(out=accT[:, m, 0:n], in0=mm_[0][:, 0:n], in1=mm_[2][:, 0:n], op=ALU.add),
                         ["d_mm0", "d_mm2"], [("d_acc", m)])
                acck = [("d_acc", m) for m in range(KC)]
                for m2 in range(KC):
                    bi_ = 3 + (m2 % 2)
                    ps = self.bank[bi_]

                    def fout(e, ps=ps, m2=m2, n=n):
                        return [e.matmul(ps[:, 0:n], Wo[:, k, m2 * 128:(m2 + 1) * 128], accT[:, k, 0:n],
                                         start=(k == 0), stop=(k == KC - 1)) for k in range(KC)]
                    P.op("pe", fout, ["d_wo"] + acck, [self.pk(bi_)])
                    P.op("dve", lambda e, ps=ps, m2=m2, xt=xt, n=n, s_=s_: e.scalar_tensor_tensor(
                        out=xt[:, m2, 0:n], in0=ps[:, 0:n], scalar=modt[:, 16 + m2, s_:s_ + 1], in1=xt[:, m2, 0:n],
                        op0=ALU.mult, op1=ALU.add), ["d_mod"], [self.pk(bi_), xk])
                P.dma(xdst[b, :, :, t0:t0 + n].rearrange("c p t -> p c t"), xt[:, :, 0:n], [xk], [("xdst", b)])
            P.barrier()

    WBLK = ((0, 6), (6, 12), (12, 17), (17, 22))

    def ffn_core(self, W1, W3, W2, wkeys, h, hks, n, aT, s1, epilogue):
        P = self.P
        blk_of = {}
        for bi_, (a_, b_) in enumerate(self.WBLK):
            for j in range(a_, b_):
                blk_of[j] = bi_
        if isinstance(wkeys, dict):
            k1 = lambda j: [wkeys["w1"][blk_of[j]]]
            k3 = lambda j: [wkeys["w3"][blk_of[j]]]
            k2 = list(wkeys["w2"])
        else:
            k1 = k3 = lambda j: list(wkeys)
            k2 = list(wkeys)
        for j in range(FC):
            b1, b3 = j % 2, 2 + (j % 2)
            p1, p3 = self.bank[b1], self.bank[b3]

            def f1(e, pp, Wt, j=j):
                return [e.matmul(pp[:, 0:n], Wt[:, k, j * 128:(j + 1) * 128], h[:, k, 0:n], start=(k == 0), stop=(k == KC - 1))
                        for k in range(KC)]
            P.op("pe", lambda e, p1=p1, f1=f1: f1(e, p1, W1), hks + k1(j), [self.pk(b1)])
            P.op("pe", lambda e, p3=p3, f1=f1: f1(e, p3, W3), hks + k3(j), [self.pk(b3)])
            st_, sk = s1[j % 2], "f_s1_%d" % (j % 2)
            P.op("act", lambda e, p1=p1, st_=st_: e.activation(out=st_[:, 0:n], in_=p1[:, 0:n], func=AF.Silu), [], [self.pk(b1), sk])
            P.op("dve", lambda e, p3=p3, st_=st_, j=j: e.tensor_tensor(out=aT[:, j, 0:n], in0=p3[:, 0:n], in1=st_[:, 0:n], op=ALU.mult),
                 [sk], [self.pk(b3), ("f_aT", j)])
        ak = [("f_aT", j) for j in range(FC)]
        for m2 in range(KC):
            bo = 4 + (m2 % 2)
            po = self.bank[bo]

            def f2(e, po=po, m2=m2):
                return [e.matmul(po[:, 0:n], W2[:, j, m2 * 128:(m2 + 1) * 128], aT[:, j, 0:n], start=(j == 0), stop=(j == FC - 1))
                        for j in range(FC)]
            P.op("pe", f2, ak + k2, [self.pk(bo)])
            epilogue(m2, po, self.pk(bo))

    def phase_E_dense(self, layer, xsrc, xdst):
        nc, P = self.nc, self.P
        last = layer == DEPTH - 1
        jd = layer // 2
        with contextlib.ExitStack() as st:
            sb = self.mk_sb(st)
            W1 = sb("e_w1", [128, KC, D_FF], BF16)
            W3 = sb("e_w3", [128, KC, D_FF], BF16)
            W2 = sb("e_w2", [128, FC, D], BF16)
            modt = sb("e_mod", [128, 48, 3], F32)
            gm = sb("e_g", [128, KC], F32)
            Am = sb("e_A", [128, KC, 3], F32)
            ones = sb("e_ones", [128, 128], BF16)
            xt = sb("e_xg", [128, KC, 512], F32)
            sq2 = [sb("e_sq%d" % i, [128, 512], BF16) for i in range(2)]
            rstd = sb("e_rstd", [128, 512], F32)
            t1 = [sb("e_t1%d" % i, [128, 512], F32) for i in range(2)]
            hT = sb("e_hT", [128, KC, 512], BF16)
            aT = sb("e_aT", [128, FC, 512], BF16)
            s1 = [sb("e_s1%d" % i, [128, 512], F32) for i in range(2)]
            w1s = self.ffn_w1[jd].rearrange("(c p) n -> p c n", p=128)
            w3s = self.ffn_w3[jd].rearrange("(c p) n -> p c n", p=128)
            w2s = self.ffn_w2[jd].rearrange("(c p) n -> p c n", p=128)
            for q, (ja, jb) in enumerate(self.WBLK):
                P.dma(W1[:, :, ja * 128:jb * 128], w1s[:, :, ja * 128:jb * 128], [], [("e_w1", q)], eng="pool")
                P.dma(W3[:, :, ja * 128:jb * 128], w3s[:, :, ja * 128:jb * 128], [], [("e_w3", q)], eng="pool")
            for q, (ja, jb) in enumerate(self.WBLK):
                P.dma(W2[:, ja:jb, :], w2s[:, ja:jb, :], [], [("e_w2", q)], eng="pool")
            wkeys = {"w1": [("e_w1", q) for q in range(4)], "w3": [("e_w3", q) for q in range(4)],
                     "w2": [("e_w2", q) for q in range(4)]}
            P.dma(modt[:], self.s_mod[layer], [], ["e_mod"])
            P.dma(gm[:], self.g_ffnT[layer], [], ["e_g"])
            P.op("pool", lambda e: e.memset(ones[:], 1.0 / D), [], ["ones_d"])
            P.op("dve", lambda e: e.tensor_scalar(out=Am[:], in0=modt[:, 32:40, :], scalar1=1.0, scalar2=None, op0=ALU.add),
                 ["e_mod"], ["e_A"])
            for s in range(3):
                P.op("dve", lambda e, s=s: e.tensor_tensor(out=Am[:, :, s], in0=Am[:, :, s], in1=gm[:], op=ALU.mult),
                     ["e_A", "e_g"], ["e_A"])
            for gi_, (b, s_, t0, n) in enumerate(self.token_groups(not last)):
                xk = "e_xg"
                P.dma(xt[:, :, 0:n], xsrc[b, :, :, t0:t0 + n].rearrange("c p t -> p c t"), [], [xk])
                self.rms_rstd(xt, xk, n, sq2, rstd, ones, 6)
                hks = [("e_hT", k) for k in range(KC)]
                for k in range(KC):
                    tt, tk_ = t1[k % 2], "e_t1%d" % (k % 2)
                    P.op("dve", lambda e, k=k, tt=tt, n=n: e.tensor_tensor(out=tt[:, 0:n], in0=xt[:, k, 0:n], in1=rstd[:, 0:n], op=ALU.mult),
                         [xk, "rstd"], [tk_])
                    P.op("act", lambda e, k=k, tt=tt, n=n, s_=s_: e.activation(
                        out=hT[:, k, 0:n], in_=tt[:, 0:n], func=AF.Identity, scale=Am[:, k, s_:s_ + 1], bias=modt[:, 24 + k, s_:s_ + 1]),
                         [tk_, "e_A", "e_mod"], [("e_hT", k)])

                def epi(m2, po, pok, n=n, s_=s_):
                    P.op("dve", lambda e: e.scalar_tensor_tensor(
                        out=xt[:, m2, 0:n], in0=po[:, 0:n], scalar=modt[:, 40 + m2, s_:s_ + 1], in1=xt[:, m2, 0:n],
                        op0=ALU.mult, op1=ALU.add), ["e_mod"], [pok, xk])
                self.ffn_core(W1, W3, W2, wkeys, hT, hks, n, aT, s1, epi)
                P.dma(xdst[b, :, :, t0:t0 + n].rearrange("c p t -> p c t"), xt[:, :, 0:n], [xk], [("xdst", b)])
            P.barrier()

    def phase_E_moe(self, layer, xsrc, xdst):
        nc, P = self.nc, self.P
        jm = layer // 2
        groups = self.token_groups(False)
        with contextlib.ExitStack() as st:
            sb = self.mk_sb(st)
            modt = sb("g_mod", [128, 48, 3], F32)
            gm = sb("g_g", [128, KC], F32)
            Am = sb("g_A", [128, KC, 3], F32)
            ones = sb("g_ones", [128, 128], BF16)
            idf = sb("g_idf", [128, 128], F32)
            Wr = sb("g_wr", [128, KC, NEXP], F32)
            rb = sb("g_rb", [128, NEXP], F32)
            xg = [sb("g_xg%d" % i, [128, KC, 512], F32) for i in range(2)]
            sq2 = [sb("g_sq%d" % i, [128, 512], BF16) for i in range(2)]
            rstd = sb("g_rstd", [128, 512], F32)
            t1 = [sb("g_t1%d" % i, [128, 512], F32) for i in range(2)]
            hF = sb("g_hF", [128, KC, 512], F32)
            hT = sb("g_hT", [128, KC, 512], BF16)
            lg = sb("g_lg", [128, NEXP], F32)
            lg2 = sb("g_lg2", [128, NEXP], F32)
            eq1 = sb("g_eq1", [128, NEXP], F32)
            eq2 = sb("g_eq2", [128, NEXP], F32)
            mx = sb("g_mx", [128, 4], F32)
            comb = sb("g_comb", [128, NEXP], F32)
            cT = sb("g_cT", [NEXP, 512], F32)
            P.dma(modt[:], self.s_mod[layer], [], ["g_mod"])
            P.dma(gm[:], self.g_ffnT[layer], [], ["g_g"])
            P.dma(idf[:], self.ident_d[:, :], [], ["g_idf"])
            P.dma(Wr[:], self.moe_router[jm].rearrange("(c p) n -> p c n", p=128), [], ["g_wr"])
            P.dma(rb[:], self.moe_router_b[jm:jm + 1, :].to_broadcast([128, NEXP]), [], ["g_rb"])
            P.op("pool", lambda e: e.memset(ones[:], 1.0 / D), [], ["ones_d"])
            P.op("dve", lambda e: e.tensor_scalar(out=Am[:], in0=modt[:, 32:40, :], scalar1=1.0, scalar2=None, op0=ALU.add),
                 ["g_mod"], ["g_A"])
            for s in range(3):
                P.op("dve", lambda e, s=s: e.tensor_tensor(out=Am[:, :, s], in0=Am[:, :, s], in1=gm[:], op=ALU.mult),
                     ["g_A", "g_g"], ["g_A"])
            for gi_, (b, s_, t0, n) in enumerate(groups):
                xt, xk = xg[gi_ % 2], "g_xg%d" % (gi_ % 2)
                tl = t0 - CTX
                P.dma(xt[:, :, 0:n], xsrc[b, :, :, t0:t0 + n].rearrange("c p t -> p c t"), [], [xk])
                P.dma(xdst[b, :, :, t0:t0 + n].rearrange("c p t -> p c t"), xt[:, :, 0:n], [xk], [("xdst", b, gi_)])
                self.rms_rstd(xt, xk, n, sq2, rstd, ones, 6)
                for k in range(KC):
                    tt, tk_ = t1[k % 2], "g_t1%d" % (k % 2)
                    P.op("dve", lambda e, k=k, tt=tt, xt=xt: e.tensor_tensor(out=tt[:, 0:n], in0=xt[:, k, 0:n], in1=rstd[:, 0:n], op=ALU.mult),
                         [xk, "rstd"], [tk_])
                    P.op("act", lambda e, k=k, tt=tt, s_=s_: e.activation(
                        out=hF[:, k, 0:n], in_=tt[:, 0:n], func=AF.Identity, scale=Am[:, k, s_:s_ + 1], bias=modt[:, 24 + k, s_:s_ + 1]),
                         [tk_, "g_A", "g_mod"], [("g_hF", k)])
                    P.op("act", lambda e, k=k, tt=tt, s_=s_: e.activation(
                        out=hT[:, k, 0:n], in_=tt[:, 0:n], func=AF.Identity, scale=Am[:, k, s_:s_ + 1], bias=modt[:, 24 + k, s_:s_ + 1]),
                         [tk_, "g_A", "g_mod"], [("g_hT", k)])
                P.dma(self.s_hffn[b, :, :, tl:tl + n].rearrange("c p t -> p c t"), hT[:, :, 0:n], [("g_hT", k) for k in range(KC)],
                      [("s_hffn", b, gi_)])
                hFk = [("g_hF", k) for k in range(KC)]
                for tt_ in range(n // 128):
                    ps = self.bank[tt_ % 2]
                    pbk = self.pk(tt_ % 2)

                    def frt(e, ps=ps, tt_=tt_):
                        return [e.matmul(ps[:, 0:NEXP], hF[:, k, tt_ * 128:(tt_ + 1) * 128], Wr[:, k, :], start=(k == 0), stop=(k == KC - 1))
                                for k in range(KC)]
                    P.op("pe", frt, hFk + ["g_wr"], [pbk])
                    P.op("dve", lambda e, ps=ps: e.tensor_tensor(out=lg[:], in0=ps[:, 0:NEXP], in1=rb[:], op=ALU.add), ["g_rb"], [pbk, "g_lg"])
                    P.op("dve", lambda e: e.tensor_reduce(out=mx[:, 0:1], in_=lg[:], axis=AX.X, op=ALU.max), ["g_lg"], [("g_mx", 0)])
                    P.op("dve", lambda e: e.tensor_scalar(out=eq1[:], in0=lg[:], scalar1=mx[:, 0:1], scalar2=None, op0=ALU.is_equal),
                         ["g_lg", ("g_mx", 0)], ["g_eq1"])
                    P.op("dve", lambda e: e.scalar_tensor_tensor(out=lg2[:], in0=eq1[:], scalar=-1.0e9, in1=lg[:], op0=ALU.mult, op1=ALU.add),
                         ["g_eq1", "g_lg"], ["g_lg2"])
                    P.op("dve", lambda e: e.tensor_reduce(out=mx[:, 1:2], in_=lg2[:], axis=AX.X, op=ALU.max), ["g_lg2"], [("g_mx", 1)])
                    P.op("dve", lambda e: e.tensor_scalar(out=eq2[:], in0=lg2[:], scalar1=mx[:, 1:2], scalar2=None, op0=ALU.is_equal),
                         ["g_lg2", ("g_mx", 1)], ["g_eq2"])
                    P.op("dve", lambda e: e.tensor_tensor(out=mx[:, 2:3], in0=mx[:, 0:1], in1=mx[:, 1:2], op=ALU.subtract),
                         [("g_mx", 0), ("g_mx", 1)], [("g_mx", 2)])
                    P.op("act", lambda e: e.activation(out=mx[:, 2:3], in_=mx[:, 2:3], func=AF.Sigmoid), [], [("g_mx", 2)])
                    P.op("dve", lambda e: e.tensor_scalar(out=mx[:, 3:4], in0=mx[:, 2:3], scalar1=-1.0, scalar2=1.0, op0=ALU.mult, op1=ALU.add),
                         [("g_mx", 2)], [("g_mx", 3)])
                    P.op("dve", lambda e: e.tensor_scalar(out=comb[:], in0=eq1[:], scalar1=mx[:, 2:3], scalar2=None, op0=ALU.mult),
                         ["g_eq1", ("g_mx", 2)], ["g_comb"])
                    P.op("dve", lambda e: e.scalar_tensor_tensor(out=comb[:], in0=eq2[:], scalar=mx[:, 3:4], in1=comb[:], op0=ALU.mult, op1=ALU.add),
                         ["g_eq2", ("g_mx", 3)], ["g_comb"])
                    pt_ = self.bank[2 + (tt_ % 2)]
                    ptk = self.pk(2 + (tt_ % 2))
                    P.op("pe", lambda e, pt_=pt_: e.transpose(pt_[0:NEXP, 0:128], comb[:, :], idf[:, :]), ["g_comb", "g_idf"], [ptk])
                    P.op("act", lambda e, pt_=pt_, tt_=tt_: e.activation(out=cT[:, tt_ * 128:(tt_ + 1) * 128], in_=pt_[0:NEXP, 0:128], func=AF.Copy),
                         [], [ptk, ("g_cT", tt_)])
                P.dma(self.s_comb[:, b * LAT + tl:b * LAT + tl + n], cT[:, 0:n], [("g_cT", q) for q in range(n // 128)], [("s_comb", b, gi_)])
            P.barrier()
        import os as _os
        if _os.environ.get("MOE_PRE_ONLY"):
            return
        with contextlib.ExitStack() as st:
            sb = self.mk_sb(st)
            W1 = sb("x_w1", [128, KC, D_FF], BF16)
            W3 = sb("x_w3", [128, KC, D_FF], BF16)
            W2 = sb("x_w2", [128, FC, D], BF16)
            modt = sb("x_mod", [128, 48, 3], F32)
            sel = sb("x_sel", [NEXP, NEXP, 128], F32)
            hT2 = [sb("x_hT%d" % i, [128, KC, 512], BF16) for i in range(2)]
            xt = sb("x_xg", [128, KC, 512], F32)
            aT = sb("x_aT", [128, FC, 512], BF16)
            s1 = [sb("x_s1%d" % i, [128, 512], BF16) for i in range(2)]
            cT2 = [sb("x_cT%d" % i, [NEXP, 512], F32) for i in range(2)]
            cb = sb("x_cb", [128, 512], F32)
            tmp = [sb("x_tmp%d" % i, [128, 512], F32) for i in range(2)]
            P.dma(modt[:], self.s_mod[layer], [], ["x_mod"])
            P.dma(sel[:], self.moe_sel[:, :, :], [], ["x_sel"])
            wkeys = {"w1": [("x_w1", q) for q in range(4)], "w3": [("x_w3", q) for q in range(4)],
                     "w2": [("x_w2", q) for q in range(4)]}
            grp = groups[:int(_os.environ.get("MOE_NGRP", 8))]
            nex = int(_os.environ.get("MOE_NEXP", NEXP))
            work = [(ex, gi_) for ex in range(nex) for gi_ in range(len(grp))]

            def load_h(wi):
                ex, gi_ = work[wi]
                b, s_, t0, n = grp[gi_]
                tl = t0 - CTX
                ht, hk = hT2[wi % 2], "x_hT%d" % (wi % 2)
                P.dma(ht[:, :, 0:n], self.s_hffn[b, :, :, tl:tl + n].rearrange("c p t -> p c t"), [], [hk])
                ct, ck = cT2[wi % 2], "x_cT%d" % (wi % 2)
                P.dma(ct[:, 0:n], self.s_comb[:, b * LAT + tl:b * LAT + tl + n], [], [ck])

            def load_x(wi):
                ex, gi_ = work[wi]
                b, s_, t0, n = grp[gi_]
                P.dma(xt[:, :, 0:n], xdst[b, :, :, t0:t0 + n].rearrange("c p t -> p c t"), [("xdst", b, gi_)], ["x_xg"])

            load_h(0)
            load_x(0)
            for wi, (ex, gi_) in enumerate(work):
                b, s_, t0, n = grp[gi_]
                if gi_ == 0:
                    w1s = self.moe_w1[jm, ex].rearrange("(c p) n -> p c n", p=128)
                    w3s = self.moe_w3[jm, ex].rearrange("(c p) n -> p c n", p=128)
                    w2s = self.moe_w2[jm, ex].rearrange("(c p) n -> p c n", p=128)
                    for q, (ja, jb) in enumerate(self.WBLK):
                        P.dma(W1[:, :, ja * 128:jb * 128], w1s[:, :, ja * 128:jb * 128], [], [("x_w1", q)], eng="pool")
                        P.dma(W3[:, :, ja * 128:jb * 128], w3s[:, :, ja * 128:jb * 128], [], [("x_w3", q)], eng="pool")
                    for q, (ja, jb) in enumerate(self.WBLK):
                        P.dma(W2[:, ja:jb, :], w2s[:, ja:jb, :], [], [("x_w2", q)], eng="pool")
                ht, hk = hT2[wi % 2], "x_hT%d" % (wi % 2)
                ct, ck = cT2[wi % 2], "x_cT%d" % (wi % 2)
                if wi + 1 < len(work):
                    load_h(wi + 1)
                pcb = self.bank[7]
                P.op("pe", lambda e: e.matmul(pcb[:, 0:n], sel[:, ex, :], ct[:, 0:n], start=True, stop=True),
                     ["x_sel", ck], [self.pk(7)])
                P.op("act", lambda e: e.activation(out=cb[:, 0:n], in_=pcb[:, 0:n], func=AF.Copy), [], [self.pk(7), "x_cb"])

                def epi(m2, po, pok, n=n, s_=s_):
                    tt, tk_ = tmp[m2 % 2], "x_tmp%d" % (m2 % 2)
                    P.op("dve", lambda e: e.tensor_tensor(out=tt[:, 0:n], in0=po[:, 0:n], in1=cb[:, 0:n], op=ALU.mult),
                         ["x_cb"], [pok, tk_])
                    P.op("dve", lambda e: e.scalar_tensor_tensor(
                        out=xt[:, m2, 0:n], in0=tt[:, 0:n], scalar=modt[:, 40 + m2, s_:s_ + 1], in1=xt[:, m2, 0:n],
                        op0=ALU.mult, op1=ALU.add), ["x_mod", tk_], ["x_xg"])
                self.ffn_core(W1, W3, W2, wkeys, ht, [hk], n, aT, s1, epi)
                P.dma(xdst[b, :, :, t0:t0 + n].rearrange("c p t -> p c t"), xt[:, :, 0:n], ["x_xg"], [("xdst", b, gi_)])
                if wi + 1 < len(work):
                    load_x(wi + 1)
            P.barrier()

    def phase_final(self, xsrc):
        nc, P = self.nc, self.P
        with contextlib.ExitStack() as st:
            sb = self.mk_sb(st)
            gf = sb("z_g", [128, KC], F32)
            ones = sb("z_ones", [128, 128], BF16)
            xg = [sb("z_xg%d" % i, [128, KC, 512], F32) for i in range(2)]
            sq2 = [sb("z_sq%d" % i, [128, 512], BF16) for i in range(2)]
            rstd = sb("z_rstd", [128, 512], F32)
            P.dma(gf[:], self.g_finT[:, :], [], ["z_g"])
            P.op("pool", lambda e: e.memset(ones[:], 1.0 / D), [], ["ones_d"])
            for gi_, (b, s_, t0, n) in enumerate(self.token_groups(False)):
                xt, xk = xg[gi_ % 2], "z_xg%d" % (gi_ % 2)
                P.dma(xt[:, :, 0:n], xsrc[b, :, :, t0:t0 + n].rearrange("c p t -> p c t"), [], [xk])
                self.rms_rstd(xt, xk, n, sq2, rstd, ones, gi_ % 2)
                for k in range(KC):
                    P.op("dve", lambda e, k=k, xt=xt, n=n: e.scalar_tensor_tensor(
                        out=xt[:, k, 0:n], in0=xt[:, k, 0:n], scalar=gf[:, k:k + 1], in1=rstd[:, 0:n], op0=ALU.mult, op1=ALU.mult),
                         ["z_g", "rstd"], [xk])
                o = P.dma(self.outT[b, :, :, t0 - CTX:t0 - CTX + n].rearrange("c p t -> p c t"), xt[:, :, 0:n], [xk], [("out", b)],
                          eng="pool")
                self.final_ops.append(o)
            P.barrier()


def prep_shared(inp):
    sh = {}
    sh["w_mod"] = np.ascontiguousarray(inp["w_mod"].reshape(DEPTH, KC, 128, 6 * D).transpose(0, 2, 1, 3))
    sh["b_modT"] = np.ascontiguousarray(inp["b_mod"].reshape(DEPTH, 48, 128).transpose(0, 2, 1))
    sh["g_mixT"] = np.ascontiguousarray(inp["g_mix"].reshape(DEPTH, KC, 128).transpose(0, 2, 1))
    sh["g_ffnT"] = np.ascontiguousarray(inp["g_ffn"].reshape(DEPTH, KC, 128).transpose(0, 2, 1))
    sh["g_finT"] = np.ascontiguousarray(inp["g_final"].reshape(KC, 128).T)
    sh["w_in"] = np.ascontiguousarray(inp["w_in"].reshape(DEPTH, KC, 128, N_IN).transpose(0, 2, 1, 3))
    b_inF = np.zeros((DEPTH, 128, len(F_CHUNKS)), np.float32)
    for ci, (_, _, col0, _) in enumerate(F_CHUNKS):
        b_inF[:, :, ci] = inp["b_in"][:, col0:col0 + 128]
    sh["b_inF"] = b_inF
    sh["b_in"] = np.ascontiguousarray(inp["b_in"])
    sh["ident"] = np.eye(128, dtype=np.float32)
    sh["hy_f1"] = np.ascontiguousarray(inp["hy_f1"])
    sh["hy_f2"] = np.ascontiguousarray(inp["hy_f2"])
    sh["hy_f3"] = np.ascontiguousarray(inp["hy_f3"])
    sh["hy_fb1"] = np.ascontiguousarray(inp["hy_fb1"].reshape(DEPTH, 64, 1))
    sh["hy_fb2"] = np.ascontiguousarray(inp["hy_fb2"].reshape(DEPTH, 64, 1))
    sh["hy_decay"] = np.ascontiguousarray(inp["hy_decay"])
    sh["hy_convT"] = np.ascontiguousarray(inp["hy_conv"].reshape(DEPTH, 3, 9, 128).transpose(0, 3, 2, 1))
    sh["hy_skipT"] = np.ascontiguousarray(inp["hy_skip"].reshape(DEPTH, 2, 3, 128).transpose(0, 3, 1, 2))
    for tag, L in (("l", LAT), ("c", CTX)):
        for k, v in hyena_consts(L).items():
            sh["hc_%s_%s" % (k, tag)] = v
    sh.update(attn_consts())
    sh["ml_convT"] = np.ascontiguousarray(inp["ml_conv"].reshape(DEPTH, 3, 4, 128).transpose(0, 3, 2, 1))
    sh["ret_decay"] = np.ascontiguousarray(inp["ret_decay"])
    for k in ("w_up", "w_out", "ffn_w1", "ffn_w3", "ffn_w2", "moe_router", "moe_router_b", "moe_w1", "moe_w3", "moe_w2"):
        sh[k] = np.ascontiguousarray(inp[k])
    sel = np.zeros((NEXP, NEXP, 128), np.float32)
    for e_ in range(NEXP):
        sel[e_, e_, :] = 1.0
    sh["moe_sel"] = sel
    sh["ret_decay_h"] = np.ascontiguousarray(inp["ret_decay"].reshape(DEPTH, 2, 3, 2).transpose(0, 1, 3, 2))
    sh["ml_gate_bias"] = np.ascontiguousarray(inp["ml_gate_bias"].reshape(DEPTH, 16))
    return sh


def prep_core(inp, i):
    m = {}
    x = inp["x"][NB * i:NB * i + NB]
    ctx = inp["ctx"][NB * i:NB * i + NB]
    full = np.concatenate([ctx, x], axis=1)
    m["xin"] = np.ascontiguousarray(full.reshape(NB, TAU, KC, 128).transpose(0, 2, 3, 1))
    cc = np.concatenate([inp["c"][NB * i:NB * i + NB], inp["c_ctx"][None, :]], axis=0)
    m["sT"] = np.ascontiguousarray(cc.reshape(3, KC, 128).transpose(2, 1, 0))
    return m


def kernel(**inputs):
    inp = {k: np.asarray(v) for k, v in inputs.items()}
    bld = Builder()
    nc = bld.build()
    sh = prep_shared(inp)
    in_maps = []
    for i in range(NCORES):
        m = dict(sh)
        m.update(prep_core(inp, i))
        in_maps.append({k: v for k, v in m.items() if k in bld.dram})
    res = run_bass_kernel_spmd(nc, in_maps, core_ids=list(range(NCORES)))
    outs = [r["outT"] for r in res.results]
    full = np.concatenate(outs, axis=0)
    return np.ascontiguousarray(full.transpose(0, 3, 1, 2).reshape(NCORES * NB, LAT, D)).astype(np.float32)


def hyena_consts(L):
    N = 2 * L
    kch = L // 128
    pos = np.arange(L, dtype=np.float32)
    t = pos / np.float32(max(L - 1, 1))
    n_bands = 16
    bands = np.linspace(1e-4, n_bands - 1, n_bands, dtype=np.float32)
    z = (np.float32(2.0 * math.pi) * pos / np.float32(L))[:, None] * bands[None, :]
    feats = np.concatenate([t[:, None], np.cos(z), -np.sin(z)], axis=-1).astype(np.float32)
    featsT = np.ascontiguousarray(feats.T)
    tcol = np.ascontiguousarray((-t).reshape(kch, 128).T).astype(np.float32)
    n = np.arange(L, dtype=np.float64)[:, None]
    k = np.arange(L, dtype=np.float64)[None, :]
    th = 2.0 * math.pi * n * (k + 0.5) / N
    C = np.cos(th)
    S = np.sin(th)
    bf = ml_dtypes.bfloat16
    fC = np.ascontiguousarray(C.reshape(kch, 128, kch, 128).transpose(2, 1, 0, 3)).astype(bf)
    fS = np.ascontiguousarray(S.reshape(kch, 128, kch, 128).transpose(2, 1, 0, 3)).astype(bf)
    gsz = min(512, L)
    ng = L // gsz
    CT = (C.T * (2.0 / N))
    ST = (S.T * (2.0 / N))
    iC = np.ascontiguousarray(CT.reshape(kch, 128, ng, gsz).transpose(2, 1, 0, 3)).astype(bf)
    iS = np.ascontiguousarray(ST.reshape(kch, 128, ng, gsz).transpose(2, 1, 0, 3)).astype(bf)
    return {"featsT": featsT, "tcol": tcol, "fC": fC, "fS": fS, "iC": iC, "iS": iS}


def attn_consts():
    c = {}
    GRID_W = 64
    t = np.arange(LAT)
    row_id = (t // GRID_W).astype(np.float32)
    col_id = (t % GRID_W).astype(np.float32)
    q4 = 16
    inv = (1.0 / (np.float32(10000.0) ** (np.arange(q4, dtype=np.float32) / np.float32(q4)))).astype(np.float32)
    cosT = np.zeros((128, LAT), np.float32)
    sinT = np.zeros((128, LAT), np.float32)
    RT = np.zeros((128, 128), np.float32)
    for p in range(128):
        d = p % 64
        j = d % 16
        ang = (row_id if d < 32 else col_id) * inv[j]
        cosT[p] = np.cos(ang)
        sinT[p] = np.sin(ang)
        if (d % 32) < 16:
            RT[p + 16, p] = -1.0
        else:
            RT[p - 16, p] = 1.0
    c["rope_cos"] = cosT
    c["rope_sin"] = sinT
    c["rope_RT"] = RT.astype(ml_dtypes.bfloat16)
    i = np.arange(128)
    s_, t_ = i[:, None], i[None, :]
    c["ac_U"] = (s_ <= t_).astype(np.float32)
    c["ac_L"] = (s_ >= t_).astype(np.float32)
    c["ac_Df"] = np.maximum(t_ - s_, 0).astype(np.float32)
    c["ac_Db"] = np.maximum(s_ - t_, 0).astype(np.float32)
    c["ac_NEGf"] = ((s_ <= t_).astype(np.float32) - 1.0) * 30000.0
    c["ac_NEGb"] = ((s_ >= t_).astype(np.float32) - 1.0) * 30000.0
    io = np.zeros((128, 256), np.float32)
    io[:, 0:128] = (i + 1)[None, :]
    io[:, 128:256] = (128 - i)[None, :]
    c["ac_io"] = io
    c["ac_ioc"] = np.stack([127 - i, i], axis=1).astype(np.float32)
    J = np.zeros((128, 128), np.float32)
    J[0:64, 0:64] = 1.0 / 64
    J[64:128, 64:128] = 1.0 / 64
    c["ac_J"] = J.astype(ml_dtypes.bfloat16)
    return c
```

```python
import contextlib
import math
import types
import numpy as np
import ml_dtypes
import concourse.bass as bass
import concourse.mybir as mybir
from concourse.bass_utils import run_bass_kernel_spmd

F32 = mybir.dt.float32
BF16 = mybir.dt.bfloat16
AF = mybir.ActivationFunctionType
ALU = mybir.AluOpType
AX = mybir.AxisListType

NCORES = 8
D = 1024
KC = 8
LAT = 2048
CTX = 256
TAU = CTX + LAT
NB = 2
DEPTH = 2
N_IN = 6800
D_FF = 2816
FC = 22
NEXP = 8
EPS = 1e-6
HY_W = 384
OFF_HY = 0
OFF_RQ, OFF_RK, OFF_RV, OFF_RG = 1152, 1536, 1920, 2304
OFF_MQ, OFF_MK, OFF_MV, OFF_MO = 2688, 2944, 3200, 3456
OFF_MG = 3712
OFF_GATE = 3728

NDMASEM = 8


def _freeze(fn, depth=0):
    if not isinstance(fn, types.FunctionType) or fn.__closure__ is None or depth > 3:
        return fn
    cells = []
    for c in fn.__closure__:
        try:
            v = c.cell_contents
        except ValueError:
            cells.append(c)
            continue
        if isinstance(v, types.FunctionType):
            v = _freeze(v, depth + 1)
        cells.append(types.CellType(v))
    g = types.FunctionType(fn.__code__, fn.__globals__, fn.__name__, fn.__defaults__, tuple(cells))
    g.__kwdefaults__ = fn.__kwdefaults__
    return g


class Op:
    __slots__ = ("eng", "fn", "deps", "dma", "needs_inc", "semval", "slot", "prev_slot_op")

    def __init__(self, eng, fn, dma):
        self.eng = eng
        self.fn = fn
        self.deps = []
        self.dma = dma
        self.needs_inc = False
        self.semval = None
        self.slot = None
        self.prev_slot_op = None


class Prog:
    ENGS = ("pe", "act", "dve", "pool", "sp")

    def __init__(self, nc):
        self.nc = nc
        self.ops = {e: [] for e in self.ENGS}
        self.last_w = {}
        self.readers = {}
        self.dma_count = {e: 0 for e in self.ENGS}
        self.dma_slot_last = {}
        self.last_op = {}
        self.nops = 0

    def begin_capture(self):
        self._cap = []

    def end_capture(self):
        c, self._cap = self._cap, None
        return c

    def replay(self, *lists):
        total = sum(len(l) for l in lists)
        pos = [0] * len(lists)
        for _ in range(total):
            best, bf_ = None, None
            for i, l in enumerate(lists):
                if pos[i] < len(l):
                    f = pos[i] / len(l)
                    if bf_ is None or f < bf_:
                        best, bf_ = i, f
            eng, fn, reads, writes, dma = lists[best][pos[best]]
            pos[best] += 1
            self.op(eng, fn, reads, writes, dma)

    def op(self, eng, fn, reads=(), writes=(), dma=False):
        if getattr(self, "_cap", None) is not None:
            self._cap.append((eng, _freeze(fn), list(reads), list(writes), dma))
            return None
        o = Op(eng, _freeze(fn), dma)
        self.nops += 1
        deps = set()
        for k in reads:
            w = self.last_w.get(k)
            if w is not None:
                deps.add(w)
        for k in writes:
            w = self.last_w.get(k)
            if w is not None:
                deps.add(w)
            for r in self.readers.get(k, ()):
                deps.add(r)
        o.deps = list(deps)
        for d in o.deps:
            d.needs_inc = True
        for k in reads:
            self.readers.setdefault(k, []).append(o)
        for k in writes:
            self.last_w[k] = o
            self.readers[k] = []
        if dma:
            j = self.dma_count[eng]
            self.dma_count[eng] = j + 1
            o.slot = (eng, j % NDMASEM)
            o.prev_slot_op = self.dma_slot_last.get(o.slot)
            self.dma_slot_last[o.slot] = o
            o.needs_inc = True
        else:
            self.last_op[eng] = o
        self.ops[eng].append(o)
        return o

    def dma(self, out, in_, reads, writes, eng="sp", **kw):
        return self.op(eng, lambda e: e.dma_start(out=out, in_=in_, **kw), reads, writes, dma=True)

    def barrier(self):
        deps = list(self.last_op.values()) + list(self.dma_slot_last.values())
        for d in deps:
            d.needs_inc = True
        for e in self.ENGS:
            o = Op(e, None, False)
            o.deps = list(deps)
            self.ops[e].append(o)
        self.last_w = {}
        self.readers = {}

    def emit(self, final_ops):
        nc = self.nc
        with contextlib.ExitStack() as st:
            csem = {e: st.enter_context(nc.semaphore("cs_" + e)) for e in ("pe", "act", "dve", "pool")}
            dsem = {}
            for e in self.ENGS:
                if self.dma_count[e] > 0:
                    for s in range(NDMASEM):
                        dsem[(e, s)] = st.enter_context(nc.semaphore("ds_%s_%d" % (e, s)))
            for e in self.ENGS:
                cnt = 0
                dcnt = {}
                for o in self.ops[e]:
                    if o.dma:
                        dcnt[o.slot] = dcnt.get(o.slot, 0) + 16
                        o.semval = dcnt[o.slot]
                    elif o.needs_inc and o.fn is not None:
                        cnt += 1
                        o.semval = cnt
            block = st.enter_context(nc.Block())
            engobj = {"pe": block.tensor, "act": block.scalar, "dve": block.vector,
                      "pool": block.gpsimd, "sp": block.sync}

            def make(e):
                ops = self.ops[e]

                def body(eng):
                    waited = {}

                    def need(semkey, sem, val):
                        if val is None or waited.get(semkey, 0) >= val:
                            return
                        waited[semkey] = val
                        eng.wait_ge(sem, val)

                    for o in ops:
                        for d in o.deps:
                            if d.dma:
                                need(d.slot, dsem[d.slot], d.semval)
                            else:
                                need(d.eng, csem[d.eng], d.semval)
                        if o.dma and o.prev_slot_op is not None:
                            p = o.prev_slot_op
                            need(p.slot, dsem[p.slot], p.semval)
                        if o.fn is None:
                            continue
                        ins = o.fn(eng)
                        if isinstance(ins, (list, tuple)):
                            ins = ins[-1]
                        if o.dma:
                            ins.then_inc(dsem[o.slot], 16)
                        elif o.needs_inc:
                            ins.then_inc(csem[e], 1)
                    if e == "sp":
                        for o in final_ops:
                            need(o.slot, dsem[o.slot], o.semval)
                return body

            for e in self.ENGS:
                engobj[e](make(e))


F_CHUNKS = []
for j in range(9):
    F_CHUNKS.append(("uhy", j, OFF_HY + 128 * j, "id"))
for j in range(3):
    F_CHUNKS.append(("rq", j, OFF_RQ + 128 * j, "id"))
for j in range(3):
    F_CHUNKS.append(("rk", j, OFF_RK + 128 * j, "id"))
for j in range(2):
    F_CHUNKS.append(("mq", j, OFF_MQ + 128 * j, "id"))
for j in range(2):
    F_CHUNKS.append(("mk", j, OFF_MK + 128 * j, "id"))
for j in range(3):
    F_CHUNKS.append(("rg", j, OFF_RG + 128 * j, "silu"))
for j in range(2):
    F_CHUNKS.append(("mo", j, OFF_MO + 128 * j, "sig"))
for j in range(24):
    F_CHUNKS.append(("gate", j, OFF_GATE + 128 * j, "sig"))
SEG_NCH = {"uhy": 9, "rq": 3, "rk": 3, "mq": 2, "mk": 2, "rg": 3, "mo": 2, "gate": 24}


class Builder:
    def __init__(self, dbg=(), stop_after=None):
        self.nc = bass.Bass("TRN2", target_bir_lowering=False)
        self.P = Prog(self.nc)
        self.dbg = set(dbg)
        self.stop_after = stop_after
        self.dram = {}
        self.final_ops = []

    def din(self, name, shape, dt=F32):
        t = self.nc.dram_tensor(name, list(shape), dt, kind="ExternalInput").ap()
        self.dram[name] = t
        return t

    def dscr(self, name, shape, dt):
        kind = "ExternalOutput" if name in self.dbg else "Internal"
        t = self.nc.dram_tensor(name, list(shape), dt, kind=kind).ap()
        self.dram[name] = t
        return t

    def dout(self, name, shape, dt=F32):
        t = self.nc.dram_tensor(name, list(shape), dt, kind="ExternalOutput").ap()
        self.dram[name] = t
        return t

    def build(self):
        nc, P = self.nc, self.P
        din, dscr = self.din, self.dscr
        self.xin = din("xin", [NB, KC, 128, TAU])
        self.sT = din("sT", [128, KC, 3])
        self.w_mod = din("w_mod", [DEPTH, 128, KC, 6 * D])
        self.b_modT = din("b_modT", [DEPTH, 128, 48])
        self.g_mixT = din("g_mixT", [DEPTH, 128, KC])
        self.g_ffnT = din("g_ffnT", [DEPTH, 128, KC])
        self.g_finT = din("g_finT", [128, KC])
        self.w_in = din("w_in", [DEPTH, 128, KC, N_IN])
        self.b_inF = din("b_inF", [DEPTH, 128, len(F_CHUNKS)])
        self.b_in = din("b_in", [DEPTH, N_IN])
        self.ident_d = din("ident", [128, 128])
        self.hy_f1 = din("hy_f1", [DEPTH, 33, 64])
        self.hy_f2 = din("hy_f2", [DEPTH, 64, 64])
        self.hy_f3 = din("hy_f3", [DEPTH, 64, 1536])
        self.hy_fb1 = din("hy_fb1", [DEPTH, 64, 1])
        self.hy_fb2 = din("hy_fb2", [DEPTH, 64, 1])
        self.hy_decay = din("hy_decay", [DEPTH, 1536])
        self.hy_convT = din("hy_convT", [DEPTH, 128, 9, 3])
        self.hy_skipT = din("hy_skipT", [DEPTH, 128, 2, 3])
        self.rope_cos = din("rope_cos", [128, LAT])
        self.rope_sin = din("rope_sin", [128, LAT])
        self.rope_RT = din("rope_RT", [128, 128], BF16)
        self.ml_convT = din("ml_convT", [DEPTH, 128, 4, 3])
        self.w_up = din("w_up", [DEPTH, D, D])
        self.w_out = din("w_out", [DEPTH, D, D])
        self.ffn_w1 = din("ffn_w1", [1, D, D_FF])
        self.ffn_w3 = din("ffn_w3", [1, D, D_FF])
        self.ffn_w2 = din("ffn_w2", [1, D_FF, D])
        self.outT = self.dout("outT", [NB, KC, 128, LAT])
        self.moe_router = din("moe_router", [1, D, NEXP])
        self.moe_router_b = din("moe_router_b", [1, NEXP])
        self.moe_w1 = din("moe_w1", [1, NEXP, D, D_FF])
        self.moe_w3 = din("moe_w3", [1, NEXP, D, D_FF])
        self.moe_w2 = din("moe_w2", [1, NEXP, D_FF, D])
        self.moe_sel = din("moe_sel", [NEXP, NEXP, 128])
        self.ret_decay = din("ret_decay", [DEPTH, 2, 6])
        self.ret_decay_h = din("ret_decay_h", [DEPTH, 2, 2, 3])
        self.ml_gate_bias = din("ml_gate_bias", [DEPTH, 16])
        self.attc = {}
        for nm, shp, dt in (("U", [128, 128], F32), ("L", [128, 128], F32), ("Df", [128, 128], F32), ("Db", [128, 128], F32),
                            ("NEGf", [128, 128], F32), ("NEGb", [128, 128], F32), ("io", [128, 256], F32), ("ioc", [128, 2], F32),
                            ("J", [128, 128], BF16)):
            self.attc[nm] = din("ac_" + nm, shp, dt)
        self.hyc = {}
        for tag, L in (("l", LAT), ("c", CTX)):
            kch = L // 128
            self.hyc[tag] = {
                "featsT": din("hc_featsT_" + tag, [33, L]),
                "tcol": din("hc_tcol_" + tag, [128, kch]),
                "fC": din("hc_fC_" + tag, [kch, 128, kch, 128], BF16),
                "fS": din("hc_fS_" + tag, [kch, 128, kch, 128], BF16),
                "iC": din("hc_iC_" + tag, [L // min(512, L), 128, kch, min(512, L)], BF16),
                "iS": din("hc_iS_" + tag, [L // min(512, L), 128, kch, min(512, L)], BF16),
            }
        self.seg = {}
        for s, n in SEG_NCH.items():
            self.seg[s] = dscr("s_" + s, [NB, n, 128, TAU], BF16)
        self.s_rv = dscr("s_rv", [NB, TAU // 128, 128, 384], BF16)
        self.s_mv = dscr("s_mv", [NB, TAU // 128, 128, 256], BF16)
        self.s_mg = dscr("s_mg", [NB, TAU // 128, 128, 16], F32)
        self.s_mod = dscr("s_mod", [DEPTH, 128, 48, 3], F32)
        self.s_hyc = dscr("s_hyc", [NB, 9, 128, TAU], BF16)
        self.s_ktok = dscr("s_ktok", [NB, TAU // 128, 128, 640], BF16)
        self.s_hffn = dscr("s_hffn", [NB, KC, 128, LAT], BF16)
        self.s_comb = dscr("s_comb", [NEXP, NB * LAT], F32)
        self.s_yatt = dscr("s_yatt", [NB, 5, 128, TAU], BF16)
        self.s_z1 = dscr("s_z1", [NB, 3, 128, TAU], BF16)
        self.seg_yhy = dscr("s_yhy", [NB, 3, 128, TAU], BF16)
        self.s_ztok = {"l": dscr("s_ztok_l", [2, LAT // 128, 128, 768], BF16),
                       "c": dscr("s_ztok_c", [2, CTX // 128, 128, 768], BF16)}
        self.s_fs = {"l": dscr("s_fs_l", [2, 2, LAT // 128, 128, 384], BF16),
                     "c": dscr("s_fs_c", [2, 2, CTX // 128, 128, 384], BF16)}
        self.xa = dscr("xa", [NB, KC, 128, TAU], F32)
        self.xb = dscr("xb", [NB, KC, 128, TAU], F32)

        with contextlib.ExitStack() as st:
            self.gst = st
            self.bank = [st.enter_context(nc.psum_tensor("bank%d" % i, [128, 512], F32)) for i in range(8)]
            if self.stop_after == "moe_only":
                xa_in = self.din("xa_in", [NB, KC, 128, TAU])
                self.s_mod = self.din("smod_in", [DEPTH, 128, 48, 3])
                self.phase_E_moe(1, xa_in, self.xb)
            else:
                self.phase_mod()
            if self.stop_after not in ("mod", "moe_only"):
                for layer in range(DEPTH):
                    xsrc = self.xin if layer == 0 else self.xb
                    self.phase_A(layer, xsrc)
                    if self.stop_after == "A%d" % layer:
                        break
                    if "skipB" not in self.dbg:
                        self.phase_B(layer)
                    if self.stop_after == "B%d" % layer:
                        break
                    self.phase_C1(layer)
                    if self.stop_after == "C1%d" % layer:
                        break
                    self.phase_C2(layer)
                    if self.stop_after == "C2%d" % layer:
                        break
                    self.phase_D(layer, xsrc, self.xa)
                    if self.stop_after == "D%d" % layer:
                        break
                    if layer % 2 == 0:
                        self.phase_E_dense(layer, self.xa, self.xb)
                    else:
                        self.phase_E_moe(layer, self.xa, self.xb)
                    if self.stop_after == "E%d" % layer:
                        break
                else:
                    self.phase_final(self.xb)
            P.barrier()
            P.emit(self.final_ops)
        return nc

    def pk(self, i):
        return "bank%d" % i

    def mk_sb(self, st):
        self.uid = getattr(self, "uid", 0) + 1
        uid = self.uid
        return lambda name, shape, dt: st.enter_context(self.nc.sbuf_tensor("%s_u%d" % (name, uid), shape, dt))

    def phase_mod(self):
        nc, P = self.nc, self.P
        with contextlib.ExitStack() as st:
            sb = self.mk_sb(st)
            sT = sb("m_sT", [128, KC, 3], F32)
            sS = sb("m_sS", [128, KC, 3], F32)
            wblk = [sb("m_w%d" % i, [128, KC, 512], F32) for i in range(2)]
            bm = sb("m_b", [128, 48], F32)
            res = sb("m_res", [128, 48, 3], F32)
            P.dma(sT[:], self.sT[:, :, :], [], ["m_sT"])
            P.op("act", lambda e: e.activation(out=sS[:], in_=sT[:], func=AF.Silu), ["m_sT"], ["m_sS"])
            for layer in range(DEPTH):
                P.dma(bm[:], self.b_modT[layer], [], ["m_b"])
                for blk in range(12):
                    wb = wblk[blk % 2]
                    wk = "m_w%d" % (blk % 2)
                    P.dma(wb[:], self.w_mod[layer, :, :, blk * 512:(blk + 1) * 512], [], [wk],
                          eng=("sp" if blk % 2 == 0 else "pool"))
                    for m in range(4):
                        col = blk * 4 + m
                        bk = self.pk(col % 2)
                        ps = self.bank[col % 2]

                        def fn(e, wb=wb, m=m, ps=ps):
                            r = []
                            for k in range(KC):
                                r.append(e.matmul(ps[:, 0:3], wb[:, k, m * 128:(m + 1) * 128], sS[:, k, :],
                                                  start=(k == 0), stop=(k == KC - 1)))
                            return r
                        P.op("pe", fn, [wk, "m_sS"], [bk])
                        P.op("dve", lambda e, col=col, ps=ps: e.tensor_scalar(
                            out=res[:, col, :], in0=ps[:, 0:3], scalar1=bm[:, col:col + 1], scalar2=None,
                            op0=ALU.add), ["m_b"], [bk, ("m_res", col)])
                P.dma(self.s_mod[layer], res[:], [("m_res", c) for c in range(48)], [("s_mod", layer)])
            P.barrier()

    def phase_A(self, layer, xsrc):
        nc, P = self.nc, self.P
        last = layer == DEPTH - 1
        with contextlib.ExitStack() as st:
            sb = self.mk_sb(st)
            W = sb("a_w", [128, KC, N_IN], BF16)
            modt = sb("a_mod", [128, 48, 3], F32)
            gm = sb("a_g", [128, KC], F32)
            Am = sb("a_A", [128, KC, 3], F32)
            binF = sb("a_binF", [128, len(F_CHUNKS)], F32)
            bT = sb("a_bT", [128, 656], F32)
            ones = sb("a_ones", [128, 128], BF16)
            xg = [sb("a_xg%d" % i, [128, KC, 512], F32) for i in range(2)]
            sq = sb("a_sq", [128, KC, 512], BF16)
            rstd = sb("a_rstd", [128, 512], F32)
            t1 = sb("a_t1", [128, KC, 512], F32)
            hT = [sb("a_hT%d" % i, [128, KC, 512], BF16) for i in range(2)]
            stg = [sb("a_stg%d" % i, [128, 6, 512], BF16) for i in range(3)]
            stT = [sb("a_stT%d" % i, [128, 640], BF16) for i in range(2)]
            stG = [sb("a_stG%d" % i, [128, 16], F32) for i in range(2)]

            WB = 1024
            nwb = (N_IN + WB - 1) // WB
            for q in range(nwb):
                c0, c1 = q * WB, min(N_IN, (q + 1) * WB)
                P.dma(W[:, :, c0:c1], self.w_in[layer, :, :, c0:c1], [], [("a_w", q)], eng="pool")

            def wk_(c0, c1):
                return [("a_w", q) for q in range(c0 // WB, (c1 - 1) // WB + 1)]
            P.dma(modt[:], self.s_mod[layer], [("s_mod", layer)], ["a_mod"])
            P.dma(gm[:], self.g_mixT[layer], [], ["a_g"])
            P.dma(binF[:], self.b_inF[layer], [], ["a_binF"])
            P.dma(bT[:, 0:384], self.b_in[layer:layer + 1, OFF_RV:OFF_RV + 384].to_broadcast([128, 384]), [], ["a_bT"])
            P.dma(bT[:, 384:640], self.b_in[layer:layer + 1, OFF_MV:OFF_MV + 256].to_broadcast([128, 256]), [], ["a_bT"])
            P.dma(bT[:, 640:656], self.b_in[layer:layer + 1, OFF_MG:OFF_MG + 16].to_broadcast([128, 16]), [], ["a_bT"])
            P.op("pool", lambda e: e.memset(ones[:], 1.0 / D), [], ["a_ones"])
            P.op("dve", lambda e: e.tensor_scalar(out=Am[:], in0=modt[:, 8:16, :], scalar1=1.0, scalar2=None,
                                                   op0=ALU.add), ["a_mod"], ["a_A"])
            for s in range(3):
                P.op("dve", lambda e, s=s: e.tensor_tensor(out=Am[:, :, s], in0=Am[:, :, s], in1=gm[:], op=ALU.mult),
                     ["a_A", "a_g"], ["a_A"])

            groups = []
            for b in range(NB):
                groups.append((b, 2, 0, CTX))
                for g in range(4):
                    groups.append((b, b, CTX + 512 * g, 512))
            nstg = 0

            def load_xA(gi):
                b, s, t0, n = groups[gi]
                P.dma(xg[gi % 2][:, :, 0:n], xsrc[b, :, :, t0:t0 + n].rearrange("c p t -> p c t"), [("x", layer)], ["a_xg%d" % (gi % 2)])

            for gi, (b, s, t0, n) in enumerate(groups):
                is_ctx = t0 < CTX
                xt = xg[gi % 2]
                xk = "a_xg%d" % (gi % 2)
                h = hT[gi % 2]
                hk = "a_hT%d" % (gi % 2)
                hks = [(hk, k) for k in range(KC)]
                if gi == 0:
                    load_xA(0)
                if gi + 1 < len(groups):
                    load_xA(gi + 1)
                P.op("act", lambda e, xt=xt, n=n: e.activation(out=sq[:, :, 0:n], in_=xt[:, :, 0:n], func=AF.Square),
                     [xk], ["a_sq"])
                ps = self.bank[0]

                def fss(e, ps=ps, n=n):
                    return [e.matmul(ps[:, 0:n], ones[:], sq[:, k, 0:n], start=(k == 0), stop=(k == KC - 1))
                            for k in range(KC)]
                P.op("pe", fss, ["a_sq", "a_ones"], [self.pk(0)])
                P.op("dve", lambda e, ps=ps, n=n: e.tensor_scalar(out=rstd[:, 0:n], in0=ps[:, 0:n], scalar1=EPS,
                                                                    scalar2=None, op0=ALU.add),
                     [], [self.pk(0), "a_rstd"])
                P.op("act", lambda e, n=n: e.activation(out=rstd[:, 0:n], in_=rstd[:, 0:n], func=AF.Sqrt),
                     [], ["a_rstd"])
                P.op("dve", lambda e, n=n: e.reciprocal(out=rstd[:, 0:n], in_=rstd[:, 0:n]), [], ["a_rstd"])
                for k in range(KC):
                    P.op("dve", lambda e, k=k, xt=xt, n=n: e.tensor_tensor(out=t1[:, k, 0:n], in0=xt[:, k, 0:n],
                                                                            in1=rstd[:, 0:n], op=ALU.mult),
                         [xk, "a_rstd"], [("a_t1", k)])
                    P.op("act", lambda e, k=k, h=h, n=n, s=s: e.activation(
                        out=h[:, k, 0:n], in_=t1[:, k, 0:n], func=AF.Identity,
                        scale=Am[:, k, s:s + 1], bias=modt[:, k, s:s + 1]),
                         [("a_t1", k), "a_A", "a_mod"], [(hk, k)])
                cur_seg = None
                for ci, (sname, j, col0, actf) in enumerate(F_CHUNKS):
                    if last and is_ctx and sname in ("uhy", "gate", "rg", "mo"):
                        continue
                    bi = 1 + (ci % 4)
                    ps = self.bank[bi]

                    def fmm(e, ps=ps, col0=col0, h=h, n=n):
                        return [e.matmul(ps[:, 0:n], W[:, k, col0:col0 + 128], h[:, k, 0:n],
                                         start=(k == 0), stop=(k == KC - 1)) for k in range(KC)]
                    P.op("pe", fmm, hks + wk_(col0, col0 + 128), [self.pk(bi)])
                    slot = j % 6
                    if slot == 0:
                        nstg += 1
                    sg = stg[nstg % 3]
                    sgk = "a_stg%d" % (nstg % 3)
                    if actf == "id":
                        P.op("dve", lambda e, sg=sg, slot=slot, ps=ps, ci=ci, n=n: e.tensor_scalar(
                            out=sg[:, slot, 0:n], in0=ps[:, 0:n], scalar1=binF[:, ci:ci + 1], scalar2=None,
                            op0=ALU.add), ["a_binF"], [self.pk(bi), (sgk, slot)])
                    else:
                        fn_ = AF.Silu if actf == "silu" else AF.Sigmoid
                        P.op("act", lambda e, sg=sg, slot=slot, ps=ps, ci=ci, n=n, fn_=fn_: e.activation(
                            out=sg[:, slot, 0:n], in_=ps[:, 0:n], func=fn_, bias=binF[:, ci:ci + 1], scale=1.0),
                             ["a_binF"], [self.pk(bi), (sgk, slot)])
                    nseg = SEG_NCH[sname]
                    if slot == 5 or j == nseg - 1:
                        j0 = j - slot
                        P.dma(self.seg[sname][b, j0:j + 1, :, t0:t0 + n].rearrange("c p t -> p c t"),
                              sg[:, 0:slot + 1, 0:n], [(sgk, q) for q in range(slot + 1)], [("seg", sname)])
                for tt in range(n // 128):
                    ti = (t0 + tt * 128) // 128
                    pv, pg = self.bank[5 + (tt % 2)], self.bank[7]
                    pvk, pgk = self.pk(5 + (tt % 2)), self.pk(7)
                    sT_ = stT[tt % 2]
                    sTk = "a_stT%d" % (tt % 2)
                    sG_ = stG[tt % 2]
                    sGk = "a_stG%d" % (tt % 2)

                    def fv(e, pv=pv, h=h, tt=tt):
                        return [e.matmul(pv[:, 0:384], h[:, k, tt * 128:(tt + 1) * 128], W[:, k, OFF_RV:OFF_RV + 384],
                                         start=(k == 0), stop=(k == KC - 1)) for k in range(KC)]
                    P.op("pe", fv, hks + wk_(OFF_RV, OFF_RV + 384), [pvk])
                    P.op("dve", lambda e, pv=pv, sT_=sT_: e.tensor_tensor(out=sT_[:, 0:384], in0=pv[:, 0:384],
                                                                          in1=bT[:, 0:384], op=ALU.add),
                         ["a_bT"], [pvk, (sTk, 0)])

                    def fm(e, pg=pg, h=h, tt=tt):
                        r = [e.matmul(pg[:, 0:256], h[:, k, tt * 128:(tt + 1) * 128], W[:, k, OFF_MV:OFF_MV + 256],
                                      start=(k == 0), stop=(k == KC - 1)) for k in range(KC)]
                        r += [e.matmul(pg[:, 256:272], h[:, k, tt * 128:(tt + 1) * 128], W[:, k, OFF_MG:OFF_MG + 16],
                                       start=(k == 0), stop=(k == KC - 1)) for k in range(KC)]
                        return r
                    P.op("pe", fm, hks + wk_(OFF_MV, OFF_MG + 16), [pgk])
                    P.op("dve", lambda e, pg=pg, sT_=sT_: e.tensor_tensor(out=sT_[:, 384:640], in0=pg[:, 0:256],
                                                                          in1=bT[:, 384:640], op=ALU.add),
                         ["a_bT"], [pgk, (sTk, 1)])
                    P.op("dve", lambda e, pg=pg, sG_=sG_: e.tensor_tensor(out=sG_[:, :], in0=pg[:, 256:272],
                                                                          in1=bT[:, 640:656], op=ALU.add),
                         ["a_bT"], [pgk, sGk])
                    P.dma(self.s_rv[b, ti], sT_[:, 0:384], [(sTk, 0)], [("seg", "rv")])
                    P.dma(self.s_mv[b, ti], sT_[:, 384:640], [(sTk, 1)], [("seg", "mv")])
                    P.dma(self.s_mg[b, ti], sG_[:, :], [sGk], [("seg", "mg")])
            P.barrier()


    def bank_bf(self, i):
        return self.bank[i][:].bitcast(BF16)

    def phase_B(self, layer):
        last = layer == DEPTH - 1
        for (L, t0, tag) in ((LAT, CTX, "l"), (CTX, 0, "c")):
            if tag == "c" and last:
                continue
            self.hy_filters(layer, L, tag)
            self.hy_shortconv(layer, L, t0, tag)
            self.hy_pass(layer, L, t0, tag, 0)
            self.hy_pass(layer, L, t0, tag, 1)

    def hy_filters(self, layer, L, tag):
        nc, P = self.nc, self.P
        kch = L // 128
        cst = self.hyc[tag]
        with contextlib.ExitStack() as st:
            sb = self.mk_sb(st)
            featsT = sb("f_feats", [33, L], F32)
            f1 = sb("f_f1", [33, 64], F32)
            f2 = sb("f_f2", [64, 64], F32)
            f3 = sb("f_f3", [64, 1536], F32)
            b1 = sb("f_b1", [64, 3], F32)
            b2 = sb("f_b2", [64, 3], F32)
            absd = sb("f_absd", [128, 1536], F32)
            negd = sb("f_negd", [128, 1536], F32)
            tcol = sb("f_tcol", [128, kch], F32)
            hid1 = sb("f_hid1", [64, 512], F32)
            hid2 = sb("f_hid2", [64, 512], F32)
            sa = sb("f_sa", [64, 512], F32)
            sq_ = sb("f_sq", [64, 512], F32)
            win = sb("f_win", [128, 1536], F32)
            hf = sb("f_hf", [128, kch, 1536], BF16)
            fcm = [sb("f_fc%d" % i, [128, kch, 128], BF16) for i in range(2)]
            fsm = [sb("f_fs%d" % i, [128, kch, 128], BF16) for i in range(2)]
            hsd = sb("f_hsd", [128, kch, 2, 2, 384], BF16)
            outt = [sb("f_out%d" % i, [128, 2, 2, 384], BF16) for i in range(2)]
            P.dma(featsT[:], cst["featsT"][:, :], [], ["f_feats"])
            P.dma(f1[:], self.hy_f1[layer], [], ["f_f1"])
            P.dma(f2[:], self.hy_f2[layer], [], ["f_f2"])
            P.dma(f3[:], self.hy_f3[layer], [], ["f_f3"])
            P.dma(b1[:, 0:1], self.hy_fb1[layer], [], ["f_b1"])
            P.dma(b2[:, 0:1], self.hy_fb2[layer], [], ["f_b2"])
            P.dma(absd[:], self.hy_decay[layer:layer + 1, :].to_broadcast([128, 1536]), [], ["f_absd"])
            P.dma(tcol[:], cst["tcol"][:, :], [], ["f_tcol"])
            for bb, bk in ((b1, "f_b1"), (b2, "f_b2")):
                P.op("dve", lambda e, bb=bb: e.tensor_scalar(out=bb[:, 1:2], in0=bb[:, 0:1], scalar1=0.5, scalar2=None,
                                                            op0=ALU.mult), [bk], [bk])
                P.op("dve", lambda e, bb=bb: e.tensor_scalar(out=bb[:, 2:3], in0=bb[:, 0:1], scalar1=0.25, scalar2=None,
                                                            op0=ALU.mult), [bk], [bk])
            P.op("dve", lambda e: e.tensor_scalar(out=negd[:], in0=absd[:], scalar1=-1.0, scalar2=None, op0=ALU.mult),
                 ["f_absd"], ["f_negd"])
            P.op("dve", lambda e: e.tensor_tensor(out=absd[:], in0=absd[:], in1=negd[:], op=ALU.max),
                 ["f_negd"], ["f_absd"])

            def sin_layer(ps, bb, bk, out, outk, n):
                P.op("act", lambda e: e.activation(out=sa[:, 0:n], in_=ps[0:64, 0:n], func=AF.Sin, bias=bb[:, 1:2], scale=0.5),
                     [bk], [self.pk(0), "f_sa"])
                P.op("act", lambda e: e.activation(out=sq_[:, 0:n], in_=ps[0:64, 0:n], func=AF.Sin, bias=bb[:, 2:3], scale=0.25),
                     [bk], [self.pk(0), "f_sq"])
                P.op("dve", lambda e: e.tensor_tensor(out=sq_[:, 0:n], in0=sq_[:, 0:n], in1=sq_[:, 0:n], op=ALU.mult),
                     [], ["f_sq"])
                P.op("dve", lambda e: e.tensor_scalar(out=sq_[:, 0:n], in0=sq_[:, 0:n], scalar1=-4.0, scalar2=2.0,
                                                       op0=ALU.mult, op1=ALU.add), [], ["f_sq"])
                P.op("dve", lambda e: e.tensor_tensor(out=out[:, 0:n], in0=sa[:, 0:n], in1=sq_[:, 0:n], op=ALU.mult),
                     ["f_sa", "f_sq"], [outk])

            nblk = max(L // 512, 1)
            bn = min(L, 512)
            for blk in range(nblk):
                ps = self.bank[0]
                P.op("pe", lambda e, blk=blk, ps=ps: e.matmul(ps[0:64, 0:bn], f1[:, :], featsT[:, blk * bn:(blk + 1) * bn],
                                                              start=True, stop=True), ["f_f1", "f_feats"], [self.pk(0)])
                sin_layer(ps, b1, "f_b1", hid1, "f_hid1", bn)
                P.op("pe", lambda e, ps=ps: e.matmul(ps[0:64, 0:bn], f2[:, :], hid1[:, 0:bn], start=True, stop=True),
                     ["f_f2", "f_hid1"], [self.pk(0)])
                sin_layer(ps, b2, "f_b2", hid2, "f_hid2", bn)
                for tt in range(bn // 128):
                    ch = blk * (bn // 128) + tt
                    P.op("act", lambda e, ch=ch: e.activation(out=win[:], in_=absd[:], func=AF.Exp, scale=tcol[:, ch:ch + 1]),
                         ["f_absd", "f_tcol"], ["f_win"])
                    for q in range(3):
                        pq = self.bank[1 + q]
                        P.op("pe", lambda e, pq=pq, tt=tt, q=q: e.matmul(pq[:, :], hid2[:, tt * 128:(tt + 1) * 128],
                                                                          f3[:, q * 512:(q + 1) * 512], start=True, stop=True),
                             ["f_hid2", "f_f3"], [self.pk(1 + q)])
                        P.op("dve", lambda e, pq=pq, ch=ch, q=q: e.tensor_tensor(
                            out=hf[:, ch, q * 512:(q + 1) * 512], in0=pq[:, :], in1=win[:, q * 512:(q + 1) * 512], op=ALU.mult),
                             ["f_win"], [self.pk(1 + q), ("f_hf", ch)])
                    if ch == 0:
                        for o in range(2):
                            P.op("pool", lambda e, o=o: e.memset(hf[0:1, 0, o * 768 + 384:o * 768 + 768], 0.0),
                                 [], [("f_hf", 0)])
            hfk = [("f_hf", c) for c in range(kch)]
            for ch in range(kch):
                hv = hf[:, ch, :].rearrange("p (o d c) -> p o d c", o=2, d=2)
                P.op("dve", lambda e, ch=ch, hv=hv: e.tensor_tensor(out=hsd[:, ch, 0, :, :], in0=hv[:, :, 0, :], in1=hv[:, :, 1, :], op=ALU.add),
                     [("f_hf", ch)], [("f_hsd", ch, 0)])
                P.op("pool", lambda e, ch=ch, hv=hv: e.tensor_tensor(out=hsd[:, ch, 1, :, :], in0=hv[:, :, 0, :], in1=hv[:, :, 1, :], op=ALU.subtract),
                     [("f_hf", ch)], [("f_hsd", ch, 1)])
            hsk = [[("f_hsd", c, pi) for c in range(kch)] for pi in range(2)]
            for kc in range(kch):
                fc_, fs_ = fcm[kc % 2], fsm[kc % 2]
                fck, fsk = "f_fc%d" % (kc % 2), "f_fs%d" % (kc % 2)
                P.dma(fc_[:], cst["fC"][kc], [], [fck])
                P.dma(fs_[:], cst["fS"][kc], [], [fsk])
                ot = outt[kc % 2]
                otk = "f_out%d" % (kc % 2)
                for pi, (mt, mk) in enumerate(((fc_, fck), (fs_, fsk))):
                    for o in range(2):
                        bi = (kc % 2) * 4 + pi * 2 + o
                        pb = self.bank[bi]

                        def fsp(e, pb=pb, mt=mt, o=o, pi=pi):
                            return [e.matmul(pb[:, 0:384], mt[:, nch, :], hsd[:, nch, pi, o, :],
                                             start=(nch == 0), stop=(nch == kch - 1)) for nch in range(kch)]
                        P.op("pe", fsp, hsk[pi] + [mk], [self.pk(bi)])
                        if (pi + o) % 2 == 0:
                            P.op("act", lambda e, pb=pb, ot=ot, o=o, pi=pi: e.activation(out=ot[:, o, pi, :], in_=pb[:, 0:384], func=AF.Copy),
                                 [], [self.pk(bi), (otk, o, pi)])
                        else:
                            P.op("dve", lambda e, pb=pb, ot=ot, o=o, pi=pi: e.tensor_copy(out=ot[:, o, pi, :], in_=pb[:, 0:384]),
                                 [], [self.pk(bi), (otk, o, pi)])
                for o in range(2):
                    P.dma(self.s_fs[tag][o, :, kc].rearrange("r p c -> p r c"), ot[:, o, :, :],
                          [(otk, o, 0), (otk, o, 1)], [("s_fs", tag)])
            P.barrier()

    def hy_shortconv(self, layer, L, t0, tag):
        nc, P = self.nc, self.P
        kch = L // 128
        ztok = self.s_ztok[tag]
        with contextlib.ExitStack() as st:
            sb = self.mk_sb(st)
            cw = sb("c_w", [128, 9, 3], F32)
            ident = sb("c_id", [128, 128], BF16)
            idf = sb("c_idf", [128, 128], F32)
            u = [sb("c_u%d" % i, [128, L], BF16) for i in range(2)]
            acc = [sb("c_acc%d" % i, [128, L], F32) for i in range(2)]
            ob = [sb("c_ob%d" % i, [128, L], BF16) for i in range(2)]
            tk = [sb("c_tk%d" % i, [128, 4, 128], BF16) for i in range(2)]
            P.dma(cw[:], self.hy_convT[layer], [], ["c_w"])
            P.dma(idf[:], self.ident_d[:, :], [], ["c_idf"])
            P.op("dve", lambda e: e.tensor_copy(out=ident[:], in_=idf[:]), ["c_idf"], ["c_id"])
            it = 0

            def load_u(it_):
                b_, j_ = it_ // 9, it_ % 9
                P.dma(u[it_ % 2][:], self.seg["uhy"][b_, j_, :, t0:t0 + L], [], ["c_u%d" % (it_ % 2)])

            load_u(0)
            for b in range(NB):
                for j in range(9):
                    ut, uk = u[it % 2], "c_u%d" % (it % 2)
                    at, ak = acc[it % 2], "c_acc%d" % (it % 2)
                    ot, ok_ = ob[it % 2], "c_ob%d" % (it % 2)
                    it += 1
                    if it < NB * 9:
                        load_u(it)
                    eng = "dve"
                    P.op(eng, lambda e, ut=ut, at=at, j=j: e.tensor_scalar(out=at[:], in0=ut[:], scalar1=cw[:, j, 1:2],
                                                                          scalar2=None, op0=ALU.mult), [uk, "c_w"], [ak])
                    P.op(eng, lambda e, ut=ut, at=at, j=j: e.scalar_tensor_tensor(
                        out=at[:, 1:L], in0=ut[:, 0:L - 1], scalar=cw[:, j, 0:1], in1=at[:, 1:L], op0=ALU.mult, op1=ALU.add),
                         [uk, "c_w"], [ak])
                    P.op(eng, lambda e, ut=ut, at=at, j=j: e.scalar_tensor_tensor(
                        out=at[:, 0:L - 1], in0=ut[:, 1:L], scalar=cw[:, j, 2:3], in1=at[:, 0:L - 1], op0=ALU.mult, op1=ALU.add),
                         [uk, "c_w"], [ak])
                    P.op("act", lambda e, at=at, ot=ot: e.activation(out=ot[:], in_=at[:], func=AF.Copy), [ak], [ok_])
                    P.dma(self.s_hyc[b, j, :, t0:t0 + L], ot[:], [ok_], [("s_hyc", b, j)])
                    if j < 3:
                        for g4 in range(kch // 4 if kch >= 4 else 1):
                            nt = min(4, kch)
                            bi = g4 % 2
                            pbf = self.bank_bf(bi)
                            tkt, tkk = tk[g4 % 2], "c_tk%d" % (g4 % 2)

                            def ftr(e, pbf=pbf, ot=ot, g4=g4, nt=nt):
                                return [e.transpose(pbf[:, q * 128:(q + 1) * 128], ot[:, (g4 * 4 + q) * 128:(g4 * 4 + q + 1) * 128],
                                                    ident[:]) for q in range(nt)]
                            P.op("pe", ftr, [ok_, "c_id"], [self.pk(bi)])
                            P.op("dve", lambda e, pbf=pbf, tkt=tkt, nt=nt: e.tensor_copy(
                                out=tkt[:, 0:nt, :], in_=pbf[:, 0:nt * 128].rearrange("p (q c) -> p q c", c=128)),
                                 [], [self.pk(bi), tkk])
                            P.dma(ztok[0, g4 * 4:g4 * 4 + nt, :, b * 384 + j * 128:b * 384 + (j + 1) * 128].rearrange("q p c -> p q c"),
                                  tkt[:, 0:nt, :], [tkk], [("s_ztok", tag, 0)])
            P.barrier()

    def hy_pass(self, layer, L, t0, tag, o):
        nc, P = self.nc, self.P
        kch = L // 128
        cst = self.hyc[tag]
        ztok = self.s_ztok[tag]
        zin_d = self.s_hyc if o == 0 else self.s_z1
        zout_d = self.s_z1 if o == 0 else self.seg_yhy
        GS = min(512, L)
        ngn = L // GS
        with contextlib.ExitStack() as st:
            sb = self.mk_sb(st)
            zt = sb("p_zt", [128, kch, 768], BF16)
            Fs2 = [sb("p_F%d" % i, [128, 2, 384], BF16) for i in range(2)]
            Y = sb("p_Y", [128, kch, 2, 768], BF16)
            fcm = [sb("p_fc%d" % i, [128, kch, 128], BF16) for i in range(2)]
            fsm = [sb("p_fs%d" % i, [128, kch, 128], BF16) for i in range(2)]
            icm = [sb("p_ic%d" % i, [128, kch, GS], BF16) for i in range(2)]
            ism = [sb("p_is%d" % i, [128, kch, GS], BF16) for i in range(2)]
            tm = [sb("p_tm%d" % i, [128, 384], F32) for i in range(4)]
            skp = sb("p_skip", [128, 2, 3], F32)
            ident = sb("p_id", [128, 128], BF16)
            idf = sb("p_idf", [128, 128], F32)
            zi = [sb("p_zi%d" % i, [128, 6, GS], BF16) for i in range(2)]
            gt = [sb("p_gt%d" % i, [128, 6, GS], BF16) for i in range(2)]
            tf = [sb("p_tf%d" % i, [128, GS], F32) for i in range(2)]
            zo = [sb("p_zo%d" % i, [128, 6, GS], BF16) for i in range(2)]
            tk = [sb("p_tk%d" % i, [128, GS // 128, 128], BF16) for i in range(2)]
            P.dma(zt[:], ztok[o].rearrange("q p c -> p q c"), [], ["p_zt"])
            P.dma(skp[:], self.hy_skipT[layer], [], ["p_skip"])
            P.dma(idf[:], self.ident_d[:, :], [], ["p_idf"])
            P.op("dve", lambda e: e.tensor_copy(out=ident[:], in_=idf[:]), ["p_idf"], ["p_id"])
            for kc in range(kch):
                fc_, fs_ = fcm[kc % 2], fsm[kc % 2]
                fck, fsk = "p_fc%d" % (kc % 2), "p_fs%d" % (kc % 2)
                P.dma(fc_[:], cst["fC"][kc], [], [fck])
                P.dma(fs_[:], cst["fS"][kc], [], [fsk])
                Fst, Fsk = Fs2[kc % 2], "p_F%d" % (kc % 2)
                P.dma(Fst[:], self.s_fs[tag][o, :, kc].rearrange("r p c -> p r c"), [], [Fsk])
                for b in range(NB):
                    br, bi_ = (kc * NB + b) % 4 * 2, (kc * NB + b) % 4 * 2 + 1
                    pr, pi_ = self.bank[br], self.bank[bi_]

                    def ffw(e, pp, mt, b=b):
                        return [e.matmul(pp[:, 0:384], mt[:, nch, :], zt[:, nch, b * 384:(b + 1) * 384],
                                         start=(nch == 0), stop=(nch == kch - 1)) for nch in range(kch)]
                    P.op("pe", lambda e, pr=pr, fc_=fc_, b=b: ffw(e, pr, fc_, b), ["p_zt", fck], [self.pk(br)])
                    P.op("pe", lambda e, pi_=pi_, fs_=fs_, b=b: ffw(e, pi_, fs_, b), ["p_zt", fsk], [self.pk(bi_)])
                    t1, t2, t3, t4 = tm
                    Fr, Fi = Fst[:, 0, :], Fst[:, 1, :]
                    P.op("dve", lambda e, pr=pr, Fr=Fr: e.tensor_tensor(out=t1[:], in0=pr[:, 0:384], in1=Fr, op=ALU.mult),
                         [Fsk], [self.pk(br), "p_tm0"])
                    P.op("dve", lambda e, pi_=pi_, Fi=Fi: e.tensor_tensor(out=t2[:], in0=pi_[:, 0:384], in1=Fi, op=ALU.mult),
                         [Fsk], [self.pk(bi_), "p_tm1"])
                    P.op("dve", lambda e, pr=pr, Fi=Fi: e.tensor_tensor(out=t3[:], in0=pr[:, 0:384], in1=Fi, op=ALU.mult),
                         [Fsk], [self.pk(br), "p_tm2"])
                    P.op("dve", lambda e, pi_=pi_, Fr=Fr: e.tensor_tensor(out=t4[:], in0=pi_[:, 0:384], in1=Fr, op=ALU.mult),
                         [Fsk], [self.pk(bi_), "p_tm3"])
                    P.op("pool", lambda e, kc=kc, b=b: e.tensor_tensor(out=Y[:, kc, 0, b * 384:(b + 1) * 384], in0=t1[:], in1=t2[:],
                                                                      op=ALU.subtract), ["p_tm0", "p_tm1"], [("p_Y", kc)])
                    P.op("pool", lambda e, kc=kc, b=b: e.tensor_tensor(out=Y[:, kc, 1, b * 384:(b + 1) * 384], in0=t3[:], in1=t4[:],
                                                                      op=ALU.add), ["p_tm2", "p_tm3"], [("p_Y", kc)])
            yk = [("p_Y", kc) for kc in range(kch)]
            def load_inv(ng_):
                P.dma(icm[ng_ % 2][:], cst["iC"][ng_], [], ["p_ic%d" % (ng_ % 2)])
                P.dma(ism[ng_ % 2][:], cst["iS"][ng_], [], ["p_is%d" % (ng_ % 2)])
                n0_ = t0 + ng_ * GS
                for b_ in range(NB):
                    P.dma(zi[ng_ % 2][:, b_ * 3:(b_ + 1) * 3, :], zin_d[b_, 0:3, :, n0_:n0_ + GS].rearrange("c p t -> p c t"),
                          [], [("p_zi%d" % (ng_ % 2), b_)], eng="pool")
                    P.dma(gt[ng_ % 2][:, b_ * 3:(b_ + 1) * 3, :],
                          self.s_hyc[b_, 3 + 3 * o:6 + 3 * o, :, n0_:n0_ + GS].rearrange("c p t -> p c t"),
                          [], [("p_gt%d" % (ng_ % 2), b_)], eng="pool")

            load_inv(0)
            for ng in range(ngn):
                ic_, is_ = icm[ng % 2], ism[ng % 2]
                ick, isk = "p_ic%d" % (ng % 2), "p_is%d" % (ng % 2)
                zit, zik = zi[ng % 2], "p_zi%d" % (ng % 2)
                gtt, gtk = gt[ng % 2], "p_gt%d" % (ng % 2)
                zot, zok = zo[ng % 2], "p_zo%d" % (ng % 2)
                n0 = t0 + ng * GS
                if ng + 1 < ngn:
                    load_inv(ng + 1)
                for b in range(NB):
                    for cj in range(3):
                        q = b * 3 + cj
                        bi = q % 4
                        pb = self.bank[bi]

                        def finv(e, pb=pb, q=q, ic_=ic_, is_=is_):
                            r = []
                            for kc in range(kch):
                                r.append(e.matmul(pb[:, 0:GS], Y[:, kc, 0, q * 128:(q + 1) * 128], ic_[:, kc, :],
                                                  start=(kc == 0), stop=False))
                                r.append(e.matmul(pb[:, 0:GS], Y[:, kc, 1, q * 128:(q + 1) * 128], is_[:, kc, :],
                                                  start=False, stop=(kc == kch - 1)))
                            return r
                        P.op("pe", finv, yk + [ick, isk], [self.pk(bi)])
                        tft, tfk = tf[q % 2], "p_tf%d" % (q % 2)
                        P.op("dve", lambda e, pb=pb, tft=tft, zit=zit, q=q, cj=cj: e.scalar_tensor_tensor(
                            out=tft[:], in0=zit[:, q, :], scalar=skp[:, o, cj:cj + 1], in1=pb[:, 0:GS],
                            op0=ALU.mult, op1=ALU.add), [(zik, b), "p_skip"], [self.pk(bi), tfk])
                        P.op("pool", lambda e, tft=tft, zot=zot, gtt=gtt, q=q: e.tensor_tensor(
                            out=zot[:, q, :], in0=tft[:], in1=gtt[:, q, :], op=ALU.mult), [tfk, (gtk, b)], [(zok, q)])
                        if o == 0:
                            bt = 4 + (q % 2)
                            pbf = self.bank_bf(bt)
                            tkt, tkk = tk[q % 2], "p_tk%d" % (q % 2)

                            def ftr(e, pbf=pbf, zot=zot, q=q):
                                return [e.transpose(pbf[:, h * 128:(h + 1) * 128], zot[:, q, h * 128:(h + 1) * 128], ident[:])
                                        for h in range(GS // 128)]
                            P.op("pe", ftr, [(zok, q), "p_id"], [self.pk(bt)])
                            P.op("act", lambda e, pbf=pbf, tkt=tkt: e.activation(
                                out=tkt[:], in_=pbf[:, 0:GS].rearrange("p (h c) -> p h c", c=128), func=AF.Copy),
                                 [], [self.pk(bt), tkk])
                            P.dma(ztok[1, ng * (GS // 128):(ng + 1) * (GS // 128), :, q * 128:(q + 1) * 128].rearrange("h p c -> p h c"),
                                  tkt[:], [tkk], [("s_ztok", tag, 1)])
                    P.dma(zout_d[b, 0:3, :, n0:n0 + GS].rearrange("c p t -> p c t"), zot[:, b * 3:(b + 1) * 3, :],
                          [(zok, b * 3 + cj) for cj in range(3)], [("zout", b)])
            P.barrier()


    def phase_C1(self, layer):
        nc, P = self.nc, self.P
        with contextlib.ExitStack() as st:
            sb = self.mk_sb(st)
            cosT = sb("r_cos", [128, LAT], F32)
            sinT = sb("r_sin", [128, LAT], F32)
            RT = sb("r_RT", [128, 128], BF16)
            ident = sb("r_id", [128, 128], BF16)
            idf = sb("r_idf", [128, 128], F32)
            mcw = sb("r_mcw", [128, 4, 3], F32)
            raw = [sb("r_raw%d" % i, [128, TAU], BF16) for i in range(2)]
            q8 = [sb("r_q8%d" % i, [128, TAU], BF16) for i in range(2)]
            tmp = [sb("r_tmp%d" % i, [128, LAT], F32) for i in range(2)]
            acc = sb("r_acc", [128, TAU], F32)
            outb = [sb("r_out%d" % i, [128, TAU], BF16) for i in range(2)]
            tk = [sb("r_tk%d" % i, [128, 6, 128], BF16) for i in range(2)]
            P.dma(cosT[:], self.rope_cos[:, :], [], ["r_cos"])
            P.dma(sinT[:], self.rope_sin[:, :], [], ["r_sin"])
            P.dma(RT[:], self.rope_RT[:, :], [], ["r_RT"])
            P.dma(mcw[:], self.ml_convT[layer], [], ["r_mcw"])
            P.dma(idf[:], self.ident_d[:, :], [], ["r_idf"])
            P.op("dve", lambda e: e.tensor_copy(out=ident[:], in_=idf[:]), ["r_idf"], ["r_id"])
            it = 0
            ntk = 0
            for b in range(NB):
                items = [("rq", j, "ret", False, None) for j in range(3)] + [("rk", j, "ret", True, j * 128) for j in range(3)] + \
                        [("mq", j, "ml", False, None) for j in range(2)] + [("mk", j, "ml", True, 384 + j * 128) for j in range(2)]
                for ii, (sname, j, kind, is_k, kcol) in enumerate(items):
                    rw, rwk = raw[it % 2], "r_raw%d" % (it % 2)
                    qq, qqk = q8[it % 2], "r_q8%d" % (it % 2)
                    tp, tpk = tmp[it % 2], "r_tmp%d" % (it % 2)
                    ob, obk = outb[it % 2], "r_out%d" % (it % 2)
                    if it == 0:
                        P.dma(rw[:], self.seg[sname][b, j], [("seg", sname, b, j)], [rwk])
                    it += 1
                    nxt = (b, ii + 1) if ii + 1 < len(items) else ((b + 1, 0) if b + 1 < NB else None)
                    if nxt is not None:
                        sn2, j2 = items[nxt[1]][0], items[nxt[1]][1]
                        P.dma(raw[it % 2][:], self.seg[sn2][nxt[0], j2], [("seg", sn2, nxt[0], j2)], ["r_raw%d" % (it % 2)])
                    if kind == "ret":
                        sc = 1.0 if is_k else 0.125
                        P.op("act", lambda e, rw=rw, ob=ob, sc=sc: e.activation(out=ob[:, 0:CTX], in_=rw[:, 0:CTX], func=AF.Copy, scale=sc),
                             [rwk], [(obk, 0)])
                        P.op("act", lambda e, rw=rw, qq=qq, sc=sc: e.activation(out=qq[:, CTX:TAU], in_=rw[:, CTX:TAU], func=AF.Copy, scale=sc),
                             [rwk], [qqk])
                        for g in range(4):
                            pb = self.bank[g]
                            P.op("pe", lambda e, pb=pb, qq=qq, g=g: e.matmul(pb[:, :], RT[:, :], qq[:, CTX + g * 512:CTX + (g + 1) * 512],
                                                                            start=True, stop=True), [qqk, "r_RT"], [self.pk(g)])
                            P.op("dve", lambda e, pb=pb, tp=tp, g=g: e.tensor_tensor(out=tp[:, g * 512:(g + 1) * 512], in0=pb[:, :],
                                                                                    in1=sinT[:, g * 512:(g + 1) * 512], op=ALU.mult),
                                 ["r_sin"], [self.pk(g), (tpk, g)])
                        P.op("dve", lambda e, qq=qq: e.tensor_tensor(out=acc[:, CTX:TAU], in0=qq[:, CTX:TAU], in1=cosT[:, :], op=ALU.mult),
                             [qqk, "r_cos"], ["r_acc"])
                        P.op("pool", lambda e, tp=tp, ob=ob: e.tensor_tensor(out=ob[:, CTX:TAU], in0=acc[:, CTX:TAU], in1=tp[:, :], op=ALU.add),
                             ["r_acc"] + [(tpk, g) for g in range(4)], [(obk, 1)])
                    else:
                        wi = (2 if is_k else 0) + j
                        for (a0, a1) in ((0, CTX), (CTX, TAU)):
                            n = a1 - a0
                            P.op("dve", lambda e, rw=rw, a0=a0, a1=a1, wi=wi: e.tensor_scalar(
                                out=acc[:, a0:a1], in0=rw[:, a0:a1], scalar1=mcw[:, wi, 1:2], scalar2=None, op0=ALU.mult),
                                 [rwk, "r_mcw"], ["r_acc"])
                            P.op("dve", lambda e, rw=rw, a0=a0, a1=a1, wi=wi: e.scalar_tensor_tensor(
                                out=acc[:, a0 + 1:a1], in0=rw[:, a0:a1 - 1], scalar=mcw[:, wi, 0:1], in1=acc[:, a0 + 1:a1],
                                op0=ALU.mult, op1=ALU.add), [rwk, "r_mcw"], ["r_acc"])
                            P.op("dve", lambda e, rw=rw, a0=a0, a1=a1, wi=wi: e.scalar_tensor_tensor(
                                out=acc[:, a0:a1 - 1], in0=rw[:, a0 + 1:a1], scalar=mcw[:, wi, 2:3], in1=acc[:, a0:a1 - 1],
                                op0=ALU.mult, op1=ALU.add), [rwk, "r_mcw"], ["r_acc"])
                        if is_k:
                            P.op("act", lambda e: e.activation(out=acc[:, :], in_=acc[:, :], func=AF.Silu), [], ["r_acc"])
                            P.op("pool", lambda e, ob=ob: e.tensor_scalar(out=ob[:, :], in0=acc[:, :], scalar1=0.125, scalar2=None,
                                                                         op0=ALU.mult), ["r_acc"], [(obk, 0), (obk, 1)])
                        else:
                            P.op("act", lambda e, ob=ob: e.activation(out=ob[:, :], in_=acc[:, :], func=AF.Silu),
                                 ["r_acc"], [(obk, 0), (obk, 1)])
                    P.dma(self.seg[sname][b, j], ob[:], [(obk, 0), (obk, 1)], [("seg", sname, b, j)])
                    if is_k:
                        for g6 in range(3):
                            bi = 4 + (ntk % 2)
                            pbf = self.bank_bf(bi)
                            tkt, tkk = tk[ntk % 2], "r_tk%d" % (ntk % 2)
                            ntk += 1

                            def ftr(e, pbf=pbf, ob=ob, g6=g6):
                                return [e.transpose(pbf[:, q * 128:(q + 1) * 128], ob[:, (g6 * 6 + q) * 128:(g6 * 6 + q + 1) * 128],
                                                    ident[:]) for q in range(6)]
                            P.op("pe", ftr, [(obk, 0), (obk, 1), "r_id"], [self.pk(bi)])
                            P.op("dve", lambda e, pbf=pbf, tkt=tkt: e.tensor_copy(
                                out=tkt[:], in_=pbf[:, 0:768].rearrange("p (q c) -> p q c", c=128)), [], [self.pk(bi), tkk])
                            P.dma(self.s_ktok[b, g6 * 6:g6 * 6 + 6, :, kcol:kcol + 128].rearrange("q p c -> p q c"), tkt[:],
                                  [tkk], [("s_ktok", b)])
            P.barrier()


    def phase_C2(self, layer):
        nc, P = self.nc, self.P
        last = layer == DEPTH - 1
        NCH = TAU // 128
        with contextlib.ExitStack() as st:
            sb = self.mk_sb(st)
            U = sb("t_U", [128, 128], F32)
            Lw = sb("t_L", [128, 128], F32)
            Df = sb("t_Df", [128, 128], F32)
            Db = sb("t_Db", [128, 128], F32)
            NEGf = sb("t_NEGf", [128, 128], F32)
            NEGb = sb("t_NEGb", [128, 128], F32)
            io = sb("t_io", [128, 256], F32)
            ioc = sb("t_ioc", [128, 2], F32)
            Jm = sb("t_J", [128, 128], BF16)
            onesF = sb("t_onesF", [128, 128], F32)
            ones64 = sb("t_ones64", [128, 64], BF16)
            epsc = sb("t_epsc", [128, 1], F32)
            lgfull = sb("t_lgfull", [128, 2, 6], F32)
            lgcol = sb("t_lgcol", [128, 2, 3], F32)
            ETr = sb("t_ETr", [128, 6, 128], F32)
            et1 = sb("t_et1", [128, 128], F32)
            et2 = sb("t_et2", [128, 128], F32)
            dqr = sb("t_dqr", [128, 2, 3, 128], F32)
            wkr = sb("t_wkr", [128, 2, 6], F32)
            decr = sb("t_decr", [128, 2, 3], F32)
            gbias = sb("t_gbias", [128, 16], F32)
            qT = sb("t_qT", [128, 5, TAU], BF16)
            kT = sb("t_kT", [128, 5, TAU], BF16)
            gT = sb("t_gT", [128, 5, TAU], BF16)
            ktok = sb("t_ktok", [128, NCH, 640], BF16)
            vtok = sb("t_vtok", [128, NCH, 640], BF16)
            mg = sb("t_mg", [128, NCH, 16], F32)
            gi = sb("t_gi", [128, NCH, 8], F32)
            lf = sb("t_lf", [128, NCH, 8], F32)
            gtmp = sb("t_gtmp", [128, NCH, 8], F32)
            gtmp2 = sb("t_gtmp2", [128, NCH, 8], F32)
            bias1 = sb("t_bias1", [128, NCH, 8], F32)
            wkm = sb("t_wkm", [128, NCH, 8], F32)
            decm = sb("t_decm", [128, NCH, 8], F32)
            decp = sb("t_decp", [128, NCH, 2, 2], F32)
            Sf = sb("t_Sf", [128, 5, 64], F32)
            Sbk = sb("t_Sb", [128, 5, 64], F32)
            Nf = sb("t_Nf", [128, 2, 64], F32)
            Nbk = sb("t_Nb", [128, 2, 64], F32)
            Sf_bf = sb("t_Sfbf", [128, 5, 64], BF16)
            Nf_bf = sb("t_Nfbf", [128, 2, 64], BF16)
            Sb_st = sb("t_Sbst", [128, NCH, 5, 64], BF16)
            Nb_st = sb("t_Nbst", [128, NCH, 2, 64], BF16)
            kw = [sb("t_kw%d" % i, [128, 640], BF16) for i in range(2)]
            lfb = [sb("t_lfb%d" % i, [128, 128], F32) for i in range(4)]
            xe = [sb("t_xe%d" % i, [128, 4, 128], F32) for i in range(2)]
            Ef = sb("t_Ef", [128, 4, 128], F32)
            Eb = sb("t_Eb", [128, 4, 128], F32)
            hh_t = sb("t_hh", [128, 512], F32)
            dqm = sb("t_dqm", [128, 2, 2, 128], F32)
            PT = [sb("t_PT%d" % i, [128, 14, 128], BF16) for i in range(2)]
            qd = [sb("t_qd%d" % i, [128, 2, 5, 128], BF16) for i in range(2)]
            den = sb("t_den", [128, 512], F32)
            o_bf = sb("t_obf", [128, 640], BF16)
            xc = sb("t_xc", [128, 640], F32)
            sq = sb("t_sq", [128, 640], BF16)
            rs = sb("t_rs", [128, 640], F32)
            yo = [sb("t_yo%d" % i, [128, 5, 128], BF16) for i in range(2)]

            for (t_, d_, k_) in ((U, "U", "t_U"), (Lw, "L", "t_L"), (Df, "Df", "t_Df"), (Db, "Db", "t_Db"), (NEGf, "NEGf", "t_NEGf"),
                                 (NEGb, "NEGb", "t_NEGb"), (io, "io", "t_io"), (ioc, "ioc", "t_ioc"), (Jm, "J", "t_J")):
                P.dma(t_[:], self.attc[d_][:, :], [], [k_])
            P.op("pool", lambda e: e.memset(onesF[:], 1.0), [], ["t_onesF"])
            P.op("pool", lambda e: e.memset(ones64[:], 1.0), [], ["t_ones64"])
            P.op("pool", lambda e: e.memset(epsc[:], EPS), [], ["t_epsc"])
            rd = self.ret_decay[layer]
            P.dma(lgfull[:], rd.rearrange("d h -> (d h)").unsqueeze(0).to_broadcast([128, 12]).rearrange("p (d h) -> p d h", d=2),
                  [], ["t_lgfull"])
            for hh in range(2):
                for d in range(2):
                    P.dma(lgcol[hh * 64:(hh + 1) * 64, d, :],
                          self.ret_decay_h[layer, d, hh:hh + 1, :].to_broadcast([64, 3]), [], ["t_lgcol"])
            for (t_, k_, n_) in ((lgfull, "t_lgfull", 12), (lgcol, "t_lgcol", 6)):
                fl = t_[:].rearrange("p a b -> p (a b)")
                P.op("act", lambda e, fl=fl: e.activation(out=fl, in_=fl, func=AF.Exp, scale=-1.0), [], [k_])
                P.op("act", lambda e, fl=fl: e.activation(out=fl, in_=fl, func=AF.Ln, bias=1.0), [], [k_])
                P.op("dve", lambda e, fl=fl: e.tensor_scalar(out=fl, in0=fl, scalar1=-1.0, scalar2=None, op0=ALU.mult), [], [k_])
            for h in range(6):
                P.op("act", lambda e, h=h: e.activation(out=et1[:], in_=Df[:], func=AF.Exp, scale=lgfull[:, 0, h:h + 1]),
                     ["t_Df", "t_lgfull"], ["t_et1"])
                P.op("dve", lambda e: e.tensor_tensor(out=et1[:], in0=et1[:], in1=U[:], op=ALU.mult), ["t_U"], ["t_et1"])
                P.op("act", lambda e, h=h: e.activation(out=et2[:], in_=Db[:], func=AF.Exp, scale=lgfull[:, 1, h:h + 1]),
                     ["t_Db", "t_lgfull"], ["t_et2"])
                P.op("dve", lambda e: e.tensor_tensor(out=et2[:], in0=et2[:], in1=Lw[:], op=ALU.mult), ["t_L"], ["t_et2"])
                P.op("pool", lambda e, h=h: e.tensor_tensor(out=ETr[:, h, :], in0=et1[:], in1=et2[:], op=ALU.add),
                     ["t_et1", "t_et2"], ["t_ETr"])
            for d in range(2):
                for j in range(3):
                    P.op("act", lambda e, d=d, j=j: e.activation(out=dqr[:, d, j, :], in_=io[:, d * 128:(d + 1) * 128], func=AF.Exp,
                                                                  scale=lgcol[:, d, j:j + 1]), ["t_io", "t_lgcol"], ["t_dqr"])
                P.op("act", lambda e, d=d: e.activation(out=wkr[:, d, :], in_=lgfull[:, d, :], func=AF.Exp, scale=ioc[:, d:d + 1]),
                     ["t_ioc", "t_lgfull"], ["t_wkr"])
            P.op("act", lambda e: e.activation(out=decr[:].rearrange("p a b -> p (a b)"), in_=lgcol[:].rearrange("p a b -> p (a b)"),
                                               func=AF.Exp, scale=128.0), ["t_lgcol"], ["t_decr"])
            P.dma(gbias[:], self.ml_gate_bias[layer:layer + 1, :].to_broadcast([128, 16]), [], ["t_gbias"])

            import os as _os
            c2stop = _os.environ.get("C2STOP", "")
            if c2stop == "k":
                P.barrier()
                return
            fwd_order = list(range(NCH))
            bwd_order = [1, 0] + list(range(NCH - 1, 1, -1))
            BK = self.bank
            pk = self.pk
            for b in range(NB):
                for j in range(3):
                    P.dma(qT[:, j, :], self.seg["rq"][b, j], [], [("t_qT", j)])
                    P.dma(kT[:, j, :], self.seg["rk"][b, j], [], [("t_kT", j)])
                    P.dma(gT[:, j, :], self.seg["rg"][b, j], [], [("t_gT", j)], eng="pool")
                for j in range(2):
                    P.dma(qT[:, 3 + j, :], self.seg["mq"][b, j], [], [("t_qT", 3 + j)])
                    P.dma(kT[:, 3 + j, :], self.seg["mk"][b, j], [], [("t_kT", 3 + j)])
                    P.dma(gT[:, 3 + j, :], self.seg["mo"][b, j], [], [("t_gT", 3 + j)], eng="pool")
                qk_keys = [("t_qT", j) for j in range(5)] + [("t_kT", j) for j in range(5)]
                P.dma(ktok[:], self.s_ktok[b].rearrange("c p n -> p c n"), [], ["t_ktok"], eng="pool")
                P.dma(vtok[:, :, 0:384], self.s_rv[b].rearrange("c p n -> p c n"), [], [("t_vtok", 0)])
                P.dma(vtok[:, :, 384:640], self.s_mv[b].rearrange("c p n -> p c n"), [], [("t_vtok", 1)])
                vkeys = [("t_vtok", 0), ("t_vtok", 1)]
                P.dma(mg[:], self.s_mg[b].rearrange("c p n -> p c n"), [], ["t_mg"])
                P.op("dve", lambda e: e.tensor_tensor(out=mg[:], in0=mg[:], in1=gbias[:].unsqueeze(1).to_broadcast([128, NCH, 16]),
                                                      op=ALU.add), ["t_gbias"], ["t_mg"])
                mg4 = mg[:].rearrange("p c (d g h) -> p c d g h", d=2, g=2)
                gi4 = gi[:].rearrange("p c (d h) -> p c d h", d=2)
                lf4 = lf[:].rearrange("p c (d h) -> p c d h", d=2)
                for d in range(2):
                    P.op("dve", lambda e, d=d: e.tensor_copy(out=gi4[:, :, d, :], in_=mg4[:, :, d, 0, :]), ["t_mg"], [("t_gi", d)])
                    P.op("dve", lambda e, d=d: e.tensor_copy(out=lf4[:, :, d, :], in_=mg4[:, :, d, 1, :]), ["t_mg"], [("t_lf", d)])
                gik = [("t_gi", 0), ("t_gi", 1)]
                lfk = [("t_lf", 0), ("t_lf", 1)]
                P.op("dve", lambda e: e.tensor_scalar(out=gtmp[:], in0=lf[:], scalar1=-1.0, scalar2=None, op0=ALU.mult), lfk, ["t_gtmp"])
                P.op("dve", lambda e: e.tensor_tensor(out=gtmp[:], in0=gtmp[:], in1=lf[:], op=ALU.max), lfk, ["t_gtmp"])
                P.op("act", lambda e: e.activation(out=gtmp[:], in_=gtmp[:], func=AF.Exp, scale=-1.0), [], ["t_gtmp"])
                P.op("act", lambda e: e.activation(out=gtmp[:], in_=gtmp[:], func=AF.Ln, bias=1.0), [], ["t_gtmp"])
                P.op("dve", lambda e: e.tensor_scalar(out=gtmp2[:], in0=lf[:], scalar1=0.0, scalar2=None, op0=ALU.min), lfk, ["t_gtmp2"])
                P.op("dve", lambda e: e.tensor_tensor(out=lf[:], in0=gtmp2[:], in1=gtmp[:], op=ALU.subtract),
                     ["t_gtmp", "t_gtmp2"], lfk)
                for c in range(NCH):
                    ps = BK[0]
                    P.op("pe", lambda e, c=c, ps=ps: [
                        e.matmul(ps[:, 0:4], U[:, :], lf[:, c, 0:4], start=True, stop=True),
                        e.matmul(ps[:, 4:8], Lw[:, :], lf[:, c, 4:8], start=True, stop=True),
                        e.matmul(ps[:, 8:16], onesF[:, :], lf[:, c, 0:8], start=True, stop=True)],
                         lfk + ["t_U", "t_L", "t_onesF"], [pk(0)])
                    P.op("dve", lambda e, c=c, ps=ps: e.tensor_tensor(out=bias1[:, c, :], in0=gi[:, c, :], in1=ps[:, 0:8],
                                                                     op=ALU.subtract), gik, [pk(0), ("t_bias1", c)])
                    P.op("dve", lambda e, c=c, ps=ps: e.tensor_tensor(out=wkm[:, c, :], in0=bias1[:, c, :], in1=ps[:, 8:16],
                                                                     op=ALU.add), [("t_bias1", c)], [pk(0), ("t_wkm", c)])
                    P.op("act", lambda e, c=c, ps=ps: e.activation(out=decm[:, c, :], in_=ps[:, 8:16], func=AF.Exp),
                         [], [pk(0), ("t_decm", c)])
                wkmk = [("t_wkm", c) for c in range(NCH)]
                decmk = [("t_decm", c) for c in range(NCH)]
                P.op("act", lambda e: e.activation(out=wkm[:], in_=wkm[:], func=AF.Exp), [], wkmk)
                dm5 = decm[:].rearrange("p c (d j t) -> p c d j t", d=2, t=2)
                for hh in range(2):
                    P.op("dve", lambda e, hh=hh: e.tensor_copy(out=decp[hh * 64:(hh + 1) * 64], in_=dm5[hh * 64:(hh + 1) * 64, :, :, :, hh]),
                         decmk, [("t_decp", hh)])
                decpk = [("t_decp", 0), ("t_decp", 1)]
                if c2stop == "g":
                    P.barrier()
                    return

                def kw_ops(c, d, kwt, kwk):
                    P.op("pool", lambda e: e.tensor_tensor(
                        out=kwt[:, 0:384].rearrange("p (h c) -> p h c", c=64), in0=ktok[:, c, 0:384].rearrange("p (h c) -> p h c", c=64),
                        in1=wkr[:, d, :].unsqueeze(2).to_broadcast([128, 6, 64]), op=ALU.mult), ["t_ktok", "t_wkr"], [(kwk, 0)])
                    P.op("pool", lambda e: e.tensor_tensor(
                        out=kwt[:, 384:640].rearrange("p (h c) -> p h c", c=64), in0=ktok[:, c, 384:640].rearrange("p (h c) -> p h c", c=64),
                        in1=wkm[:, c, d * 4:(d + 1) * 4].unsqueeze(2).to_broadcast([128, 4, 64]), op=ALU.mult),
                         ["t_ktok"] + wkmk, [(kwk, 1)])

                def state_update(c, d, kwt, kwk, S_, Sk, N_, Nk, bank_i):
                    ps = BK[bank_i]

                    def fds(e):
                        r = []
                        for h in range(10):
                            j, hh = h // 2, h % 2
                            r.append(e.matmul(ps[hh * 64:(hh + 1) * 64, j * 64:(j + 1) * 64], kwt[:, h * 64:(h + 1) * 64],
                                              vtok[:, c, h * 64:(h + 1) * 64], start=True, stop=True))
                        for h in range(6, 10):
                            j, hh = h // 2, h % 2
                            r.append(e.matmul(ps[hh * 64:(hh + 1) * 64, 320 + (j - 3) * 64:320 + (j - 2) * 64], kwt[:, h * 64:(h + 1) * 64],
                                              ones64[:, :], start=True, stop=True))
                        return r
                    P.op("pe", fds, [(kwk, 0), (kwk, 1), "t_ones64"] + vkeys, [pk(bank_i)])
                    for j in range(5):
                        dec_ap = decr[:, d, j:j + 1] if j < 3 else decp[:, c, d, j - 3:j - 2]
                        P.op("dve", lambda e, j=j, dec_ap=dec_ap: e.scalar_tensor_tensor(
                            out=S_[:, j, :], in0=S_[:, j, :], scalar=dec_ap, in1=ps[:, j * 64:(j + 1) * 64], op0=ALU.mult, op1=ALU.add),
                             ["t_decr"] + decpk, [pk(bank_i), (Sk, j)])
                    for j in range(2):
                        dec_ap = decp[:, c, d, j:j + 1]
                        P.op("dve", lambda e, j=j, dec_ap=dec_ap: e.scalar_tensor_tensor(
                            out=N_[:, j, :], in0=N_[:, j, :], scalar=dec_ap, in1=ps[:, 320 + j * 64:320 + (j + 1) * 64],
                            op0=ALU.mult, op1=ALU.add), decpk, [pk(bank_i), (Nk, j)])

                Sfk = [("t_Sf", j) for j in range(5)]
                Sbk_ = [("t_Sb", j) for j in range(5)]
                Nfk = [("t_Nf", j) for j in range(2)]
                Nbk_ = [("t_Nb", j) for j in range(2)]
                P.op("pool", lambda e: e.memset(Sf[:], 0.0), [], Sfk)
                P.op("pool", lambda e: e.memset(Sbk[:], 0.0), [], Sbk_)
                P.op("pool", lambda e: e.memset(Nf[:], 0.0), [], Nfk)
                P.op("pool", lambda e: e.memset(Nbk[:], 0.0), [], Nbk_)
                P.op("pool", lambda e: e.memset(Sf_bf[:], 0.0), [], ["t_Sfbf"])
                P.op("pool", lambda e: e.memset(Nf_bf[:], 0.0), [], ["t_Nfbf"])
                for ic, c in enumerate(bwd_order):
                    P.op("act", lambda e, c=c: e.activation(out=Sb_st[:, c], in_=Sbk[:], func=AF.Copy), Sbk_, [("t_Sbst", c)])
                    P.op("act", lambda e, c=c: e.activation(out=Nb_st[:, c], in_=Nbk[:], func=AF.Copy), Nbk_, [("t_Nbst", c)])
                    kwt, kwk = kw[ic % 2], "t_kw%d" % (ic % 2)
                    kw_ops(c, 1, kwt, kwk)
                    state_update(c, 1, kwt, kwk, Sbk, "t_Sb", Nbk, "t_Nb", 1 + (ic % 2))
                if c2stop == "b":
                    P.barrier()
                    return
                def fwd_vars(ic, c):
                    return (slice(c * 128, (c + 1) * 128), not (last and c < 2), PT[ic % 2], "t_PT%d" % (ic % 2),
                            qd[ic % 2], "t_qd%d" % (ic % 2), yo[ic % 2], "t_yo%d" % (ic % 2))

                def stage1(ic, c):
                    ts, need_out, PTt, PTk, qdt, qdk, yot, yok = fwd_vars(ic, c)
                    if not need_out:
                        return
                    for d in range(2):
                        tri = U if d == 0 else Lw
                        trik = "t_U" if d == 0 else "t_L"
                        neg = NEGf if d == 0 else NEGb
                        negk = "t_NEGf" if d == 0 else "t_NEGb"
                        pa = BK[d]
                        for hm in range(4):
                            lb, lbk = lfb[hm], "t_lfb%d" % hm
                            P.op("dve", lambda e, lb=lb, c=c, d=d, hm=hm: e.tensor_copy(
                                out=lb[:], in_=lf[:, c, d * 4 + hm:d * 4 + hm + 1].to_broadcast([128, 128])), lfk, [lbk])
                            P.op("pe", lambda e, pa=pa, lb=lb, tri=tri, hm=hm: e.matmul(
                                pa[:, hm * 128:(hm + 1) * 128], lb[:, :], tri[:, :], start=True, stop=True), [lbk, trik], [pk(d)])
                        xet, xek = xe[d], "t_xe%d" % d
                        P.op("dve", lambda e, pa=pa, xet=xet, neg=neg: e.tensor_tensor(
                            out=xet[:], in0=pa[:, :].rearrange("p (h t) -> p h t", t=128),
                            in1=neg[:].unsqueeze(1).to_broadcast([128, 4, 128]), op=ALU.add), [negk], [pk(d), xek])
                        Et = Ef if d == 0 else Eb
                        Ek = "t_Ef" if d == 0 else "t_Eb"
                        for hm in range(4):
                            P.op("act", lambda e, Et=Et, xet=xet, hm=hm, c=c, d=d: e.activation(
                                out=Et[:, hm, :], in_=xet[:, hm, :], func=AF.Exp, bias=bias1[:, c, d * 4 + hm:d * 4 + hm + 1], scale=1.0),
                                 [xek, ("t_bias1", c)], [(Ek, hm)])
                            j, hh = hm // 2, hm % 2
                            P.op("act", lambda e, pa=pa, d=d, j=j, hh=hh, hm=hm: e.activation(
                                out=dqm[hh * 64:(hh + 1) * 64, d, j, :], in_=pa[hh * 64:(hh + 1) * 64, hm * 128:(hm + 1) * 128], func=AF.Exp),
                                 [], [pk(d), ("t_dqm", d, j, hh)])
                    dqmk = [("t_dqm", d, j, hh) for d in range(2) for j in range(2) for hh in range(2)]
                    sc_dst = {}
                    for h in range(10):
                        if h < 8:
                            sc_dst[h] = (2 + h % 2, (h // 2) * 128)
                        else:
                            sc_dst[h] = (0 if h == 8 else 1, 0)

                    def fsc(e, c=c):
                        r = []
                        for h in range(10):
                            j, hh = h // 2, h % 2
                            bnk, col = sc_dst[h]
                            r.append(e.matmul(BK[bnk][:, col:col + 128],
                                              kT[hh * 64:(hh + 1) * 64, j, c * 128:(c + 1) * 128],
                                              qT[hh * 64:(hh + 1) * 64, j, c * 128:(c + 1) * 128], start=True, stop=True))
                        return r
                    P.op("pe", fsc, qk_keys, [pk(0), pk(1), pk(2), pk(3)])
                    for par in range(2):
                        P.op("dve", lambda e, PTt=PTt, par=par: e.tensor_tensor(
                            out=PTt[:, par:6:2, :], in0=BK[2 + par][:, 0:384].rearrange("p (h t) -> p h t", t=128),
                            in1=ETr[:, par:6:2, :], op=ALU.mult), ["t_ETr"], [pk(2 + par), (PTk, par)])
                    for hm in range(4):
                        bnk, col = sc_dst[6 + hm]
                        P.op("dve", lambda e, PTt=PTt, hm=hm, bnk=bnk, col=col: e.tensor_tensor(
                            out=PTt[:, 6 + hm, :], in0=BK[bnk][:, col:col + 128], in1=Ef[:, hm, :], op=ALU.mult),
                             [("t_Ef", hm)], [pk(bnk), (PTk, 2 + hm)])
                        P.op("dve", lambda e, PTt=PTt, hm=hm, bnk=bnk, col=col: e.tensor_tensor(
                            out=PTt[:, 10 + hm, :], in0=BK[bnk][:, col:col + 128], in1=Eb[:, hm, :], op=ALU.mult),
                             [("t_Eb", hm)], [pk(bnk), (PTk, 6 + hm)])
                    PTks = [(PTk, i) for i in range(10)]
                    for d in range(2):
                        P.op("pool", lambda e, qdt=qdt, d=d, ts=ts: e.tensor_tensor(out=qdt[:, d, 0:3, :], in0=qT[:, 0:3, ts],
                                                                                   in1=dqr[:, d, :, :], op=ALU.mult),
                             qk_keys + ["t_dqr"], [(qdk, d, 0)])
                        P.op("pool", lambda e, qdt=qdt, d=d, ts=ts: e.tensor_tensor(out=qdt[:, d, 3:5, :], in0=qT[:, 3:5, ts],
                                                                                   in1=dqm[:, d, :, :], op=ALU.mult),
                             qk_keys + dqmk, [(qdk, d, 1)])
                    qdks = [(qdk, d, i) for d in range(2) for i in range(2)]

                def stage2(ic, c):
                    ts, need_out, PTt, PTk, qdt, qdk, yot, yok = fwd_vars(ic, c)
                    PTks = [(PTk, i) for i in range(10)]
                    qdks = [(qdk, d, i) for d in range(2) for i in range(2)]
                    dqmk = [("t_dqm", d, j, hh) for d in range(2) for j in range(2) for hh in range(2)]
                    if need_out:
                        def fo(e, c=c, PTt=PTt, qdt=qdt):
                            r = []
                            for h in range(6):
                                j, hh = h // 2, h % 2
                                rows = slice(hh * 64, (hh + 1) * 64)
                                dst = BK[4][rows, j * 128:(j + 1) * 128]
                                r.append(e.matmul(dst, vtok[:, c, h * 64:(h + 1) * 64], PTt[:, h, :], start=True, stop=False))
                                r.append(e.matmul(dst, Sf_bf[rows, j, :], qdt[rows, 0, j, :], start=False, stop=False))
                                r.append(e.matmul(dst, Sb_st[rows, c, j, :], qdt[rows, 1, j, :], start=False, stop=True))
                            for hm in range(4):
                                h = 6 + hm
                                j, hh, jj = h // 2, h % 2, hm // 2
                                rows = slice(hh * 64, (hh + 1) * 64)
                                for d in range(2):
                                    pt = PTt[:, 6 + 4 * d + hm, :]
                                    st_S = Sf_bf[rows, j, :] if d == 0 else Sb_st[rows, c, j, :]
                                    st_N = Nf_bf[rows, jj, :] if d == 0 else Nb_st[rows, c, jj, :]
                                    dn = BK[5][rows, d * 256 + jj * 128:d * 256 + (jj + 1) * 128]
                                    dd = BK[6][rows, d * 256 + jj * 128:d * 256 + (jj + 1) * 128]
                                    r.append(e.matmul(dn, vtok[:, c, h * 64:(h + 1) * 64], pt, start=True, stop=False))
                                    r.append(e.matmul(dn, st_S, qdt[rows, d, j, :], start=False, stop=True))
                                    r.append(e.matmul(dd, ones64[:, :], pt, start=True, stop=False))
                                    r.append(e.matmul(dd, st_N, qdt[rows, d, j, :], start=False, stop=True))
                            return r
                        P.op("pe", fo, PTks + qdks + vkeys + ["t_Sfbf", "t_Nfbf", ("t_Sbst", c), ("t_Nbst", c), "t_ones64"],
                             [pk(4), pk(5), pk(6)])
                        P.op("act", lambda e: e.activation(out=den[:], in_=BK[6][:, :], func=AF.Abs), [], [pk(6), "t_den"])
                        P.op("dve", lambda e: e.tensor_scalar(out=den[:], in0=den[:], scalar1=1.0, scalar2=None, op0=ALU.max), [], ["t_den"])
                        P.op("act", lambda e: e.activation(out=den[:], in_=den[:], func=AF.Ln), [], ["t_den"])
                        P.op("act", lambda e: e.activation(out=den[:], in_=den[:], func=AF.Exp, scale=-1.0), [], ["t_den"])
                        P.op("act", lambda e: e.activation(out=o_bf[:, 0:384], in_=BK[4][:, 0:384], func=AF.Copy), [], [pk(4), ("t_obf", 0)])
                        P.op("dve", lambda e: e.tensor_tensor(out=hh_t[:], in0=BK[5][:, :], in1=den[:], op=ALU.mult),
                             ["t_den"], [pk(5), "t_hh"])
                        P.op("pool", lambda e: e.tensor_tensor(out=o_bf[:, 384:640], in0=hh_t[:, 0:256], in1=hh_t[:, 256:512], op=ALU.add),
                             ["t_hh"], [("t_obf", 1), ("t_obf", 2)])
                        obk = [("t_obf", i) for i in range(3)]
                        P.op("pe", lambda e: [e.matmul(BK[4][:, 0:512], Jm[:, :], o_bf[:, 0:512], start=True, stop=True),
                                              e.matmul(BK[5][:, 0:128], Jm[:, :], o_bf[:, 512:640], start=True, stop=True)],
                             obk + ["t_J"], [pk(4), pk(5)])
                        P.op("dve", lambda e: e.tensor_tensor(out=xc[:, 0:512], in0=o_bf[:, 0:512], in1=BK[4][:, 0:512], op=ALU.subtract),
                             obk, [pk(4), ("t_xc", 0)])
                        P.op("dve", lambda e: e.tensor_tensor(out=xc[:, 512:640], in0=o_bf[:, 512:640], in1=BK[5][:, 0:128], op=ALU.subtract),
                             obk, [pk(5), ("t_xc", 1)])
                        xck = [("t_xc", 0), ("t_xc", 1)]
                        P.op("act", lambda e: e.activation(out=sq[:], in_=xc[:], func=AF.Square), xck, ["t_sq"])
                        P.op("pe", lambda e: [e.matmul(BK[4][:, 0:512], Jm[:, :], sq[:, 0:512], start=True, stop=True),
                                              e.matmul(BK[5][:, 0:128], Jm[:, :], sq[:, 512:640], start=True, stop=True)],
                             ["t_sq", "t_J"], [pk(4), pk(5)])
                        P.op("act", lambda e: e.activation(out=rs[:, 0:512], in_=BK[4][:, 0:512], func=AF.Ln, bias=epsc[:, 0:1]),
                             ["t_epsc"], [pk(4), ("t_rs", 0)])
                        P.op("act", lambda e: e.activation(out=rs[:, 512:640], in_=BK[5][:, 0:128], func=AF.Ln, bias=epsc[:, 0:1]),
                             ["t_epsc"], [pk(5), ("t_rs", 1)])
                        rsk = [("t_rs", 0), ("t_rs", 1)]
                        P.op("act", lambda e: e.activation(out=rs[:], in_=rs[:], func=AF.Exp, scale=-0.5), [], rsk)
                        P.op("dve", lambda e: e.tensor_tensor(out=xc[:], in0=xc[:], in1=rs[:], op=ALU.mult), rsk, xck)
                        P.op("pool", lambda e, yot=yot, ts=ts: e.tensor_tensor(out=yot[:], in0=xc[:].rearrange("p (j t) -> p j t", t=128),
                                                                               in1=gT[:, :, ts], op=ALU.mult),
                             xck + [("t_gT", j) for j in range(5)], [yok])
                        P.dma(self.s_yatt[b, :, :, ts].rearrange("j p t -> p j t"), yot[:], [yok], [("s_yatt", b)])
                    kwt, kwk = kw[ic % 2], "t_kw%d" % (ic % 2)
                    kw_ops(c, 0, kwt, kwk)
                    state_update(c, 0, kwt, kwk, Sf, "t_Sf", Nf, "t_Nf", 7)
                    P.op("act", lambda e: e.activation(out=Sf_bf[:], in_=Sf[:], func=AF.Copy), Sfk, ["t_Sfbf"])
                    P.op("act", lambda e: e.activation(out=Nf_bf[:], in_=Nf[:], func=AF.Copy), Nfk, ["t_Nfbf"])

                stage1(0, fwd_order[0])
                for ic, c in enumerate(fwd_order):
                    P.begin_capture()
                    stage2(ic, c)
                    capA = P.end_capture()
                    P.begin_capture()
                    if ic + 1 < len(fwd_order):
                        stage1(ic + 1, fwd_order[ic + 1])
                    capB = P.end_capture()
                    P.replay(capA, capB)
            P.barrier()


    def rms_rstd(self, xt, xk, n, sq2, rstd, ones, bank_i):
        P = self.P
        ps = self.bank[bank_i]
        for k in range(KC):
            sqt, sqk = sq2[k % 2], "sq2_%d" % (k % 2)
            P.op("act", lambda e, k=k, sqt=sqt: e.activation(out=sqt[:, 0:n], in_=xt[:, k, 0:n], func=AF.Square), [xk], [sqk])
            P.op("pe", lambda e, k=k, sqt=sqt: e.matmul(ps[:, 0:n], ones[:], sqt[:, 0:n], start=(k == 0), stop=(k == KC - 1)),
                 [sqk, "ones_d"], [self.pk(bank_i)])
        P.op("dve", lambda e: e.tensor_scalar(out=rstd[:, 0:n], in0=ps[:, 0:n], scalar1=EPS, scalar2=None, op0=ALU.add),
             [], [self.pk(bank_i), "rstd"])
        P.op("act", lambda e: e.activation(out=rstd[:, 0:n], in_=rstd[:, 0:n], func=AF.Sqrt), [], ["rstd"])
        P.op("dve", lambda e: e.reciprocal(out=rstd[:, 0:n], in_=rstd[:, 0:n]), [], ["rstd"])

    def token_groups(self, with_ctx):
        groups = []
        for b in range(NB):
            if with_ctx:
                groups.append((b, 2, 0, CTX))
            for g in range(4):
                groups.append((b, b, CTX + 512 * g, 512))
        return groups

    def phase_D(self, layer, xsrc, xdst):
        nc, P = self.nc, self.P
        last = layer == DEPTH - 1
        with contextlib.ExitStack() as st:
            sb = self.mk_sb(st)
            Wu = sb("d_wu", [128, KC, D], BF16)
            Wo = sb("d_wo", [128, KC, D], BF16)
            modt = sb("d_mod", [128, 48, 3], F32)
            xg = [sb("d_xg%d" % i, [128, KC, 512], F32) for i in range(2)]
            yb = [sb("d_yb%d" % i, [128, 8, 512], BF16) for i in range(2)]
            gtile = [sb("d_gt%d" % i, [128, 24, 512], BF16) for i in range(2)]
            mm_ = [sb("d_mm%d" % i, [128, 512], F32) for i in range(3)]
            accT = sb("d_acc", [128, KC, 512], BF16)
            P.dma(Wu[:], self.w_up[layer].rearrange("(c p) n -> p c n", p=128), [], ["d_wu"], eng="pool")
            P.dma(Wo[:], self.w_out[layer].rearrange("(c p) n -> p c n", p=128), [], ["d_wo"], eng="pool")
            P.dma(modt[:], self.s_mod[layer], [], ["d_mod"])
            branches = ((0, (0, 1, 2)), (1, (3, 4, 5)), (2, (6, 7)))
            dgroups = self.token_groups(not last)

            def load_D(gi2):
                b2, s2, t2, n2 = dgroups[gi2]
                P.dma(xg[gi2 % 2][:, :, 0:n2], xsrc[b2, :, :, t2:t2 + n2].rearrange("c p t -> p c t"), [], ["d_xg%d" % (gi2 % 2)])
                P.dma(yb[gi2 % 2][:, 0:3, 0:n2], self.seg_yhy[b2, :, :, t2:t2 + n2].rearrange("c p t -> p c t"), [],
                      [("d_yb%d" % (gi2 % 2), 0)])
                P.dma(yb[gi2 % 2][:, 3:8, 0:n2], self.s_yatt[b2, :, :, t2:t2 + n2].rearrange("c p t -> p c t"), [],
                      [("d_yb%d" % (gi2 % 2), 1)])
                P.dma(gtile[gi2 % 2][:, :, 0:n2], self.seg["gate"][b2, :, :, t2:t2 + n2].rearrange("c p t -> p c t"), [],
                      ["d_gt%d" % (gi2 % 2)], eng="pool")

            load_D(0)
            for gi_, (b, s_, t0, n) in enumerate(dgroups):
                xt, xk = xg[gi_ % 2], "d_xg%d" % (gi_ % 2)
                yt, yk = yb[gi_ % 2], "d_yb%d" % (gi_ % 2)
                gt_, gk = gtile[gi_ % 2], "d_gt%d" % (gi_ % 2)
                if gi_ + 1 < len(dgroups):
                    load_D(gi_ + 1)
                for m in range(KC):
                    for (bi_, ks) in branches:
                        ps = self.bank[bi_]

                        def fup(e, ps=ps, ks=ks, m=m, yt=yt, n=n):
                            return [e.matmul(ps[:, 0:n], Wu[:, k, m * 128:(m + 1) * 128], yt[:, k, 0:n],
                                             start=(k == ks[0]), stop=(k == ks[-1])) for k in ks]
                        P.op("pe", fup, ["d_wu", (yk, 0), (yk, 1)], [self.pk(bi_)])
                        P.op("dve", lambda e, ps=ps, bi_=bi_, m=m, gt_=gt_, n=n: e.tensor_tensor(
                            out=mm_[bi_][:, 0:n], in0=ps[:, 0:n], in1=gt_[:, bi_ * 8 + m, 0:n], op=ALU.mult),
                             [gk], [self.pk(bi_), "d_mm%d" % bi_])
                    P.op("pool", lambda e, n=n: e.tensor_tensor(out=mm_[0][:, 0:n], in0=mm_[0][:, 0:n], in1=mm_[1][:, 0:n], op=ALU.add),
                         ["d_mm1"], ["d_mm0"])
                    P.op("dve", lambda e, m=m, n=n: e.tensor_tensor(out=accT[:, m, 0:n], in0=mm_[0][:, 0:n], in1=mm_[2][:, 0:n], op=ALU.add),
                         ["d_mm0", "d_mm2"], [("d_acc", m)])
                acck = [("d_acc", m) for m in range(KC)]
                for m2 in range(KC):
                    bi_ = 3 + (m2 % 2)
                    ps = self.bank[bi_]

                    def fout(e, ps=ps, m2=m2, n=n):
                        return [e.matmul(ps[:, 0:n], Wo[:, k, m2 * 128:(m2 + 1) * 128], accT[:, k, 0:n],
                                         start=(k == 0), stop=(k == KC - 1)) for k in range(KC)]
                    P.op("pe", fout, ["d_wo"] + acck, [self.pk(bi_)])
                    P.op("dve", lambda e, ps=ps, m2=m2, xt=xt, n=n, s_=s_: e.scalar_tensor_tensor(
                        out=xt[:, m2, 0:n], in0=ps[:, 0:n], scalar=modt[:, 16 + m2, s_:s_ + 1], in1=xt[:, m2, 0:n],
                        op0=ALU.mult, op1=ALU.add), ["d_mod"], [self.pk(bi_), xk])
                P.dma(xdst[b, :, :, t0:t0 + n].rearrange("c p t -> p c t"), xt[:, :, 0:n], [xk], [("xdst", b)])
            P.barrier()

    WBLK = ((0, 6), (6, 12), (12, 17), (17, 22))

    def ffn_core(self, W1, W3, W2, wkeys, h, hks, n, aT, s1, epilogue):
        P = self.P
        blk_of = {}
        for bi_, (a_, b_) in enumerate(self.WBLK):
            for j in range(a_, b_):
                blk_of[j] = bi_
        if isinstance(wkeys, dict):
            k1 = lambda j: [wkeys["w1"][blk_of[j]]]
            k3 = lambda j: [wkeys["w3"][blk_of[j]]]
            k2 = list(wkeys["w2"])
        else:
            k1 = k3 = lambda j: list(wkeys)
            k2 = list(wkeys)
        for j in range(FC):
            b1, b3 = j % 2, 2 + (j % 2)
            p1, p3 = self.bank[b1], self.bank[b3]

            def f1(e, pp, Wt, j=j):
                return [e.matmul(pp[:, 0:n], Wt[:, k, j * 128:(j + 1) * 128], h[:, k, 0:n], start=(k == 0), stop=(k == KC - 1))
                        for k in range(KC)]
            P.op("pe", lambda e, p1=p1, f1=f1: f1(e, p1, W1), hks + k1(j), [self.pk(b1)])
            P.op("pe", lambda e, p3=p3, f1=f1: f1(e, p3, W3), hks + k3(j), [self.pk(b3)])
            st_, sk = s1[j % 2], "f_s1_%d" % (j % 2)
            P.op("act", lambda e, p1=p1, st_=st_: e.activation(out=st_[:, 0:n], in_=p1[:, 0:n], func=AF.Silu), [], [self.pk(b1), sk])
            P.op("dve", lambda e, p3=p3, st_=st_, j=j: e.tensor_tensor(out=aT[:, j, 0:n], in0=p3[:, 0:n], in1=st_[:, 0:n], op=ALU.mult),
                 [sk], [self.pk(b3), ("f_aT", j)])
        ak = [("f_aT", j) for j in range(FC)]
        for m2 in range(KC):
            bo = 4 + (m2 % 2)
            po = self.bank[bo]

            def f2(e, po=po, m2=m2):
                return [e.matmul(po[:, 0:n], W2[:, j, m2 * 128:(m2 + 1) * 128], aT[:, j, 0:n], start=(j == 0), stop=(j == FC - 1))
                        for j in range(FC)]
            P.op("pe", f2, ak + k2, [self.pk(bo)])
            epilogue(m2, po, self.pk(bo))

    def phase_E_dense(self, layer, xsrc, xdst):
        nc, P = self.nc, self.P
        last = layer == DEPTH - 1
        jd = layer // 2
        with contextlib.ExitStack() as st:
            sb = self.mk_sb(st)
            W1 = sb("e_w1", [128, KC, D_FF], BF16)
            W3 = sb("e_w3", [128, KC, D_FF], BF16)
            W2 = sb("e_w2", [128, FC, D], BF16)
            modt = sb("e_mod", [128, 48, 3], F32)
            gm = sb("e_g", [128, KC], F32)
            Am = sb("e_A", [128, KC, 3], F32)
            ones = sb("e_ones", [128, 128], BF16)
            xt = sb("e_xg", [128, KC, 512], F32)
            sq2 = [sb("e_sq%d" % i, [128, 512], BF16) for i in range(2)]
            rstd = sb("e_rstd", [128, 512], F32)
            t1 = [sb("e_t1%d" % i, [128, 512], F32) for i in range(2)]
            hT = sb("e_hT", [128, KC, 512], BF16)
            aT = sb("e_aT", [128, FC, 512], BF16)
            s1 = [sb("e_s1%d" % i, [128, 512], F32) for i in range(2)]
            w1s = self.ffn_w1[jd].rearrange("(c p) n -> p c n", p=128)
            w3s = self.ffn_w3[jd].rearrange("(c p) n -> p c n", p=128)
            w2s = self.ffn_w2[jd].rearrange("(c p) n -> p c n", p=128)
            for q, (ja, jb) in enumerate(self.WBLK):
                P.dma(W1[:, :, ja * 128:jb * 128], w1s[:, :, ja * 128:jb * 128], [], [("e_w1", q)], eng="pool")
                P.dma(W3[:, :, ja * 128:jb * 128], w3s[:, :, ja * 128:jb * 128], [], [("e_w3", q)], eng="pool")
            for q, (ja, jb) in enumerate(self.WBLK):
                P.dma(W2[:, ja:jb, :], w2s[:, ja:jb, :], [], [("e_w2", q)], eng="pool")
            wkeys = {"w1": [("e_w1", q) for q in range(4)], "w3": [("e_w3", q) for q in range(4)],
                     "w2": [("e_w2", q) for q in range(4)]}
            P.dma(modt[:], self.s_mod[layer], [], ["e_mod"])
            P.dma(gm[:], self.g_ffnT[layer], [], ["e_g"])
            P.op("pool", lambda e: e.memset(ones[:], 1.0 / D), [], ["ones_d"])
            P.op("dve", lambda e: e.tensor_scalar(out=Am[:], in0=modt[:, 32:40, :], scalar1=1.0, scalar2=None, op0=ALU.add),
                 ["e_mod"], ["e_A"])
            for s in range(3):
                P.op("dve", lambda e, s=s: e.tensor_tensor(out=Am[:, :, s], in0=Am[:, :, s], in1=gm[:], op=ALU.mult),
                     ["e_A", "e_g"], ["e_A"])
            for gi_, (b, s_, t0, n) in enumerate(self.token_groups(not last)):
                xk = "e_xg"
                P.dma(xt[:, :, 0:n], xsrc[b, :, :, t0:t0 + n].rearrange("c p t -> p c t"), [], [xk])
                self.rms_rstd(xt, xk, n, sq2, rstd, ones, 6)
                hks = [("e_hT", k) for k in range(KC)]
                for k in range(KC):
                    tt, tk_ = t1[k % 2], "e_t1%d" % (k % 2)
                    P.op("dve", lambda e, k=k, tt=tt, n=n: e.tensor_tensor(out=tt[:, 0:n], in0=xt[:, k, 0:n], in1=rstd[:, 0:n], op=ALU.mult),
                         [xk, "rstd"], [tk_])
                    P.op("act", lambda e, k=k, tt=tt, n=n, s_=s_: e.activation(
                        out=hT[:, k, 0:n], in_=tt[:, 0:n], func=AF.Identity, scale=Am[:, k, s_:s_ + 1], bias=modt[:, 24 + k, s_:s_ + 1]),
                         [tk_, "e_A", "e_mod"], [("e_hT", k)])

                def epi(m2, po, pok, n=n, s_=s_):
                    P.op("dve", lambda e: e.scalar_tensor_tensor(
                        out=xt[:, m2, 0:n], in0=po[:, 0:n], scalar=modt[:, 40 + m2, s_:s_ + 1], in1=xt[:, m2, 0:n],
                        op0=ALU.mult, op1=ALU.add), ["e_mod"], [pok, xk])
                self.ffn_core(W1, W3, W2, wkeys, hT, hks, n, aT, s1, epi)
                P.dma(xdst[b, :, :, t0:t0 + n].rearrange("c p t -> p c t"), xt[:, :, 0:n], [xk], [("xdst", b)])
            P.barrier()

    def phase_E_moe(self, layer, xsrc, xdst):
        nc, P = self.nc, self.P
        jm = layer // 2
        groups = self.token_groups(False)
        with contextlib.ExitStack() as st:
            sb = self.mk_sb(st)
            modt = sb("g_mod", [128, 48, 3], F32)
            gm = sb("g_g", [128, KC], F32)
            Am = sb("g_A", [128, KC, 3], F32)
            ones = sb("g_ones", [128, 128], BF16)
            idf = sb("g_idf", [128, 128], F32)
            Wr = sb("g_wr", [128, KC, NEXP], F32)
            rb = sb("g_rb", [128, NEXP], F32)
            xg = [sb("g_xg%d" % i, [128, KC, 512], F32) for i in range(2)]
            sq2 = [sb("g_sq%d" % i, [128, 512], BF16) for i in range(2)]
            rstd = sb("g_rstd", [128, 512], F32)
            t1 = [sb("g_t1%d" % i, [128, 512], F32) for i in range(2)]
            hF = sb("g_hF", [128, KC, 512], F32)
            hT = sb("g_hT", [128, KC, 512], BF16)
            lg = sb("g_lg", [128, 4 * NEXP], F32)
            lg2 = sb("g_lg2", [128, 4 * NEXP], F32)
            eq1 = sb("g_eq1", [128, 4 * NEXP], F32)
            eq2 = sb("g_eq2", [128, 4 * NEXP], F32)
            mx = sb("g_mx", [128, 4, 4], F32)
            comb = sb("g_comb", [128, 4 * NEXP], F32)
            cT = sb("g_cT", [NEXP, 512], F32)
            P.dma(modt[:], self.s_mod[layer], [], ["g_mod"])
            P.dma(gm[:], self.g_ffnT[layer], [], ["g_g"])
            P.dma(idf[:], self.ident_d[:, :], [], ["g_idf"])
            P.dma(Wr[:], self.moe_router[jm].rearrange("(c p) n -> p c n", p=128), [], ["g_wr"])
            P.dma(rb[:], self.moe_router_b[jm:jm + 1, :].to_broadcast([128, NEXP]), [], ["g_rb"])
            P.op("pool", lambda e: e.memset(ones[:], 1.0 / D), [], ["ones_d"])
            P.op("dve", lambda e: e.tensor_scalar(out=Am[:], in0=modt[:, 32:40, :], scalar1=1.0, scalar2=None, op0=ALU.add),
                 ["g_mod"], ["g_A"])
            for s in range(3):
                P.op("dve", lambda e, s=s: e.tensor_tensor(out=Am[:, :, s], in0=Am[:, :, s], in1=gm[:], op=ALU.mult),
                     ["g_A", "g_g"], ["g_A"])
            for gi_, (b, s_, t0, n) in enumerate(groups):
                xt, xk = xg[gi_ % 2], "g_xg%d" % (gi_ % 2)
                tl = t0 - CTX
                P.dma(xt[:, :, 0:n], xsrc[b, :, :, t0:t0 + n].rearrange("c p t -> p c t"), [], [xk])
                P.dma(xdst[b, :, :, t0:t0 + n].rearrange("c p t -> p c t"), xt[:, :, 0:n], [xk], [("xdst", b, gi_)])
                self.rms_rstd(xt, xk, n, sq2, rstd, ones, 6)
                for k in range(KC):
                    tt, tk_ = t1[k % 2], "g_t1%d" % (k % 2)
                    P.op("dve", lambda e, k=k, tt=tt, xt=xt: e.tensor_tensor(out=tt[:, 0:n], in0=xt[:, k, 0:n], in1=rstd[:, 0:n], op=ALU.mult),
                         [xk, "rstd"], [tk_])
                    P.op("act", lambda e, k=k, tt=tt, s_=s_: e.activation(
                        out=hF[:, k, 0:n], in_=tt[:, 0:n], func=AF.Identity, scale=Am[:, k, s_:s_ + 1], bias=modt[:, 24 + k, s_:s_ + 1]),
                         [tk_, "g_A", "g_mod"], [("g_hF", k)])
                    P.op("act", lambda e, k=k, tt=tt, s_=s_: e.activation(
                        out=hT[:, k, 0:n], in_=tt[:, 0:n], func=AF.Identity, scale=Am[:, k, s_:s_ + 1], bias=modt[:, 24 + k, s_:s_ + 1]),
                         [tk_, "g_A", "g_mod"], [("g_hT", k)])
                P.dma(self.s_hffn[b, :, :, tl:tl + n].rearrange("c p t -> p c t"), hT[:, :, 0:n], [("g_hT", k) for k in range(KC)],
                      [("s_hffn", b, gi_)])
                hFk = [("g_hF", k) for k in range(KC)]
                nt = n // 128
                ps = self.bank[gi_ % 2]
                pbk = self.pk(gi_ % 2)

                def frt(e, ps=ps, nt=nt):
                    r = []
                    for tt_ in range(nt):
                        r += [e.matmul(ps[:, tt_ * NEXP:(tt_ + 1) * NEXP], hF[:, k, tt_ * 128:(tt_ + 1) * 128], Wr[:, k, :],
                                       start=(k == 0), stop=(k == KC - 1)) for k in range(KC)]
                    return r
                P.op("pe", frt, hFk + ["g_wr"], [pbk])
                v3 = lambda t_: t_[:, 0:nt * NEXP].rearrange("p (t e) -> p t e", e=NEXP)
                bc3 = lambda col: col.unsqueeze(2).to_broadcast([128, nt, NEXP])
                P.op("dve", lambda e: e.tensor_tensor(out=v3(lg), in0=v3(ps), in1=rb[:].unsqueeze(1).to_broadcast([128, nt, NEXP]),
                                                      op=ALU.add), ["g_rb"], [pbk, "g_lg"])
                P.op("dve", lambda e: e.tensor_reduce(out=mx[:, 0, 0:nt], in_=v3(lg), axis=AX.X, op=ALU.max), ["g_lg"], [("g_mx", 0)])
                P.op("dve", lambda e: e.tensor_tensor(out=v3(eq1), in0=v3(lg), in1=bc3(mx[:, 0, 0:nt]), op=ALU.is_equal),
                     ["g_lg", ("g_mx", 0)], ["g_eq1"])
                P.op("dve", lambda e: e.scalar_tensor_tensor(out=lg2[:, 0:nt * NEXP], in0=eq1[:, 0:nt * NEXP], scalar=-1.0e9,
                                                             in1=lg[:, 0:nt * NEXP], op0=ALU.mult, op1=ALU.add),
                     ["g_eq1", "g_lg"], ["g_lg2"])
                P.op("dve", lambda e: e.tensor_reduce(out=mx[:, 1, 0:nt], in_=v3(lg2), axis=AX.X, op=ALU.max), ["g_lg2"], [("g_mx", 1)])
                P.op("dve", lambda e: e.tensor_tensor(out=v3(eq2), in0=v3(lg2), in1=bc3(mx[:, 1, 0:nt]), op=ALU.is_equal),
                     ["g_lg2", ("g_mx", 1)], ["g_eq2"])
                P.op("dve", lambda e: e.tensor_tensor(out=mx[:, 2, 0:nt], in0=mx[:, 0, 0:nt], in1=mx[:, 1, 0:nt], op=ALU.subtract),
                     [("g_mx", 0), ("g_mx", 1)], [("g_mx", 2)])
                P.op("act", lambda e: e.activation(out=mx[:, 2, 0:nt], in_=mx[:, 2, 0:nt], func=AF.Sigmoid), [], [("g_mx", 2)])
                P.op("dve", lambda e: e.tensor_scalar(out=mx[:, 3, 0:nt], in0=mx[:, 2, 0:nt], scalar1=-1.0, scalar2=1.0,
                                                       op0=ALU.mult, op1=ALU.add), [("g_mx", 2)], [("g_mx", 3)])
                P.op("dve", lambda e: e.tensor_tensor(out=v3(eq1), in0=v3(eq1), in1=bc3(mx[:, 2, 0:nt]), op=ALU.mult),
                     [("g_mx", 2)], ["g_eq1"])
                P.op("dve", lambda e: e.tensor_tensor(out=v3(eq2), in0=v3(eq2), in1=bc3(mx[:, 3, 0:nt]), op=ALU.mult),
                     [("g_mx", 3)], ["g_eq2"])
                P.op("dve", lambda e: e.tensor_tensor(out=comb[:, 0:nt * NEXP], in0=eq1[:, 0:nt * NEXP], in1=eq2[:, 0:nt * NEXP], op=ALU.add),
                     ["g_eq1", "g_eq2"], ["g_comb"])
                pt_ = self.bank[2 + (gi_ % 2)]
                ptk = self.pk(2 + (gi_ % 2))
                P.op("pe", lambda e, pt_=pt_, nt=nt: [e.transpose(pt_[0:NEXP, tt_ * 128:(tt_ + 1) * 128],
                                                               comb[:, tt_ * NEXP:(tt_ + 1) * NEXP], idf[:, :]) for tt_ in range(nt)],
                     ["g_comb", "g_idf"], [ptk])
                P.op("act", lambda e, pt_=pt_: e.activation(out=cT[:, 0:n], in_=pt_[0:NEXP, 0:n], func=AF.Copy),
                     [], [ptk] + [("g_cT", q) for q in range(4)])
                P.dma(self.s_comb[:, b * LAT + tl:b * LAT + tl + n], cT[:, 0:n], [("g_cT", q) for q in range(n // 128)], [("s_comb", b, gi_)])
            P.barrier()
        import os as _os
        if _os.environ.get("MOE_PRE_ONLY"):
            return
        with contextlib.ExitStack() as st:
            sb = self.mk_sb(st)
            W1 = sb("x_w1", [128, KC, D_FF], BF16)
            W3 = sb("x_w3", [128, KC, D_FF], BF16)
            W2 = sb("x_w2", [128, FC, D], BF16)
            modt = sb("x_mod", [128, 48, 3], F32)
            sel = sb("x_sel", [NEXP, NEXP, 128], F32)
            hT2 = [sb("x_hT%d" % i, [128, KC, 512], BF16) for i in range(2)]
            xt = sb("x_xg", [128, KC, 512], F32)
            aT = sb("x_aT", [128, FC, 512], BF16)
            s1 = [sb("x_s1%d" % i, [128, 512], BF16) for i in range(2)]
            cT2 = [sb("x_cT%d" % i, [NEXP, 512], F32) for i in range(2)]
            cb = sb("x_cb", [128, 512], F32)
            tmp = [sb("x_tmp%d" % i, [128, 512], F32) for i in range(2)]
            P.dma(modt[:], self.s_mod[layer], [], ["x_mod"])
            P.dma(sel[:], self.moe_sel[:, :, :], [], ["x_sel"])
            wkeys = {"w1": [("x_w1", q) for q in range(4)], "w3": [("x_w3", q) for q in range(4)],
                     "w2": [("x_w2", q) for q in range(4)]}
            grp = groups[:int(_os.environ.get("MOE_NGRP", 8))]
            nex = int(_os.environ.get("MOE_NEXP", NEXP))
            work = [(ex, gi_) for ex in range(nex) for gi_ in range(len(grp))]

            def load_h(wi):
                ex, gi_ = work[wi]
                b, s_, t0, n = grp[gi_]
                tl = t0 - CTX
                ht, hk = hT2[wi % 2], "x_hT%d" % (wi % 2)
                P.dma(ht[:, :, 0:n], self.s_hffn[b, :, :, tl:tl + n].rearrange("c p t -> p c t"), [], [hk])
                ct, ck = cT2[wi % 2], "x_cT%d" % (wi % 2)
                P.dma(ct[:, 0:n], self.s_comb[:, b * LAT + tl:b * LAT + tl + n], [], [ck])

            def load_x(wi):
                ex, gi_ = work[wi]
                b, s_, t0, n = grp[gi_]
                P.dma(xt[:, :, 0:n], xdst[b, :, :, t0:t0 + n].rearrange("c p t -> p c t"), [("xdst", b, gi_)], ["x_xg"])

            load_h(0)
            load_x(0)
            for wi, (ex, gi_) in enumerate(work):
                b, s_, t0, n = grp[gi_]
                if gi_ == 0:
                    w1s = self.moe_w1[jm, ex].rearrange("(c p) n -> p c n", p=128)
                    w3s = self.moe_w3[jm, ex].rearrange("(c p) n -> p c n", p=128)
                    w2s = self.moe_w2[jm, ex].rearrange("(c p) n -> p c n", p=128)
                    for q, (ja, jb) in enumerate(self.WBLK):
                        P.dma(W1[:, :, ja * 128:jb * 128], w1s[:, :, ja * 128:jb * 128], [], [("x_w1", q)], eng="pool")
                        P.dma(W3[:, :, ja * 128:jb * 128], w3s[:, :, ja * 128:jb * 128], [], [("x_w3", q)], eng="pool")
                    for q, (ja, jb) in enumerate(self.WBLK):
                        P.dma(W2[:, ja:jb, :], w2s[:, ja:jb, :], [], [("x_w2", q)], eng="pool")
                ht, hk = hT2[wi % 2], "x_hT%d" % (wi % 2)
                ct, ck = cT2[wi % 2], "x_cT%d" % (wi % 2)
                if wi + 1 < len(work):
                    load_h(wi + 1)
                pcb = self.bank[7]
                P.op("pe", lambda e: e.matmul(pcb[:, 0:n], sel[:, ex, :], ct[:, 0:n], start=True, stop=True),
                     ["x_sel", ck], [self.pk(7)])
                P.op("act", lambda e: e.activation(out=cb[:, 0:n], in_=pcb[:, 0:n], func=AF.Copy), [], [self.pk(7), "x_cb"])

                def epi(m2, po, pok, n=n, s_=s_):
                    tt, tk_ = tmp[m2 % 2], "x_tmp%d" % (m2 % 2)
                    P.op("dve", lambda e: e.tensor_tensor(out=tt[:, 0:n], in0=po[:, 0:n], in1=cb[:, 0:n], op=ALU.mult),
                         ["x_cb"], [pok, tk_])
                    P.op("dve", lambda e: e.scalar_tensor_tensor(
                        out=xt[:, m2, 0:n], in0=tt[:, 0:n], scalar=modt[:, 40 + m2, s_:s_ + 1], in1=xt[:, m2, 0:n],
                        op0=ALU.mult, op1=ALU.add), ["x_mod", tk_], ["x_xg"])
                self.ffn_core(W1, W3, W2, wkeys, ht, [hk], n, aT, s1, epi)
                P.dma(xdst[b, :, :, t0:t0 + n].rearrange("c p t -> p c t"), xt[:, :, 0:n], ["x_xg"], [("xdst", b, gi_)])
                if wi + 1 < len(work):
                    load_x(wi + 1)
            P.barrier()

    def phase_final(self, xsrc):
        nc, P = self.nc, self.P
        with contextlib.ExitStack() as st:
            sb = self.mk_sb(st)
            gf = sb("z_g", [128, KC], F32)
            ones = sb("z_ones", [128, 128], BF16)
            xg = [sb("z_xg%d" % i, [128, KC, 512], F32) for i in range(2)]
            sq2 = [sb("z_sq%d" % i, [128, 512], BF16) for i in range(2)]
            rstd = sb("z_rstd", [128, 512], F32)
            P.dma(gf[:], self.g_finT[:, :], [], ["z_g"])
            P.op("pool", lambda e: e.memset(ones[:], 1.0 / D), [], ["ones_d"])
            for gi_, (b, s_, t0, n) in enumerate(self.token_groups(False)):
                xt, xk = xg[gi_ % 2], "z_xg%d" % (gi_ % 2)
                P.dma(xt[:, :, 0:n], xsrc[b, :, :, t0:t0 + n].rearrange("c p t -> p c t"), [], [xk])
                self.rms_rstd(xt, xk, n, sq2, rstd, ones, gi_ % 2)
                for k in range(KC):
                    P.op("dve", lambda e, k=k, xt=xt, n=n: e.scalar_tensor_tensor(
                        out=xt[:, k, 0:n], in0=xt[:, k, 0:n], scalar=gf[:, k:k + 1], in1=rstd[:, 0:n], op0=ALU.mult, op1=ALU.mult),
                         ["z_g", "rstd"], [xk])
                o = P.dma(self.outT[b, :, :, t0 - CTX:t0 - CTX + n].rearrange("c p t -> p c t"), xt[:, :, 0:n], [xk], [("out", b)],
                          eng="pool")
                self.final_ops.append(o)
            P.barrier()


def prep_shared(inp):
    sh = {}
    sh["w_mod"] = np.ascontiguousarray(inp["w_mod"].reshape(DEPTH, KC, 128, 6 * D).transpose(0, 2, 1, 3))
    sh["b_modT"] = np.ascontiguousarray(inp["b_mod"].reshape(DEPTH, 48, 128).transpose(0, 2, 1))
    sh["g_mixT"] = np.ascontiguousarray(inp["g_mix"].reshape(DEPTH, KC, 128).transpose(0, 2, 1))
    sh["g_ffnT"] = np.ascontiguousarray(inp["g_ffn"].reshape(DEPTH, KC, 128).transpose(0, 2, 1))
    sh["g_finT"] = np.ascontiguousarray(inp["g_final"].reshape(KC, 128).T)
    sh["w_in"] = np.ascontiguousarray(inp["w_in"].reshape(DEPTH, KC, 128, N_IN).transpose(0, 2, 1, 3))
    b_inF = np.zeros((DEPTH, 128, len(F_CHUNKS)), np.float32)
    for ci, (_, _, col0, _) in enumerate(F_CHUNKS):
        b_inF[:, :, ci] = inp["b_in"][:, col0:col0 + 128]
    sh["b_inF"] = b_inF
    sh["b_in"] = np.ascontiguousarray(inp["b_in"])
    sh["ident"] = np.eye(128, dtype=np.float32)
    sh["hy_f1"] = np.ascontiguousarray(inp["hy_f1"])
    sh["hy_f2"] = np.ascontiguousarray(inp["hy_f2"])
    sh["hy_f3"] = np.ascontiguousarray(inp["hy_f3"])
    sh["hy_fb1"] = np.ascontiguousarray(inp["hy_fb1"].reshape(DEPTH, 64, 1))
    sh["hy_fb2"] = np.ascontiguousarray(inp["hy_fb2"].reshape(DEPTH, 64, 1))
    sh["hy_decay"] = np.ascontiguousarray(inp["hy_decay"])
    sh["hy_convT"] = np.ascontiguousarray(inp["hy_conv"].reshape(DEPTH, 3, 9, 128).transpose(0, 3, 2, 1))
    sh["hy_skipT"] = np.ascontiguousarray(inp["hy_skip"].reshape(DEPTH, 2, 3, 128).transpose(0, 3, 1, 2))
    for tag, L in (("l", LAT), ("c", CTX)):
        for k, v in hyena_consts(L).items():
            sh["hc_%s_%s" % (k, tag)] = v
    sh.update(attn_consts())
    sh["ml_convT"] = np.ascontiguousarray(inp["ml_conv"].reshape(DEPTH, 3, 4, 128).transpose(0, 3, 2, 1))
    sh["ret_decay"] = np.ascontiguousarray(inp["ret_decay"])
    for k in ("w_up", "w_out", "ffn_w1", "ffn_w3", "ffn_w2", "moe_router", "moe_router_b", "moe_w1", "moe_w3", "moe_w2"):
        sh[k] = np.ascontiguousarray(inp[k])
    sel = np.zeros((NEXP, NEXP, 128), np.float32)
    for e_ in range(NEXP):
        sel[e_, e_, :] = 1.0
    sh["moe_sel"] = sel
    sh["ret_decay_h"] = np.ascontiguousarray(inp["ret_decay"].reshape(DEPTH, 2, 3, 2).transpose(0, 1, 3, 2))
    sh["ml_gate_bias"] = np.ascontiguousarray(inp["ml_gate_bias"].reshape(DEPTH, 16))
    return sh


def prep_core(inp, i):
    m = {}
    x = inp["x"][NB * i:NB * i + NB]
    ctx = inp["ctx"][NB * i:NB * i + NB]
    full = np.concatenate([ctx, x], axis=1)
    m["xin"] = np.ascontiguousarray(full.reshape(NB, TAU, KC, 128).transpose(0, 2, 3, 1))
    cc = np.concatenate([inp["c"][NB * i:NB * i + NB], inp["c_ctx"][None, :]], axis=0)
    m["sT"] = np.ascontiguousarray(cc.reshape(3, KC, 128).transpose(2, 1, 0))
    return m


def kernel(**inputs):
    inp = {k: np.asarray(v) for k, v in inputs.items()}
    bld = Builder()
    nc = bld.build()
    sh = prep_shared(inp)
    in_maps = []
    for i in range(NCORES):
        m = dict(sh)
        m.update(prep_core(inp, i))
        in_maps.append({k: v for k, v in m.items() if k in bld.dram})
    res = run_bass_kernel_spmd(nc, in_maps, core_ids=list(range(NCORES)))
    outs = [r["outT"] for r in res.results]
    full = np.concatenate(outs, axis=0)
    return np.ascontiguousarray(full.transpose(0, 3, 1, 2).reshape(NCORES * NB, LAT, D)).astype(np.float32)


def hyena_consts(L):
    N = 2 * L
    kch = L // 128
    pos = np.arange(L, dtype=np.float32)
    t = pos / np.float32(max(L - 1, 1))
    n_bands = 16
    bands = np.linspace(1e-4, n_bands - 1, n_bands, dtype=np.float32)
    z = (np.float32(2.0 * math.pi) * pos / np.float32(L))[:, None] * bands[None, :]
    feats = np.concatenate([t[:, None], np.cos(z), -np.sin(z)], axis=-1).astype(np.float32)
    featsT = np.ascontiguousarray(feats.T)
    tcol = np.ascontiguousarray((-t).reshape(kch, 128).T).astype(np.float32)
    n = np.arange(L, dtype=np.float64)[:, None]
    k = np.arange(L, dtype=np.float64)[None, :]
    th = 2.0 * math.pi * n * (k + 0.5) / N
    C = np.cos(th)
    S = np.sin(th)
    bf = ml_dtypes.bfloat16
    fC = np.ascontiguousarray(C.reshape(kch, 128, kch, 128).transpose(2, 1, 0, 3)).astype(bf)
    fS = np.ascontiguousarray(S.reshape(kch, 128, kch, 128).transpose(2, 1, 0, 3)).astype(bf)
    gsz = min(512, L)
    ng = L // gsz
    CT = (C.T * (2.0 / N))
    ST = (S.T * (2.0 / N))
    iC = np.ascontiguousarray(CT.reshape(kch, 128, ng, gsz).transpose(2, 1, 0, 3)).astype(bf)
    iS = np.ascontiguousarray(ST.reshape(kch, 128, ng, gsz).transpose(2, 1, 0, 3)).astype(bf)
    return {"featsT": featsT, "tcol": tcol, "fC": fC, "fS": fS, "iC": iC, "iS": iS}


def attn_consts():
    c = {}
    GRID_W = 64
    t = np.arange(LAT)
    row_id = (t // GRID_W).astype(np.float32)
    col_id = (t % GRID_W).astype(np.float32)
    q4 = 16
    inv = (1.0 / (np.float32(10000.0) ** (np.arange(q4, dtype=np.float32) / np.float32(q4)))).astype(np.float32)
    cosT = np.zeros((128, LAT), np.float32)
    sinT = np.zeros((128, LAT), np.float32)
    RT = np.zeros((128, 128), np.float32)
    for p in range(128):
        d = p % 64
        j = d % 16
        ang = (row_id if d < 32 else col_id) * inv[j]
        cosT[p] = np.cos(ang)
        sinT[p] = np.sin(ang)
        if (d % 32) < 16:
            RT[p + 16, p] = -1.0
        else:
            RT[p - 16, p] = 1.0
    c["rope_cos"] = cosT
    c["rope_sin"] = sinT
    c["rope_RT"] = RT.astype(ml_dtypes.bfloat16)
    i = np.arange(128)
    s_, t_ = i[:, None], i[None, :]
    c["ac_U"] = (s_ <= t_).astype(np.float32)
    c["ac_L"] = (s_ >= t_).astype(np.float32)
    c["ac_Df"] = np.maximum(t_ - s_, 0).astype(np.float32)
    c["ac_Db"] = np.maximum(s_ - t_, 0).astype(np.float32)
    c["ac_NEGf"] = ((s_ <= t_).astype(np.float32) - 1.0) * 30000.0
    c["ac_NEGb"] = ((s_ >= t_).astype(np.float32) - 1.0) * 30000.0
    io = np.zeros((128, 256), np.float32)
    io[:, 0:128] = (i + 1)[None, :]
    io[:, 128:256] = (128 - i)[None, :]
    c["ac_io"] = io
    c["ac_ioc"] = np.stack([127 - i, i], axis=1).astype(np.float32)
    J = np.zeros((128, 128), np.float32)
    J[0:64, 0:64] = 1.0 / 64
    J[64:128, 64:128] = 1.0 / 64
    c["ac_J"] = J.astype(ml_dtypes.bfloat16)
    return c
```
